# Optimizing a Trainium2 kernel written in Bass

```python
import math
import jax, jax.numpy as jnp
from jax import lax
import numpy as np

D_MODEL = 1024
BATCH = 8
SEQ = 4096
DEPTH = 1

GRID_W = 64
CTX_LEN = 256
RMS_EPS = 1e-6
SHORT_CONV = 3
D_HYENA = 512
HYENA_ORDER = 2
HYENA_EMB_BANDS = 16
HYENA_EMB_DIM = 1 + 2 * HYENA_EMB_BANDS
HYENA_FILTER_HIDDEN = 64
HYENA_DECAY_TARGET = 1e-2
HYENA_MIN_DECAY = math.log(HYENA_DECAY_TARGET) / 1.5
HYENA_MAX_DECAY = math.log(HYENA_DECAY_TARGET) / 0.3
MLSTM_HEADS = 4
MLSTM_HEAD_DIM = 128
D_MLSTM = MLSTM_HEADS * MLSTM_HEAD_DIM
MLSTM_CHUNK = 128
N_BRANCHES = 2
HY_COLS = (HYENA_ORDER + 1) * D_HYENA
ML_COLS = 4 * D_MLSTM
GATE_COLS = 4 * MLSTM_HEADS
MERGE_COLS = N_BRANCHES * D_MODEL
ML_START = HY_COLS
GATE_START = ML_START + ML_COLS
MERGE_START = GATE_START + GATE_COLS
D_IN_PROJ = MERGE_START + MERGE_COLS
N_EXPERTS = 16
EC_CAPACITY_FACTOR = 2
D_FF_EXPERT = 2 * D_MODEL

kernel_name = 'hybrid_hyena_mlstm_ecmoe_dit_layer'


def rmsnorm(x, g):
    x32 = x.astype(jnp.float32)
    y = x32 * lax.rsqrt(jnp.mean(x32 * x32, axis=-1, keepdims=True) + RMS_EPS)
    return y.astype(x.dtype) * g


def modulate(h, shift, scale):
    return h * (1.0 + scale[:, None, :]) + shift[:, None, :]


def short_conv(u, w, rows, row_len):
    B, L, C = u.shape
    K = w.shape[0]
    ur = u.reshape(B, rows, row_len, C)
    up = jnp.pad(ur, ((0, 0), (0, 0), (K // 2, K // 2), (0, 0)))
    y = sum(up[:, :, j:j + row_len] * w[j] for j in range(K))
    return y.reshape(B, L, C)


def hyena_filters(L, w1, b1, w2, b2, w3):
    f32 = lambda a: a.astype(jnp.float32)
    t = jnp.linspace(0.0, 1.0, L, dtype=jnp.float32)[:, None]
    bands = jnp.linspace(1e-4, HYENA_EMB_BANDS - 1, HYENA_EMB_BANDS, dtype=jnp.float32)[None, :]
    ang = (2.0 * math.pi / L) * jnp.arange(L, dtype=jnp.float32)[:, None] * bands
    z = jnp.concatenate([t, jnp.cos(ang), jnp.sin(ang)], axis=-1)
    h = jnp.sin(z @ f32(w1) + f32(b1))
    h = jnp.sin(h @ f32(w2) + f32(b2))
    h = (h @ f32(w3)).reshape(L, HYENA_ORDER, 2, D_HYENA)
    deltas = jnp.abs(jnp.linspace(HYENA_MIN_DECAY, HYENA_MAX_DECAY, D_HYENA, dtype=jnp.float32))
    return h * jnp.exp(-t * deltas)[:, None, None, :]


def bidir_long_conv(u, h, skip):
    B, L, C = u.shape
    k = jnp.concatenate([h[:, 0], jnp.zeros((1, C), h.dtype), h[:0:-1, 1]], axis=0)
    U = jnp.fft.rfft(u.astype(jnp.float32), n=2 * L, axis=1)
    Kf = jnp.fft.rfft(k, n=2 * L, axis=0)
    y = jnp.fft.irfft(U * Kf[None], n=2 * L, axis=1)[:, :L]
    return (y + u.astype(jnp.float32) * skip.astype(jnp.float32)).astype(u.dtype)


def hyena_branch(hy_p, conv_w, f_w1, f_b1, f_w2, f_b2, f_w3, skip, rows, row_len):
    L = hy_p.shape[1]
    parts = jnp.split(short_conv(hy_p, conv_w, rows, row_len), HYENA_ORDER + 1, axis=-1)
    filt = hyena_filters(L, f_w1, f_b1, f_w2, f_b2, f_w3)
    z = parts[0]
    for o in range(HYENA_ORDER):
        z = parts[o + 1] * bidir_long_conv(z, filt[:, o], skip[o])
    return z


def mlstm_chunk_scan(q, k, v, log_i, log_f, state0):
    G, H, L, dh = q.shape
    nc = L // MLSTM_CHUNK

    def chunks(a):
        return jnp.moveaxis(a.reshape(G, H, nc, MLSTM_CHUNK, *a.shape[3:]), 2, 0)

    tril = jnp.tril(jnp.ones((MLSTM_CHUNK, MLSTM_CHUNK), dtype=bool))

    def step(carry, inp):
        C, n, m = carry
        qc, kc, vc, li, lf = inp
        b = jnp.cumsum(lf, axis=-1)
        logw = jnp.where(tril, b[..., :, None] - b[..., None, :] + li[..., None, :], -jnp.inf)
        m_inter = b + m[..., None]
        m_t = jnp.maximum(m_inter, logw.max(-1))
        a_intra = jnp.exp(logw - m_t[..., None]) * jnp.einsum('ghtd,ghsd->ghts', qc, kc)
        a_inter = jnp.exp(m_inter - m_t)
        num = jnp.einsum('ghts,ghse->ghte', a_intra, vc) + a_inter[..., None] * jnp.einsum('ghtd,ghde->ghte', qc, C)
        den = a_intra.sum(-1) + a_inter * jnp.einsum('ghtd,ghd->ght', qc, n)
        h = num / jnp.maximum(jnp.abs(den), jnp.exp(-m_t))[..., None]
        b_end = b[..., -1]
        logw_end = b_end[..., None] - b + li
        m_new = jnp.maximum(b_end + m, logw_end.max(-1))
        w_end = jnp.exp(logw_end - m_new[..., None])
        decay = jnp.exp(b_end + m - m_new)
        C_new = decay[..., None, None] * C + jnp.einsum('ghs,ghsd,ghse->ghde', w_end, kc, vc)
        n_new = decay[..., None] * n + jnp.einsum('ghs,ghsd->ghd', w_end, kc)
        return (C_new, n_new, m_new), h

    state, hs = lax.scan(step, state0, (chunks(q), chunks(k), chunks(v), chunks(log_i), chunks(log_f)))
    return jnp.moveaxis(hs, 0, 2).reshape(G, H, L, dh), state


def mlstm_zero_state(batch):
    g = 2 * batch
    return (jnp.zeros((g, MLSTM_HEADS, MLSTM_HEAD_DIM, MLSTM_HEAD_DIM), jnp.float32),
            jnp.zeros((g, MLSTM_HEADS, MLSTM_HEAD_DIM), jnp.float32),
            jnp.zeros((g, MLSTM_HEADS), jnp.float32))


def mlstm_branch(ml_p, gate_p, conv_w, norm_g, rows, row_len, state0):
    B, L, _ = ml_p.shape
    q_pre, k_pre, v, o = jnp.split(ml_p, 4, axis=-1)
    qk = jax.nn.silu(short_conv(jnp.concatenate([q_pre, k_pre], axis=-1), conv_w, rows, row_len))
    q, k = jnp.split(qk, 2, axis=-1)

    def heads(a):
        return a.reshape(B, L, MLSTM_HEADS, MLSTM_HEAD_DIM).transpose(0, 2, 1, 3).astype(jnp.float32)

    q = heads(q) * (MLSTM_HEAD_DIM ** -0.5)
    k = heads(k)
    v = heads(v)
    g = gate_p.astype(jnp.float32).reshape(B, L, 4, MLSTM_HEADS).transpose(2, 0, 3, 1)
    i_f, f_f, i_b, f_b = g[0], g[1], g[2], g[3]
    flip = lambda a: jnp.flip(a, axis=2)
    q2 = jnp.concatenate([q, flip(q)], axis=0)
    k2 = jnp.concatenate([k, flip(k)], axis=0)
    v2 = jnp.concatenate([v, flip(v)], axis=0)
    log_i = jnp.concatenate([i_f, flip(i_b)], axis=0)
    log_f = jax.nn.log_sigmoid(jnp.concatenate([f_f, flip(f_b)], axis=0))
    h2, state = mlstm_chunk_scan(q2, k2, v2, log_i, log_f, state0)
    h = h2[:B] + flip(h2[B:])
    h = h * lax.rsqrt(jnp.mean(h * h, axis=-1, keepdims=True) + RMS_EPS)
    h = h.transpose(0, 2, 1, 3).reshape(B, L, D_MLSTM).astype(ml_p.dtype) * norm_g
    return h * jax.nn.sigmoid(o), state


def token_mixer(h, w_in, b_in, hy_conv, hy_f_w1, hy_f_b1, hy_f_w2, hy_f_b2, hy_f_w3, hy_skip,
                ml_conv, ml_norm_g, w_branch_hy, w_branch_ml, w_out, rows, row_len, state0):
    p = h @ w_in + b_in
    y_a = hyena_branch(p[..., :ML_START], hy_conv, hy_f_w1, hy_f_b1, hy_f_w2, hy_f_b2, hy_f_w3, hy_skip, rows, row_len)
    y_b, state = mlstm_branch(p[..., ML_START:GATE_START], p[..., GATE_START:MERGE_START], ml_conv, ml_norm_g, rows, row_len, state0)
    g_a, g_b = jnp.split(jax.nn.sigmoid(p[..., MERGE_START:]), N_BRANCHES, axis=-1)
    y = (g_a * (y_a @ w_branch_hy) + g_b * (y_b @ w_branch_ml)) @ w_out
    return y, state


def expert_choice_ffn(h, w_router, w1, w3, w2):
    B, N, D = h.shape
    cap = EC_CAPACITY_FACTOR * N // N_EXPERTS
    aff = jax.nn.softmax((h @ w_router).astype(jnp.float32), axis=-1)
    gate, idx = lax.top_k(jnp.swapaxes(aff, 1, 2), cap)
    bidx = jnp.arange(B)[:, None, None]
    xg = h[bidx, idx]
    a = jnp.einsum('becd,edf->becf', xg, w1)
    b = jnp.einsum('becd,edf->becf', xg, w3)
    ye = jnp.einsum('becf,efd->becd', jax.nn.silu(a) * b, w2) * gate[..., None].astype(h.dtype)
    return jnp.zeros_like(h).at[bidx, idx].add(ye)


def setup_inputs(seed: int = 0) -> dict:
    key = jax.random.key(seed)
    ks = jax.random.split(key, 32)
    D = D_MODEL
    nrm = lambda k, shape, scale: jax.random.normal(k, shape, jnp.float32) * scale
    forget_offset = np.zeros((D_IN_PROJ,), np.float32)
    forget_offset[GATE_START + MLSTM_HEADS:GATE_START + 2 * MLSTM_HEADS] = np.linspace(3.0, 6.0, MLSTM_HEADS)
    forget_offset[GATE_START + 3 * MLSTM_HEADS:GATE_START + 4 * MLSTM_HEADS] = np.linspace(3.0, 6.0, MLSTM_HEADS)
    return {
        'x': nrm(ks[0], (BATCH, SEQ, D), 1.0),
        'c': nrm(ks[1], (BATCH, D), 1.0),
        'ctx': nrm(ks[2], (BATCH, CTX_LEN, D), 1.0),
        'c_ctx': nrm(ks[3], (D,), 1.0),
        'w_ada': nrm(ks[4], (DEPTH, D, 6 * D), 0.5 * D ** -0.5),
        'b_ada': nrm(ks[5], (DEPTH, 6 * D), 0.02),
        'norm_mix_g': 1.0 + nrm(ks[6], (DEPTH, D), 0.02),
        'norm_ffn_g': 1.0 + nrm(ks[7], (DEPTH, D), 0.02),
        'w_in': nrm(ks[8], (DEPTH, D, D_IN_PROJ), D ** -0.5),
        'b_in': nrm(ks[9], (DEPTH, D_IN_PROJ), 0.02) + jnp.asarray(forget_offset),
        'hy_conv': nrm(ks[10], (DEPTH, SHORT_CONV, HY_COLS), 0.5),
        'hy_f_w1': nrm(ks[11], (DEPTH, HYENA_EMB_DIM, HYENA_FILTER_HIDDEN), HYENA_EMB_DIM ** -0.5),
        'hy_f_b1': nrm(ks[12], (DEPTH, HYENA_FILTER_HIDDEN), 0.1),
        'hy_f_w2': nrm(ks[13], (DEPTH, HYENA_FILTER_HIDDEN, HYENA_FILTER_HIDDEN), HYENA_FILTER_HIDDEN ** -0.5),
        'hy_f_b2': nrm(ks[14], (DEPTH, HYENA_FILTER_HIDDEN), 0.1),
        'hy_f_w3': nrm(ks[15], (DEPTH, HYENA_FILTER_HIDDEN, HYENA_ORDER * 2 * D_HYENA), 0.03 * HYENA_FILTER_HIDDEN ** -0.5),
        'hy_skip': nrm(ks[16], (DEPTH, HYENA_ORDER, D_HYENA), 1.0),
        'ml_conv': nrm(ks[17], (DEPTH, SHORT_CONV, 2 * D_MLSTM), 0.5),
        'ml_norm_g': 1.0 + nrm(ks[18], (DEPTH, D_MLSTM), 0.02),
        'w_branch_hy': nrm(ks[19], (DEPTH, D_HYENA, D), D_HYENA ** -0.5),
        'w_branch_ml': nrm(ks[20], (DEPTH, D_MLSTM, D), D_MLSTM ** -0.5),
        'w_out': nrm(ks[21], (DEPTH, D, D), D ** -0.5),
        'w_router': nrm(ks[22], (DEPTH, D, N_EXPERTS), D ** -0.5),
        'w_exp1': nrm(ks[23], (DEPTH, N_EXPERTS, D, D_FF_EXPERT), D ** -0.5),
        'w_exp3': nrm(ks[24], (DEPTH, N_EXPERTS, D, D_FF_EXPERT), D ** -0.5),
        'w_exp2': nrm(ks[25], (DEPTH, N_EXPERTS, D_FF_EXPERT, D), D_FF_EXPERT ** -0.5),
        'final_norm_g': 1.0 + nrm(ks[26], (D,), 0.02),
    }


def reference(x, c, ctx, c_ctx, w_ada, b_ada, norm_mix_g, norm_ffn_g, w_in, b_in,
              hy_conv, hy_f_w1, hy_f_b1, hy_f_w2, hy_f_b2, hy_f_w3, hy_skip,
              ml_conv, ml_norm_g, w_branch_hy, w_branch_ml, w_out,
              w_router, w_exp1, w_exp3, w_exp2, final_norm_g):
    B, L, _ = x.shape
    rows = L // GRID_W
    Lc = ctx.shape[1]
    s_lat = jax.nn.silu(c)
    s_ctx = jax.nn.silu(c_ctx)[None, :]
    for l in range(DEPTH):
        last = l == DEPTH - 1
        ada_x = jnp.split(s_lat @ w_ada[l] + b_ada[l], 6, axis=-1)
        ada_c = jnp.split(s_ctx @ w_ada[l] + b_ada[l], 6, axis=-1)
        mix_params = (w_in[l], b_in[l], hy_conv[l], hy_f_w1[l], hy_f_b1[l], hy_f_w2[l], hy_f_b2[l], hy_f_w3[l],
                      hy_skip[l], ml_conv[l], ml_norm_g[l], w_branch_hy[l], w_branch_ml[l], w_out[l])
        hc = modulate(rmsnorm(ctx, norm_mix_g[l]), ada_c[0], ada_c[1])
        if last:
            pc = hc @ w_in[l][:, ML_START:MERGE_START] + b_in[l][ML_START:MERGE_START]
            _, ctx_state = mlstm_branch(pc[..., :ML_COLS], pc[..., ML_COLS:], ml_conv[l], ml_norm_g[l],
                                        1, Lc, mlstm_zero_state(B))
        else:
            yc, ctx_state = token_mixer(hc, *mix_params, 1, Lc, mlstm_zero_state(B))
            ctx = ctx + ada_c[2][:, None, :] * yc
            hc2 = modulate(rmsnorm(ctx, norm_ffn_g[l]), ada_c[3], ada_c[4])
            ctx = ctx + ada_c[5][:, None, :] * expert_choice_ffn(hc2, w_router[l], w_exp1[l], w_exp3[l], w_exp2[l])
        hx = modulate(rmsnorm(x, norm_mix_g[l]), ada_x[0], ada_x[1])
        yx, _ = token_mixer(hx, *mix_params, rows, GRID_W, ctx_state)
        x = x + ada_x[2][:, None, :] * yx
        hx2 = modulate(rmsnorm(x, norm_ffn_g[l]), ada_x[3], ada_x[4])
        x = x + ada_x[5][:, None, :] * expert_choice_ffn(hx2, w_router[l], w_exp1[l], w_exp3[l], w_exp2[l])
    return rmsnorm(x, final_norm_g)
```

```python
import math
import numpy as np
import ml_dtypes
import concourse.bass as bass
import concourse.mybir as mybir
from concourse.bass_utils import run_bass_kernel_spmd

F32 = mybir.dt.float32
BF16 = mybir.dt.bfloat16
I32 = mybir.dt.int32
U32 = mybir.dt.uint32
AF = mybir.ActivationFunctionType
ALU = mybir.AluOpType
AX = mybir.AxisListType

D = 1024
L = 4096
LC = 256
NTT = L // 128
DIN = 5648
HY0, ML0, GT0, MG0 = 0, 1536, 3584, 3600
NEXP = 16
CAP = 512
DFF = 2048
RMS_EPS = 1e-6
NFFT = 8192

ENGS = ("pe", "act", "dve", "pool", "sp")
N_DMA_SEMS = 12


class _Op:
    __slots__ = ("eng", "emit", "waits", "inc", "is_dma")


class Prog:
    def __init__(self, nc):
        self.nc = nc
        self.ops = {e: [] for e in ENGS}
        self.cnt = {e: 0 for e in ENGS}
        self.known = {e: {} for e in ENGS}
        self.last_w = {}
        self.readers = {}
        self.dma_cnt = {}
        self.dma_rr = {e: 0 for e in ("sp", "act", "pool")}
        self.semobj = {}

    def _need(self, eng, tok, waits):
        if tok is None:
            return
        k, v = tok
        if self.known[eng].get(k, 0) >= v:
            return
        if waits.get(k, 0) < v:
            waits[k] = v

    def _deps(self, eng, reads, writes):
        waits = {}
        for k in reads:
            self._need(eng, self.last_w.get(k), waits)
        for k in writes:
            self._need(eng, self.last_w.get(k), waits)
            for t in self.readers.get(k, ()):
                self._need(eng, t, waits)
        for k, v in waits.items():
            self.known[eng][k] = v
        return waits

    def _commit(self, tok, reads, writes):
        for k in reads:
            self.readers.setdefault(k, []).append(tok)
        for k in writes:
            self.last_w[k] = tok
            self.readers[k] = []

    def op(self, eng, emit, reads=(), writes=()):
        o = _Op()
        o.eng, o.emit, o.is_dma = eng, emit, False
        o.waits = self._deps(eng, reads, writes)
        self.cnt[eng] += 1
        tok = (("c", eng), self.cnt[eng])
        o.inc = tok
        self.ops[eng].append(o)
        self._commit(tok, reads, writes)
        return tok

    def dma(self, eng, emit, reads=(), writes=()):
        o = _Op()
        o.eng, o.emit, o.is_dma = eng, emit, True
        i = self.dma_rr[eng]
        self.dma_rr[eng] = (i + 1) % N_DMA_SEMS
        k = ("d", eng, i)
        waits = self._deps(eng, reads, writes)
        prev = self.dma_cnt.get(k, 0)
        if prev:
            self._need(eng, (k, 16 * prev), waits)
            self.known[eng][k] = max(self.known[eng].get(k, 0), 16 * prev)
        o.waits = waits
        self.dma_cnt[k] = prev + 1
        tok = (k, 16 * (prev + 1))
        o.inc = tok
        self.ops[eng].append(o)
        self._commit(tok, reads, writes)
        return tok

    def barrier(self):
        toks = [(("c", e), self.cnt[e]) for e in ENGS if self.cnt[e]]
        toks += [(k, 16 * n) for k, n in self.dma_cnt.items()]
        for eng in ENGS:
            waits = {}
            for t in toks:
                self._need(eng, t, waits)
            for k, v in waits.items():
                self.known[eng][k] = v
            if waits:
                o = _Op()
                o.eng, o.emit, o.is_dma, o.waits, o.inc = eng, None, False, waits, None
                self.ops[eng].append(o)

    def final_wait(self, eng):
        toks = [(("c", e), self.cnt[e]) for e in ENGS if self.cnt[e]]
        toks += [(k, 16 * n) for k, n in self.dma_cnt.items()]
        waits = {}
        for t in toks:
            self._need(eng, t, waits)
        o = _Op()
        o.eng, o.emit, o.is_dma, o.waits, o.inc = eng, None, False, waits, None
        self.ops[eng].append(o)

    def run(self):
        import contextlib
        nc = self.nc
        keys = [("c", e) for e in ENGS if self.cnt[e]] + list(self.dma_cnt.keys())
        with contextlib.ExitStack() as st:
            for k in keys:
                self.semobj[k] = st.enter_context(nc.semaphore("s_" + "_".join(map(str, k))))
            block = st.enter_context(nc.Block())

            def body(ename):
                def f(eng):
                    for o in self.ops[ename]:
                        for k, v in o.waits.items():
                            eng.wait_ge(self.semobj[k], v)
                        if o.emit is None:
                            continue
                        ins = o.emit(eng)
                        ins.then_inc(self.semobj[o.inc[0]], 16 if o.is_dma else 1)
                return f

            if self.ops["pe"]:
                block.tensor(body("pe"))
            if self.ops["act"]:
                block.scalar(body("act"))
            if self.ops["dve"]:
                block.vector(body("dve"))
            if self.ops["pool"]:
                block.gpsimd(body("pool"))
            if self.ops["sp"]:
                block.sync(body("sp"))


def _inproj_jobs():
    jobs = [(128 * t, 128) for t in range(28)]
    jobs.append((GT0, 16))
    jobs += [(MG0 + 128 * t, 128) for t in range(16)]
    return jobs


JOBS = _inproj_jobs()
NJ = len(JOBS)

COLS = {}
_off = 0
for _name, _n in [("gmix", 8), ("bada", 48), ("bin", NJ), ("hyconv", 36), ("mlconv", 24), ("hyskip", 8),
                  ("mlnorm", 4), ("delta", 4), ("fb1", 1), ("fb2", 1)]:
    COLS[_name] = _off
    _off += _n
NCOLS = _off


def _col(vec, n=None):
    v = np.asarray(vec, np.float32).reshape(-1)
    if v.size % 128:
        v = np.concatenate([v, np.zeros(128 - v.size % 128, np.float32)])
    return np.ascontiguousarray(v.reshape(-1, 128).T)


def _host_consts():
    c = {}
    c["ident_bf"] = np.eye(128, dtype=np.float32).astype(ml_dtypes.bfloat16)
    c["ident_f"] = np.eye(128, dtype=np.float32)
    c["ones_f"] = np.ones((128, 128), np.float32)
    n1 = np.arange(128)[:, None]
    f1 = np.arange(128)[None, :]
    FA = np.exp(-2j * np.pi * (n1 * f1 / 128 + n1 / 256))
    c["fa_re"] = FA.real.astype(np.float32).astype(ml_dtypes.bfloat16)
    c["fa_im"] = FA.imag.astype(np.float32).astype(ml_dtypes.bfloat16)
    f1c = np.arange(128)[:, None]
    n2r = np.arange(64)[None, :]
    Tw = np.exp(-2j * np.pi * (n2r * f1c / 8192 + n2r / 16384))
    c["tw"] = np.concatenate([Tw.real, Tw.imag, -Tw.imag], axis=1).astype(np.float32)
    n2c = np.arange(64)[:, None]
    f2r = np.arange(32)[None, :]
    FB = np.exp(-2j * np.pi * n2c * f2r / 64)
    FBr, FBi = FB.real, FB.imag

    def bd(a, b):
        m = np.zeros((128, 128))
        blk = np.concatenate([a, b], axis=1)
        m[0:64, 0:64] = blk
        m[64:128, 64:128] = blk
        return m

    lb = [bd(FBr, FBi), bd(-FBi, FBr),
          bd(-FBi, FBr), bd(-FBr, -FBi),
          bd(FBr, FBr), bd(-FBi, -FBi),
          bd(FBi, FBi), bd(FBr, FBr)]
    Cm = np.exp(2j * np.pi * np.arange(32)[:, None] * np.arange(64)[None, :] / 64)

    def bdi(top, bot):
        m = np.zeros((128, 128))
        blk = np.concatenate([top, bot], axis=0)
        m[0:64, 0:64] = blk
        m[64:128, 64:128] = blk
        return m

    lb += [bdi(Cm.real, -Cm.imag), bdi(Cm.imag, Cm.real)]
    c["lb"] = np.stack(lb, axis=1).astype(np.float32).astype(ml_dtypes.bfloat16)
    t1 = np.arange(64)[None, None, :]
    f1_ = np.arange(128)[:, None, None]
    t2 = np.arange(64)[None, :, None]
    E = np.exp(2j * np.pi * (t1 * f1_ / 128 + t1 / 256 + t2 * f1_ / 8192 + t2 / 16384)) * (2.0 / NFFT)
    c["ma_re"] = E.real.astype(np.float32).astype(ml_dtypes.bfloat16)
    c["ma_imn"] = (-E.imag).astype(np.float32).astype(ml_dtypes.bfloat16)
    ii = np.arange(128)
    c["tri_f"] = (ii[:, None] <= ii[None, :]).astype(np.float32)
    c["tri_b"] = (ii[:, None] >= ii[None, :]).astype(np.float32)
    NEG = -1.0e30
    mf_ = np.where(ii[None, :] <= ii[:, None], 0.0, NEG).astype(np.float32)
    mb_ = np.where(ii[None, :] >= ii[:, None], 0.0, NEG).astype(np.float32)
    c["mask4_f"] = np.tile(mf_, (1, 4))
    c["mask4_b"] = np.tile(mb_, (1, 4))
    gsel = np.zeros((16, 2), np.float32)
    gsel[[0, 1, 2, 3, 8, 9, 10, 11], 0] = 1.0
    gsel[[4, 5, 6, 7, 12, 13, 14, 15], 1] = -1.0
    c["gsel"] = gsel
    pos = np.arange(NFFT)
    pos = np.where(pos < L, pos, NFFT - pos)
    pos[L] = 0
    tlin = np.linspace(0.0, 1.0, L, dtype=np.float32)
    bands = np.linspace(1e-4, 15, 16, dtype=np.float32)[None, :]
    ang = (np.float32(2.0 * math.pi / L) * np.arange(L, dtype=np.float32)[:, None]) * bands
    z = np.concatenate([tlin[:, None], np.cos(ang), np.sin(ang)], axis=-1).astype(np.float32)
    c["zext"] = np.ascontiguousarray(z[pos].T)
    c["text"] = np.ascontiguousarray(np.broadcast_to(tlin[pos][None, :], (128, NFFT))).astype(np.float32)
    return c


def _delta_col():
    mn = math.log(1e-2) / 1.5
    mx = math.log(1e-2) / 0.3
    d = np.abs(np.linspace(mn, mx, 512, dtype=np.float32))
    return _col(-d)


class K:
    pass


def _esz(dt):
    return 2 if dt == BF16 else 4


class Arena:
    def __init__(self, nc, nbytes):
        self.t = nc.alloc_sbuf_tensor("arena", [128, nbytes // 2], BF16)[:]
        self.off = 0
        self.cap = nbytes
        self.peak = 0

    def mark(self):
        return self.off

    def release(self, m):
        self.off = m

    def alloc(self, shape, dt):
        n = int(np.prod(shape[1:]))
        nb = (n * _esz(dt) + 31) // 32 * 32
        assert self.off + nb <= self.cap, f"arena overflow: need {nb} at {self.off} cap {self.cap}"
        v = self.t[:, self.off // 2:(self.off + nb) // 2]
        self.off += nb
        self.peak = max(self.peak, self.off)
        if dt != BF16:
            v = v.bitcast(dt)
        v = v[:, 0:n]
        if len(shape) > 2:
            names = [f"a{i}" for i in range(len(shape) - 1)]
            kw = {nm: int(sz) for nm, sz in zip(names[:-1], shape[1:-1])}
            v = v.rearrange("p (" + " ".join(names) + ") -> p " + " ".join(names), **kw)
        return v[0:shape[0]]


def build(debug=None, cut=99):
    nc = bass.Bass("TRN2", target_bir_lowering=False)
    P = Prog(nc)
    k = K()
    k.nc, k.P, k.debug = nc, P, debug
    dbg_kind = "ExternalOutput" if debug else "Internal"

    in_names = []
    k.in_names = in_names
    nc._k = k

    def din(name, shape, dt=F32):
        in_names.append(name)
        return nc.dram_tensor(name, list(shape), dt, kind="ExternalInput").ap()

    DBG_OUT = {"inproj": ("pT", "pcT"), "hyena": ("yaT",), "mlstm": ("yaT", "ybT"), "merge": ("x1d", "hx2d", "affd"),
               "route": ("idxd", "gated"), "moe": ("x1d",), "moe1": ("x1d",)}

    def dscr(name, shape, dt):
        kind = "ExternalOutput" if (debug and name in DBG_OUT.get(debug, ())) else "Internal"
        return nc.dram_tensor(name, list(shape), dt, kind=kind).ap()

    x = din("x", [L, D])
    cc = din("c", [128, 8])
    cctx = din("c_ctx", [128, 8])
    ctx = din("ctx", [LC, D])
    w_ada = din("w_ada", [D, 6 * D])
    bada_row = din("bada_row", [2, 6 * D])
    cols_d = din("cols", [128, NCOLS])
    rows_d = din("rows", [1, 2 * D])
    w_in = din("w_in", [D, DIN])
    f_w1 = din("f_w1", [33, 64])
    f_w2 = din("f_w2", [64, 64])
    f_w3 = din("f_w3", [64, 2048])
    w_bh = din("w_bh", [512, D])
    w_bm = din("w_bm", [512, D])
    w_out = din("w_out", [D, D])
    w_rt = din("w_rt", [D, NEXP])
    if debug in (None, "moe", "moe1", "all"):
        w_e1 = din("w_e1", [NEXP, D, DFF])
        w_e3 = din("w_e3", [NEXP, D, DFF])
        w_e2 = din("w_e2", [NEXP, DFF, D])
    cst = {}
    hc = _host_consts()
    k.hc = hc
    for name, arr in hc.items():
        cst[name] = din("k_" + name, arr.shape, BF16 if arr.dtype == ml_dtypes.bfloat16 else F32)
    out = nc.dram_tensor("out", [L, D], F32, kind="ExternalOutput").ap()

    pT = dscr("pT", [NJ * 128, L], BF16)
    pcT = dscr("pcT", [17 * 128, LC], BF16)
    gTf = dscr("gTf", [16, L], F32)
    gcTf = dscr("gcTf", [16, LC], F32)

    def sb(name, shape, dt):
        return nc.alloc_sbuf_tensor(name, list(shape), dt)[:]

    ar = Arena(nc, 196 * 1024)
    k.ar = ar
    ps_f = [nc.alloc_psum_tensor(f"psf{i}", [128, 512], F32)[:] for i in range(6)]
    ps_b = [nc.alloc_psum_tensor(f"psb{i}", [128, 1024], BF16)[:] for i in range(2)]
    PSF = [f"psf{i}" for i in range(6)]
    PSB = [f"psb{i}" for i in range(2)]
    k.ps_f, k.ps_b, k.PSF, k.PSB = ps_f, ps_b, PSF, PSB

    ident_bf = sb("ident_bf", [128, 128], BF16)
    ident_f = sb("ident_f", [128, 128], F32)
    ones_f = sb("ones_f", [128, 128], F32)
    colt = sb("colt", [128, NCOLS], F32)
    adaT = sb("adaT", [128, 48, 2], F32)
    G1 = sb("G1", [128, 8, 2], F32)
    ssq = sb("ssq", [128, NTT + 2], F32)
    rstd = sb("rstd", [128, NTT + 2], F32)
    P.dma("sp", lambda e: e.dma_start(out=ident_bf, in_=cst["ident_bf"]), writes=["ident_bf"])
    P.dma("sp", lambda e: e.dma_start(out=ident_f, in_=cst["ident_f"]), writes=["ident_f"])
    P.dma("sp", lambda e: e.dma_start(out=ones_f, in_=cst["ones_f"]), writes=["ones_f"])
    P.dma("sp", lambda e: e.dma_start(out=colt, in_=cols_d), writes=["colt"])
    k.ident_bf, k.ident_f, k.ones_f, k.colt = ident_bf, ident_f, ones_f, colt

    def C(name, j=0, n=1):
        o = COLS[name] + j
        return colt[:, o:o + n]
    k.C = C

    def finish(dump=None, shape=None, dt=F32):
        print("arena peak", ar.peak, {e_: len(v_) for e_, v_ in P.ops.items()})
        P.barrier()
        if dump is not None:
            dd = nc.dram_tensor("dbg_dump", list(shape), dt, kind="ExternalOutput").ap()
            P.dma("sp", lambda e: e.dma_start(out=dd, in_=dump), writes=["dbg_dump"])
        P.final_wait("sp")
        P.run()
        return nc
    k.finish = finish

    bc = {}
    for name in ("ada2", "ada5", "S2", "G2", "fing"):
        bc[name] = ar.alloc([128, D], F32)
    k.bc = bc

    mA = ar.mark()
    cA = ar.alloc([128, 8], F32)
    cB = ar.alloc([128, 8], F32)
    s2 = ar.alloc([128, 8, 2], BF16)
    R = ar.alloc([2, 6 * D], F32)
    brow = ar.alloc([2, 6 * D], F32)
    wab = [ar.alloc([128, 8, 1024], BF16) for i in range(2)]
    rowt = ar.alloc([1, 2 * D], F32)
    g2row = ar.alloc([1, D], F32)
    P.dma("sp", lambda e: e.dma_start(out=cA, in_=cc), writes=["cA"])
    P.dma("sp", lambda e: e.dma_start(out=cB, in_=cctx), writes=["cB"])
    P.op("act", lambda e: e.activation(out=s2[:, :, 0], in_=cA, func=AF.Silu), reads=["cA"], writes=["s2a"])
    P.op("act", lambda e: e.activation(out=s2[:, :, 1], in_=cB, func=AF.Silu), reads=["cB"], writes=["s2b"])
    P.dma("sp", lambda e: e.dma_start(out=brow, in_=bada_row), writes=["brow"])
    P.dma("sp", lambda e: e.dma_start(out=rowt, in_=rows_d), writes=["rowt"])
    def _cut_here():
        dbgc = nc.dram_tensor("dbg_cut", [128, 16], BF16, kind="ExternalOutput").ap()
        P.barrier()
        P.dma("sp", lambda e: e.dma_start(out=dbgc, in_=s2.rearrange("p a b -> p (a b)")), writes=["dbgc"])
        P.final_wait("sp")
        P.run()
        return nc
    if cut == 1:
        return _cut_here()
    wada_v = w_ada.rearrange("(p dc) n -> p dc n", dc=8)
    for i in range(6 if cut > 2 else 1):
        wb = wab[i % 2]
        wk = f"wab{i % 2}"
        P.dma("pool", lambda e, wb=wb, i=i: e.dma_start(out=wb, in_=wada_v[:, :, i * 1024:(i + 1) * 1024]),
              writes=[wk])
        for h in range(2):
            pst, pk = ps_f[h], PSF[h]

            def mm_row(e, wb=wb, h=h, pst=pst):
                ins = None
                for dc in range(8):
                    ins = e.matmul(pst[0:2, :], lhsT=s2[:, dc, :], rhs=wb[:, dc, h * 512:(h + 1) * 512],
                                   start=(dc == 0), stop=(dc == 7))
                return ins
            P.op("pe", mm_row, reads=[wk, "s2a", "s2b"], writes=[pk])
            sl = slice(i * 1024 + h * 512, i * 1024 + (h + 1) * 512)
            P.op("dve", lambda e, pst=pst, sl=sl: e.tensor_tensor(out=R[:, sl], in0=pst[0:2, :], in1=brow[:, sl],
                                                                 op=ALU.add),
                 reads=[pk, "brow"], writes=[("R", i, h)])
        pst, pk = ps_f[2 + (i % 2)], PSF[2 + (i % 2)]

        def mm_col(e, wb=wb, pst=pst):
            ins = None
            for dco in range(8):
                for dc in range(8):
                    ins = e.matmul(pst[:, 2 * dco:2 * dco + 2], lhsT=wb[:, dc, dco * 128:(dco + 1) * 128],
                                   rhs=s2[:, dc, :], start=(dc == 0), stop=(dc == 7))
            return ins
        P.op("pe", mm_col, reads=[wk, "s2a", "s2b"], writes=[pk])
        P.op("dve", lambda e, pst=pst, i=i: e.tensor_tensor(
            out=adaT[:, i * 8:(i + 1) * 8, :], in0=pst[:, 0:16].rearrange("p (a b) -> p a b", b=2),
            in1=C("bada", i * 8, 8).unsqueeze(2).to_broadcast([128, 8, 2]), op=ALU.add),
            reads=[pk, "colt"], writes=[("adaT", i)])

    if cut in (2, 3):
        return _cut_here()
    P.op("dve", lambda e: e.tensor_scalar(out=G1, in0=adaT[:, 8:16, :], scalar1=1.0, scalar2=None, op0=ALU.add),
         reads=[("adaT", 1)], writes=["G1"])
    P.op("dve", lambda e: e.tensor_tensor(out=G1, in0=G1, in1=C("gmix", 0, 8).unsqueeze(2).to_broadcast([128, 8, 2]),
                                          op=ALU.mult), reads=["G1", "colt"], writes=["G1"])
    k.adaT, k.G1 = adaT, G1

    if cut == 4:
        return _cut_here()
    P.op("dve", lambda e: e.tensor_scalar(out=g2row, in0=R[0:1, 4 * D:5 * D], scalar1=1.0, scalar2=None, op0=ALU.add),
         reads=[("R", 4, 0), ("R", 4, 1)], writes=["g2row"])
    P.op("dve", lambda e: e.tensor_tensor(out=g2row, in0=g2row, in1=rowt[:, 0:D], op=ALU.mult),
         reads=["g2row", "rowt"], writes=["g2row"])
    srcs = {"ada2": (R[0:1, 2 * D:3 * D], [("R", 2, 0), ("R", 2, 1)]),
            "ada5": (R[0:1, 5 * D:6 * D], [("R", 5, 0), ("R", 5, 1)]),
            "S2": (R[0:1, 3 * D:4 * D], [("R", 3, 0), ("R", 3, 1)]),
            "G2": (g2row[:, :], ["g2row"]),
            "fing": (rowt[:, D:2 * D], ["rowt"])}
    for bi, (name, (src, rk)) in enumerate(srcs.items()):
        for h in range(2):
            pst, pk = ps_f[(2 * bi + h) % 4], PSF[(2 * bi + h) % 4]
            P.op("pe", lambda e, pst=pst, src=src, h=h: e.matmul(pst, lhsT=ones_f[0:1, :], rhs=src[:, h * 512:(h + 1) * 512],
                                                                start=True, stop=True),
                 reads=rk + ["ones_f"], writes=[pk])
            P.op("act", lambda e, pst=pst, name=name, h=h: e.copy(out=bc[name][:, h * 512:(h + 1) * 512], in_=pst),
                 reads=[pk], writes=[("bc", name, h)])
    if debug == "ada":
        dbg = nc.dram_tensor("dbg_adaT", [128, 96], F32, kind="ExternalOutput").ap()
        P.dma("sp", lambda e: e.dma_start(out=dbg, in_=adaT.rearrange("p a b -> p (a b)")),
              reads=[("adaT", i) for i in range(6)], writes=["dbg"])
        dbg3 = nc.dram_tensor("dbg_bc", [128, D], F32, kind="ExternalOutput").ap()
        P.dma("sp", lambda e: e.dma_start(out=dbg3, in_=bc["G2"]), reads=[("bc", "G2", 0), ("bc", "G2", 1)], writes=["dbg3"])
        P.final_wait("sp")
        P.run()
        return nc
    P.barrier()
    ar.release(mA)

    mB = ar.mark()
    hT = ar.alloc([128, 8, L], BF16)
    hcT = ar.alloc([128, 8, LC], BF16)
    xt = [ar.alloc([128, D], F32) for i in range(3)]
    junk = ar.alloc([128, D], BF16)
    ybf = [ar.alloc([128, D], BF16) for i in range(2)]
    x_t = x.rearrange("(n p) d -> n p d", p=128)
    ctx_t = ctx.rearrange("(n p) d -> n p d", p=128)
    srcs1 = [x_t[i] for i in range(NTT)] + [ctx_t[i] for i in range(2)]
    for i, src in enumerate(srcs1):
        xb_, xk = xt[i % 3], f"xt{i % 3}"
        P.dma("sp", lambda e, xb_=xb_, src=src: e.dma_start(out=xb_, in_=src), writes=[xk])
        P.op("act", lambda e, xb_=xb_, i=i: e.activation(out=junk, in_=xb_, func=AF.Square, accum_out=ssq[:, i:i + 1]),
             reads=[xk], writes=["junk", ("ssq", i)])
    if cut == 10:
        return finish(ssq, [128, NTT + 2])
    allss = [("ssq", i) for i in range(NTT + 2)]
    P.op("dve", lambda e: e.tensor_scalar(out=rstd, in0=ssq, scalar1=1.0 / D, scalar2=RMS_EPS, op0=ALU.mult, op1=ALU.add),
         reads=allss, writes=["rstd"])
    P.op("act", lambda e: e.activation(out=rstd, in_=rstd, func=AF.Sqrt), reads=["rstd"], writes=["rstd"])
    P.op("dve", lambda e: e.reciprocal(out=rstd, in_=rstd), reads=["rstd"], writes=["rstd"])
    if cut == 11:
        return finish(rstd, [128, NTT + 2])
    for i, src in enumerate(srcs1):
        if cut == 12 and i >= 2:
            break
        xb_, xk = xt[i % 3], f"xt{i % 3}"
        yb_, yk = ybf[i % 2], f"ybf{i % 2}"
        pb, pbk = ps_b[i % 2], PSB[i % 2]
        P.dma("sp", lambda e, xb_=xb_, src=src: e.dma_start(out=xb_, in_=src), writes=[xk])
        P.op("dve", lambda e, xb_=xb_, yb_=yb_, i=i: e.tensor_scalar(out=yb_, in0=xb_, scalar1=rstd[:, i:i + 1], scalar2=None,
                                                                  op0=ALU.mult), reads=[xk, "rstd"], writes=[yk])

        def tr8(e, yb_=yb_, pb=pb):
            ins = None
            for dc in range(8):
                ins = e.transpose(pb[:, dc * 128:(dc + 1) * 128], yb_[:, dc * 128:(dc + 1) * 128], ident_bf)
            return ins
        P.op("pe", tr8, reads=[yk, "ident_bf"], writes=[pbk])
        lat = i < NTT
        for dc in range(8):
            if lat:
                dst = hT[:, dc, i * 128:(i + 1) * 128]
                wkey = ("hT", i // 4, dc, i % 4)
            else:
                dst = hcT[:, dc, (i - NTT) * 128:(i - NTT + 1) * 128]
                wkey = ("hcT", 0, dc, i - NTT)
            col = 0 if lat else 1
            if i % 2 == 0:
                P.op("act", lambda e, dst=dst, pb=pb, dc=dc, col=col: e.activation(
                    out=dst, in_=pb[:, dc * 128:(dc + 1) * 128], func=AF.Identity,
                    scale=G1[:, dc, col:col + 1], bias=adaT[:, dc, col:col + 1]),
                    reads=[pbk, "G1", ("adaT", 0)], writes=[wkey])
            else:
                P.op("dve", lambda e, dst=dst, pb=pb, dc=dc, col=col: e.tensor_scalar(
                    out=dst, in0=pb[:, dc * 128:(dc + 1) * 128], scalar1=G1[:, dc, col:col + 1],
                    scalar2=adaT[:, dc, col:col + 1], op0=ALU.mult, op1=ALU.add),
                    reads=[pbk, "G1", ("adaT", 0)], writes=[wkey])

    if cut in (12, 13):
        return finish(hT[:, 0, 0:256], [128, 256], BF16)
    win_v = w_in.rearrange("(dc p) n -> p dc n", p=128)
    wib = [ar.alloc([128, 8, 512], BF16) for i in range(2)]
    pacc = [ar.alloc([128, L], BF16) for i in range(2)]
    gacc = ar.alloc([16, L], F32)
    groups_lat = [list(range(4 * g, 4 * g + 4)) for g in range(7)] + [[28]] + \
                 [list(range(29 + 4 * g, 29 + 4 * g + 4)) for g in range(4)]
    groups_ctx = [list(range(12 + 4 * g, 12 + 4 * g + 4)) for g in range(4)] + [[28]]
    cnt = {"g": 0, "t": 0}

    def inproj(groups, src_T, src_key, ntok, dst_dram, row_of):
        nblk = max(1, ntok // 512)
        bw = min(512, ntok)
        nsub = bw // 128
        for grp in groups:
            gc0 = JOBS[grp[0]][0]
            gn = sum(JOBS[j][1] for j in grp)
            wb, wk = wib[cnt["g"] % 2], f"wib{cnt['g'] % 2}"
            cnt["g"] += 1
            P.dma("pool", lambda e, wb=wb, gc0=gc0, gn=gn: e.dma_start(out=wb[:, :, 0:gn], in_=win_v[:, :, gc0:gc0 + gn]),
                  writes=[wk])
            for j in grp:
                c0, n = JOBS[j]
                o = c0 - gc0
                pa, pak = pacc[cnt["t"] % 2], f"pacc{cnt['t'] % 2}"
                cnt["t"] += 1
                is_sig = (c0 >= MG0) or (ML0 + 1536 <= c0 < GT0)
                for tb in range(nblk):
                    pst, pk = ps_f[tb % 4], PSF[tb % 4]

                    def mm(e, wb=wb, n=n, o=o, tb=tb, pst=pst):
                        ins = None
                        for dc in range(8):
                            ins = e.matmul(pst[0:n, 0:bw], lhsT=wb[:, dc, o:o + n], rhs=src_T[:, dc, tb * bw:(tb + 1) * bw],
                                           start=(dc == 0), stop=(dc == 7))
                        return ins
                    rk = [(src_key, tb, dc, q) for dc in range(8) for q in range(nsub)]
                    P.op("pe", mm, reads=[wk] + rk, writes=[pk])
                    if j == 28:
                        P.op("act", lambda e, pst=pst, n=n, tb=tb, j=j: e.activation(
                            out=gacc[0:n, tb * bw:(tb + 1) * bw], in_=pst[0:n, 0:bw], func=AF.Identity, bias=C("bin", j)[0:n, :]),
                            reads=[pk, "colt"], writes=[("gacc", tb)])
                        continue
                    P.op("act", lambda e, pa=pa, pst=pst, n=n, tb=tb, j=j, is_sig=is_sig: e.activation(
                        out=pa[0:n, tb * bw:(tb + 1) * bw], in_=pst[0:n, 0:bw],
                        func=(AF.Sigmoid if is_sig else AF.Identity), bias=C("bin", j)[0:n, :]),
                        reads=[pk, "colt"], writes=[(pak, tb)])
                if j == 28:
                    gd = gTf if dst_dram is pT else gcTf
                    P.dma("sp", lambda e, gd=gd: e.dma_start(out=gd, in_=gacc[:, 0:ntok]),
                          reads=[("gacc", tb) for tb in range(nblk)], writes=[("gTf", ntok)])
                    continue
                r0 = row_of(j)
                P.dma("sp", lambda e, pa=pa, n=n, r0=r0: e.dma_start(out=dst_dram[r0:r0 + n, :], in_=pa[0:n, 0:ntok]),
                      reads=[(pak, tb) for tb in range(nblk)], writes=[("pT" if dst_dram is pT else "pcT", r0)])

    if cut == 14:
        inproj(groups_ctx[:1], hcT, "hcT", LC, pcT, lambda j: (j - 12) * 128)
        return finish(pacc[0][:, 0:256], [128, 256], BF16)
    if cut == 15:
        inproj(groups_ctx, hcT, "hcT", LC, pcT, lambda j: (j - 12) * 128)
        return finish(pacc[0][:, 0:256], [128, 256], BF16)
    if cut == 16:
        inproj(groups_lat[:1], hT, "hT", L, pT, lambda j: j * 128)
        return finish(pacc[0][:, 0:256], [128, 256], BF16)
    inproj(groups_ctx, hcT, "hcT", LC, pcT, lambda j: (j - 12) * 128)
    inproj(groups_lat, hT, "hT", L, pT, lambda j: j * 128)
    k.pT, k.pcT = pT, pcT

    if debug == "inproj":
        return finish()
    P.barrier()
    ar.release(mB)

    TWO_PI = 2.0 * math.pi
    MAGIC = 12582912.0
    lock = lambda pk: ("lock", pk)

    KF = dscr("KF", [8, 2, 128, NFFT], BF16)
    yaT = dscr("yaT", [512, L], BF16)
    k.KF, k.yaT = KF, yaT
    mH = ar.mark()
    fa_re = ar.alloc([128, 128], BF16)
    fa_im = ar.alloc([128, 128], BF16)
    tw = ar.alloc([128, 192], F32)
    lb = ar.alloc([128, 10, 128], BF16)
    ma_re = ar.alloc([128, 64, 64], BF16)
    ma_imn = ar.alloc([128, 64, 64], BF16)
    for t_, nm in ((fa_re, "fa_re"), (fa_im, "fa_im"), (tw, "tw"), (lb, "lb"), (ma_re, "ma_re"), (ma_imn, "ma_imn")):
        P.dma("sp", lambda e, t_=t_, nm=nm: e.dma_start(out=t_, in_=cst[nm]), writes=[nm])
    R1 = ar.alloc([128, 8192], BF16)
    R2 = ar.alloc([128, 8192], BF16)
    BA = ar.alloc([128, 16384], BF16)
    BB = ar.alloc([128, 16384], BF16)
    mH1 = ar.mark()
    h2T = ar.alloc([64, NFFT], BF16)
    w1f = ar.alloc([33, 64], F32)
    w2f = ar.alloc([64, 64], F32)
    w3b = ar.alloc([64, 2048], BF16)
    w3n = ar.alloc([64, 2048], BF16)
    dsc = ar.alloc([128, 4], F32)
    zb = [ar.alloc([33, 512], F32) for _ in range(2)]
    sx = [ar.alloc([64, 512], F32) for _ in range(3)]
    itile = [ar.alloc([128, 512], F32) for _ in range(2)]
    dtile = [ar.alloc([128, 512], F32) for _ in range(2)]
    tmpf = [ar.alloc([128, 512], F32) for _ in range(4)]
    kst = [ar.alloc([128, 2, 512], BF16) for _ in range(2)]
    P.dma("sp", lambda e: e.dma_start(out=w1f, in_=f_w1), writes=["w1f"])
    P.dma("sp", lambda e: e.dma_start(out=w2f, in_=f_w2), writes=["w2f"])
    P.dma("pool", lambda e: e.dma_start(out=w3b, in_=f_w3), writes=["w3b"])
    P.op("dve", lambda e: e.tensor_scalar(out=w3n, in0=w3b, scalar1=-1.0, scalar2=None, op0=ALU.mult),
         reads=["w3b"], writes=["w3n"])
    P.op("dve", lambda e: e.tensor_scalar(out=dsc, in0=C("delta", 0, 4), scalar1=1.0 / (L - 1), scalar2=None, op0=ALU.mult),
         reads=["colt"], writes=["dsc"])

    def sin_chain(pst, pk, bias_col, out_ap, okey, q):
        a, b_, c_ = sx[0], sx[1], sx[2]
        P.op("dve", lambda e: e.tensor_scalar(out=a, in0=pst[0:64, :], scalar1=bias_col, scalar2=None, op0=ALU.add),
             reads=[pk, "colt"], writes=["sx0", lock(pk)])
        P.op("dve", lambda e: e.tensor_scalar(out=b_, in0=a, scalar1=1.0 / TWO_PI, scalar2=MAGIC, op0=ALU.mult, op1=ALU.add),
             reads=["sx0"], writes=["sx1"])
        P.op("dve", lambda e: e.tensor_scalar(out=c_, in0=b_, scalar1=MAGIC, scalar2=None, op0=ALU.subtract),
             reads=["sx1"], writes=["sx2"])
        P.op("dve", lambda e: e.scalar_tensor_tensor(out=b_, in0=c_, scalar=-TWO_PI, in1=a, op0=ALU.mult, op1=ALU.add),
             reads=["sx2", "sx0"], writes=["sx1"])
        P.op("dve", lambda e: e.tensor_scalar(out=b_, in0=b_, scalar1=-math.pi, scalar2=math.pi, op0=ALU.max, op1=ALU.min),
             reads=["sx1"], writes=["sx1"])
        P.op("act", lambda e: e.activation(out=out_ap, in_=b_, func=AF.Sin), reads=["sx1"], writes=[okey])

    h1t = ar.alloc([64, 512], F32)
    for blk in range(16):
        zt, zk = zb[blk % 2], f"zb{blk % 2}"
        P.dma("sp", lambda e, zt=zt, blk=blk: e.dma_start(out=zt, in_=cst["zext"][:, blk * 512:(blk + 1) * 512]), writes=[zk])
        P.op("pe", lambda e, zt=zt: e.matmul(ps_f[0][0:64, :], lhsT=w1f, rhs=zt, start=True, stop=True),
             reads=["w1f", zk], writes=[PSF[0]])
        sin_chain(ps_f[0], PSF[0], C("fb1")[0:64, :], h1t, "h1t", 0)
        P.op("pe", lambda e: e.matmul(ps_f[1][0:64, :], lhsT=w2f, rhs=h1t, start=True, stop=True),
             reads=["w2f", "h1t"], writes=[PSF[1]])
        sin_chain(ps_f[1], PSF[1], C("fb2")[0:64, :], h2T[:, blk * 512:(blk + 1) * 512], ("h2T", blk), 1)

    ktil = R1
    Uf = R2.rearrange("p (a b) -> p a b", b=128)
    Aa = BA.rearrange("p (r c n) -> p r c n", r=2, n=64)
    At = BB.rearrange("p (r q f) -> p r q f", r=2, f=128)

    def fft_A(U_, ukey, K_):
        for g in range(16):
            pre, prk = ps_f[(g % 2) * 2], PSF[(g % 2) * 2]
            pim, pik = ps_f[(g % 2) * 2 + 1], PSF[(g % 2) * 2 + 1]

            def mmA(e, g=g, pre=pre, pim=pim):
                ins = None
                for j in range(4):
                    e.matmul(pre[:, j * 128:(j + 1) * 128], lhsT=fa_re[0:K_, :], rhs=U_[0:K_, 4 * g + j, :], start=True, stop=True)
                    ins = e.matmul(pim[:, j * 128:(j + 1) * 128], lhsT=fa_im[0:K_, :], rhs=U_[0:K_, 4 * g + j, :], start=True, stop=True)
                return ins
            P.op("pe", mmA, reads=[(ukey, g), "fa_re", "fa_im"], writes=[prk, pik])
            n0 = 4 * g
            Trb = tw[:, n0:n0 + 4].unsqueeze(2).to_broadcast([128, 4, 128])
            Tib = tw[:, 64 + n0:64 + n0 + 4].unsqueeze(2).to_broadcast([128, 4, 128])
            pre3 = pre.rearrange("p (a b) -> p a b", b=128)
            pim3 = pim.rearrange("p (a b) -> p a b", b=128)
            tb_ = (g % 2) * 4 if len(tmpf) >= 8 else 0
            ms = [tmpf[tb_ + q_] for q_ in range(4)]
            mk = [f"tmpf{tb_ + q_}" for q_ in range(4)]
            m3v = [m_.rearrange("p (a b) -> p a b", b=128) for m_ in ms]
            P.op("dve", lambda e, m3v=m3v, pre3=pre3, Trb=Trb: e.tensor_tensor(out=m3v[0], in0=pre3, in1=Trb, op=ALU.mult),
                 reads=[prk, "tw"], writes=[mk[0], lock(prk)])
            P.op("dve", lambda e, m3v=m3v, pim3=pim3, Tib=Tib: e.tensor_tensor(out=m3v[1], in0=pim3, in1=Tib, op=ALU.mult),
                 reads=[pik, "tw"], writes=[mk[1], lock(pik)])
            P.op("dve", lambda e, m3v=m3v, pre3=pre3, Tib=Tib: e.tensor_tensor(out=m3v[2], in0=pre3, in1=Tib, op=ALU.mult),
                 reads=[prk, "tw"], writes=[mk[2], lock(prk)])
            P.op("dve", lambda e, m3v=m3v, pim3=pim3, Trb=Trb: e.tensor_tensor(out=m3v[3], in0=pim3, in1=Trb, op=ALU.mult),
                 reads=[pik, "tw"], writes=[mk[3], lock(pik)])
            mp = [m_.rearrange("p (a b) -> p b a", b=128) for m_ in ms]
            P.op("pool", lambda e, mp=mp, n0=n0: e.tensor_tensor(out=Aa[:, 0, :, n0:n0 + 4], in0=mp[0], in1=mp[1], op=ALU.subtract),
                 reads=[mk[0], mk[1]], writes=[("Aa", 0, n0 + q_) for q_ in range(4)])
            P.op("pool", lambda e, mp=mp, n0=n0: e.tensor_tensor(out=Aa[:, 1, :, n0:n0 + 4], in0=mp[2], in1=mp[3], op=ALU.add),
                 reads=[mk[2], mk[3]], writes=[("Aa", 1, n0 + q_) for q_ in range(4)])

    def fft_T1():
        bi = 0
        for ri in range(2):
            for q in range(8):
                pb, pbk = ps_b[bi % 2], PSB[bi % 2]

                def trT(e, ri=ri, q=q, pb=pb):
                    ins = None
                    for j in range(8):
                        p_ = 8 * q + j
                        ins = e.transpose(pb[:, j * 128:(j + 1) * 128],
                                          Aa[:, ri, 2 * p_:2 * p_ + 2, :].rearrange("p a b -> p (a b)"), ident_bf)
                    return ins
                P.op("pe", trT, reads=[("Aa", ri, n2) for n2 in range(64)] + ["ident_bf"], writes=[pbk])
                dst = At[:, ri, 8 * q:8 * q + 8, :].rearrange("p a b -> p (a b)")
                if bi % 2 == 0:
                    P.op("act", lambda e, dst=dst, pb=pb: e.copy(out=dst, in_=pb), reads=[pbk], writes=[("At", ri, q), lock(pbk)])
                else:
                    P.op("dve", lambda e, dst=dst, pb=pb: e.tensor_copy(out=dst, in_=pb), reads=[pbk], writes=[("At", ri, q), lock(pbk)])
                bi += 1

    for o in range(2):
        for ct in range(4):
            fidx = o * 4 + ct
            for blk in range(16):
                side = blk // 8
                col0 = o * 1024 + side * 512 + ct * 128
                w3x, w3k = (w3b, "w3b") if side == 0 else (w3n, "w3n")
                pst, pk = ps_f[4 + blk % 2], PSF[4 + blk % 2]
                it_, itk = itile[blk % 2], f"it{blk % 2}"
                dt_, dtk = dtile[blk % 2], f"dt{blk % 2}"
                P.op("pe", lambda e, pst=pst, w3x=w3x, col0=col0, blk=blk: e.matmul(
                    pst, lhsT=w3x[:, col0:col0 + 128], rhs=h2T[:, blk * 512:(blk + 1) * 512], start=True, stop=True),
                    reads=[w3k, ("h2T", blk)], writes=[pk])
                if side == 0:
                    P.op("pool", lambda e, it_=it_, blk=blk: e.iota(it_, pattern=[[1, 512]], base=blk * 512, channel_multiplier=0,
                                                                    allow_small_or_imprecise_dtypes=True), writes=[itk])
                else:
                    P.op("pool", lambda e, it_=it_, blk=blk: e.iota(it_, pattern=[[-1, 512]], base=NFFT - blk * 512, channel_multiplier=0,
                                                                    allow_small_or_imprecise_dtypes=True), writes=[itk])
                P.op("act", lambda e, it_=it_, dt_=dt_, ct=ct: e.activation(out=dt_, in_=it_, func=AF.Exp, scale=dsc[:, ct:ct + 1]),
                     reads=[itk, "dsc"], writes=[dtk])
                P.op("dve", lambda e, pst=pst, dt_=dt_, blk=blk: e.tensor_tensor(
                    out=ktil[:, blk * 512:(blk + 1) * 512], in0=pst, in1=dt_, op=ALU.mult),
                    reads=[pk, dtk], writes=[("ktil", blk), lock(pk)])
            P.op("dve", lambda e: e.memset(ktil[:, L:L + 1], 0.0), reads=[("ktil", 8)], writes=[("ktil", 8)])
            kv = ktil.rearrange("p (a b) -> p a b", b=64)
            for g in range(16):
                pb, pbk = ps_b[g % 2], PSB[g % 2]

                def tr0(e, g=g, pb=pb):
                    ins = None
                    for j in range(4):
                        ins = e.transpose(pb[:, j * 128:(j + 1) * 128], kv[:, :, 4 * g + j], ident_bf)
                    return ins
                P.op("pe", tr0, reads=[("ktil", b_) for b_ in range(16)] + ["ident_bf"], writes=[pbk])
                dst = Uf[:, 4 * g:4 * g + 4, :].rearrange("p a b -> p (a b)")
                if g % 2 == 0:
                    P.op("act", lambda e, dst=dst, pb=pb: e.copy(out=dst, in_=pb[:, 0:512]), reads=[pbk], writes=[("Uf", g), lock(pbk)])
                else:
                    P.op("dve", lambda e, dst=dst, pb=pb: e.tensor_copy(out=dst, in_=pb[:, 0:512]), reads=[pbk], writes=[("Uf", g), lock(pbk)])
            fft_A(Uf, "Uf", 128)
            fft_T1()
            for g in range(16):
                pa_, pak_ = ps_f[(g % 2) * 2], PSF[(g % 2) * 2]
                pb_, pbk_ = ps_f[(g % 2) * 2 + 1], PSF[(g % 2) * 2 + 1]
                rr = At[:, 0, 4 * g:4 * g + 4, :].rearrange("p a b -> p (a b)")
                ri_ = At[:, 1, 4 * g:4 * g + 4, :].rearrange("p a b -> p (a b)")

                def mmB(e, pa_=pa_, pb_=pb_, rr=rr, ri_=ri_):
                    e.matmul(pa_, lhsT=lb[:, 4, :], rhs=rr, start=True, stop=False)
                    e.matmul(pa_, lhsT=lb[:, 5, :], rhs=ri_, start=False, stop=True)
                    e.matmul(pb_, lhsT=lb[:, 6, :], rhs=rr, start=True, stop=False)
                    return e.matmul(pb_, lhsT=lb[:, 7, :], rhs=ri_, start=False, stop=True)
                rkeys = [("At", r_, g // 2) for r_ in range(2)]
                P.op("pe", mmB, reads=rkeys + ["lb"], writes=[pak_, pbk_])
                ks_, ksk = kst[g % 2], f"kst{g % 2}"
                P.op("act", lambda e, ks_=ks_, pa_=pa_: e.copy(out=ks_[:, 0, :], in_=pa_), reads=[pak_], writes=[(ksk, 0), lock(pak_)])
                P.op("dve", lambda e, ks_=ks_, pb_=pb_: e.tensor_copy(out=ks_[:, 1, :], in_=pb_), reads=[pbk_], writes=[(ksk, 1), lock(pbk_)])
                for r_ in range(2):
                    P.dma("sp", lambda e, ks_=ks_, r_=r_, g=g, fidx=fidx: e.dma_start(
                        out=KF[fidx, r_, :, g * 512:(g + 1) * 512], in_=ks_[:, r_, :]),
                        reads=[(ksk, r_)], writes=[("KF", fidx, r_, g)])
    if debug == "filt":
        return finish()
    P.barrier()
    ar.release(mH1)

    raw = ar.alloc([128, L], BF16)
    ubuf = [ar.alloc([128, L], BF16) for _ in range(2)]
    gbuf = ar.alloc([128, L], BF16)
    kfb = [ar.alloc([128, 2, 512], BF16) for _ in range(2)]
    tmpf = [ar.alloc([128, 512], F32) for _ in range(8)]
    U_ = R1.rearrange("p (a b) -> p a b", b=128)
    Zs = R2.rearrange("p (q f) -> p q f", f=128)
    Vs = BA.rearrange("p (r q f) -> p r q f", r=2, f=128)
    Vt = BB.rearrange("p (r t c) -> p r t c", r=2, c=128)

    def sconv(dst, dkey, src, skey, wname, widx, ntile, rows, rowlen, post=None):
        w0, w1_, w2_ = (C(wname, t * ntile + widx) for t in range(3))
        n_ = rows * rowlen
        d3 = dst[:, 0:n_].rearrange("p (r j) -> p r j", j=rowlen)
        s3 = src[:, 0:n_].rearrange("p (r j) -> p r j", j=rowlen)
        P.op("pool", lambda e: e.tensor_scalar(out=dst[:, 0:n_], in0=src[:, 0:n_], scalar1=w1_, scalar2=None, op0=ALU.mult),
             reads=[skey, "colt"], writes=[dkey])
        P.op("dve", lambda e: e.scalar_tensor_tensor(out=d3[:, :, 1:rowlen], in0=s3[:, :, 0:rowlen - 1], scalar=w0,
                                                     in1=d3[:, :, 1:rowlen], op0=ALU.mult, op1=ALU.add),
             reads=[skey, dkey, "colt"], writes=[dkey])
        P.op("dve", lambda e: e.scalar_tensor_tensor(out=d3[:, :, 0:rowlen - 1], in0=s3[:, :, 1:rowlen], scalar=w2_,
                                                     in1=d3[:, :, 0:rowlen - 1], op0=ALU.mult, op1=ALU.add),
             reads=[skey, dkey, "colt"], writes=[dkey])
        if post is not None:
            P.op("act", lambda e: e.activation(out=dst[:, 0:n_], in_=dst[:, 0:n_], func=post), reads=[dkey], writes=[dkey])
    k.sconv = sconv

    def long_conv(u, ukey, fidx, skipcol, gate, gkey, zout, zkey):
        uv = u.rearrange("p (a b) -> p a b", b=64)
        for g in range(16):
            pb, pbk = ps_b[g % 2], PSB[g % 2]

            def tr0(e, g=g, pb=pb):
                ins = None
                for j in range(4):
                    ins = e.transpose(pb[0:64, j * 128:(j + 1) * 128], uv[:, :, 4 * g + j], ident_bf)
                return ins
            P.op("pe", tr0, reads=[ukey, "ident_bf"], writes=[pbk])
            dst = U_[0:64, 4 * g:4 * g + 4, :].rearrange("p a b -> p (a b)")
            if g % 2 == 0:
                P.op("act", lambda e, dst=dst, pb=pb: e.copy(out=dst, in_=pb[0:64, 0:512]), reads=[pbk], writes=[("U", g), lock(pbk)])
            else:
                P.op("dve", lambda e, dst=dst, pb=pb: e.tensor_copy(out=dst, in_=pb[0:64, 0:512]), reads=[pbk], writes=[("U", g), lock(pbk)])
        fft_A(U_, "U", 64)
        fft_T1()
        for g in range(16):
            pa_, pak_ = ps_f[(g % 2) * 2], PSF[(g % 2) * 2]
            pb_, pbk_ = ps_f[(g % 2) * 2 + 1], PSF[(g % 2) * 2 + 1]
            rr = At[:, 0, 4 * g:4 * g + 4, :].rearrange("p a b -> p (a b)")
            ri_ = At[:, 1, 4 * g:4 * g + 4, :].rearrange("p a b -> p (a b)")
            kf_, kfk = kfb[g % 2], f"kfb{g % 2}"
            P.dma("sp", lambda e, kf_=kf_, g=g: e.dma_start(out=kf_, in_=KF[fidx, :, :, g * 512:(g + 1) * 512].rearrange("r p f -> p r f")),
                  reads=[("KF", fidx, r_, g) for r_ in range(2)], writes=[kfk])

            def mmB(e, pa_=pa_, pb_=pb_, rr=rr, ri_=ri_):
                e.matmul(pa_, lhsT=lb[:, 0, :], rhs=rr, start=True, stop=False)
                e.matmul(pa_, lhsT=lb[:, 1, :], rhs=ri_, start=False, stop=True)
                e.matmul(pb_, lhsT=lb[:, 2, :], rhs=rr, start=True, stop=False)
                return e.matmul(pb_, lhsT=lb[:, 3, :], rhs=ri_, start=False, stop=True)
            P.op("pe", mmB, reads=[("At", r_, g // 2) for r_ in range(2)] + ["lb"], writes=[pak_, pbk_])
            ta, tak = tmpf[(g % 2) * 2], f"tmpf{(g % 2) * 2}"
            tb_, tbk = tmpf[(g % 2) * 2 + 1], f"tmpf{(g % 2) * 2 + 1}"
            P.op("dve", lambda e, ta=ta, pa_=pa_, kf_=kf_: e.tensor_tensor(out=ta, in0=pa_, in1=kf_[:, 0, :], op=ALU.mult),
                 reads=[pak_, kfk], writes=[tak, lock(pak_)])
            P.op("dve", lambda e, tb_=tb_, pb_=pb_, kf_=kf_: e.tensor_tensor(out=tb_, in0=pb_, in1=kf_[:, 1, :], op=ALU.mult),
                 reads=[pbk_, kfk], writes=[tbk, lock(pbk_)])
            zd = Zs[:, 4 * g:4 * g + 4, :].rearrange("p a b -> p (a b)")
            P.op("pool", lambda e, zd=zd, ta=ta, tb_=tb_: e.tensor_tensor(out=zd, in0=ta, in1=tb_, op=ALU.add),
                 reads=[tak, tbk], writes=[("Zs", g)])
        for g in range(16):
            pa_, pak_ = ps_f[(g % 2) * 2], PSF[(g % 2) * 2]
            pb_, pbk_ = ps_f[(g % 2) * 2 + 1], PSF[(g % 2) * 2 + 1]
            zr = Zs[:, 4 * g:4 * g + 4, :].rearrange("p a b -> p (a b)")

            def mmBi(e, pa_=pa_, pb_=pb_, zr=zr):
                e.matmul(pa_, lhsT=lb[:, 8, :], rhs=zr, start=True, stop=True)
                return e.matmul(pb_, lhsT=lb[:, 9, :], rhs=zr, start=True, stop=True)
            P.op("pe", mmBi, reads=[("Zs", g), "lb"], writes=[pak_, pbk_])
            P.op("act", lambda e, pa_=pa_, g=g: e.copy(out=Vs[:, 0, 4 * g:4 * g + 4, :].rearrange("p a b -> p (a b)"), in_=pa_),
                 reads=[pak_], writes=[("Vs", 0, g), lock(pak_)])
            P.op("dve", lambda e, pb_=pb_, g=g: e.tensor_copy(out=Vs[:, 1, 4 * g:4 * g + 4, :].rearrange("p a b -> p (a b)"), in_=pb_),
                 reads=[pbk_], writes=[("Vs", 1, g), lock(pbk_)])
        bi = 0
        for ri in range(2):
            for q in range(8):
                pb, pbk = ps_b[bi % 2], PSB[bi % 2]

                def trV(e, ri=ri, q=q, pb=pb):
                    ins = None
                    for j in range(8):
                        ins = e.transpose(pb[:, j * 128:(j + 1) * 128], Vs[:, ri, 8 * q + j, :], ident_bf)
                    return ins
                P.op("pe", trV, reads=[("Vs", ri, 2 * q), ("Vs", ri, 2 * q + 1), "ident_bf"], writes=[pbk])
                dst = Vt[:, ri, :, 16 * q:16 * q + 16]
                src = pb.rearrange("p (c t) -> p t c", t=64)
                if bi % 2 == 0:
                    P.op("act", lambda e, dst=dst, src=src: e.copy(out=dst, in_=src), reads=[pbk], writes=[("Vt", ri, q), lock(pbk)])
                else:
                    P.op("dve", lambda e, dst=dst, src=src: e.tensor_copy(out=dst, in_=src), reads=[pbk], writes=[("Vt", ri, q), lock(pbk)])
                bi += 1
        u3 = u.rearrange("p (a b) -> p a b", b=64)
        g3 = gate.rearrange("p (a b) -> p a b", b=64)
        z3 = zout.rearrange("p (a b) -> p a b", b=64)
        for g in range(8):
            pst, pk = ps_f[g % 4], PSF[g % 4]

            def mmAi(e, g=g, pst=pst):
                ins = None
                for j in range(8):
                    t2 = 8 * g + j
                    e.matmul(pst[:, j * 64:(j + 1) * 64], lhsT=Vt[:, 0, t2, :], rhs=ma_re[:, t2, :], start=True, stop=False)
                    ins = e.matmul(pst[:, j * 64:(j + 1) * 64], lhsT=Vt[:, 1, t2, :], rhs=ma_imn[:, t2, :], start=False, stop=True)
                return ins
            P.op("pe", mmAi, reads=[("Vt", r_, q) for r_ in range(2) for q in range(8)] + ["ma_re", "ma_imn"], writes=[pk])
            tf, tfk = tmpf[g % 4], f"tmpf{g % 4}"
            tf3 = tf.rearrange("p (a b) -> p a b", b=8)
            psv = pst.rearrange("p (j t) -> p t j", t=64)
            P.op("dve", lambda e, tf3=tf3, psv=psv, g=g: e.scalar_tensor_tensor(
                out=tf3, in0=u3[:, :, 8 * g:8 * g + 8], scalar=skipcol, in1=psv, op0=ALU.mult, op1=ALU.add),
                reads=[pk, ukey, "colt"], writes=[tfk, lock(pk)])
            P.op("pool", lambda e, tf3=tf3, g=g: e.tensor_tensor(out=z3[:, :, 8 * g:8 * g + 8], in0=tf3, in1=g3[:, :, 8 * g:8 * g + 8], op=ALU.mult),
                 reads=[tfk, gkey], writes=[(zkey, g)])

    dbg_cv = None
    for ct in range(4):
        P.dma("sp", lambda e, ct=ct: e.dma_start(out=raw, in_=pT[ct * 128:(ct + 1) * 128, :]), reads=[("pT", ct * 128)], writes=["raw"])
        sconv(ubuf[0], "ub0", raw, "raw", "hyconv", ct, 12, 64, 64)
        P.dma("sp", lambda e, ct=ct: e.dma_start(out=raw, in_=pT[(4 + ct) * 128:(5 + ct) * 128, :]), reads=[("pT", (4 + ct) * 128)], writes=["raw"])
        sconv(gbuf, "gbuf", raw, "raw", "hyconv", 4 + ct, 12, 64, 64)
        long_conv(ubuf[0], "ub0", 0 * 4 + ct, C("hyskip", ct), gbuf, "gbuf", ubuf[1], "ub1w")
        if debug == "hy1" and ct == 0:
            dd = nc.dram_tensor("dbg_z1", [128, L], BF16, kind="ExternalOutput").ap()
            P.dma("sp", lambda e: e.dma_start(out=dd, in_=ubuf[1]), reads=[("ub1w", g) for g in range(8)], writes=["dbgz1"])
            return finish()
        P.op("pool", lambda e: e.tensor_copy(out=ubuf[0], in_=ubuf[1]), reads=[("ub1w", g) for g in range(8)], writes=["ub0"])
        P.dma("sp", lambda e, ct=ct: e.dma_start(out=raw, in_=pT[(8 + ct) * 128:(9 + ct) * 128, :]), reads=[("pT", (8 + ct) * 128)], writes=["raw"])
        sconv(gbuf, "gbuf", raw, "raw", "hyconv", 8 + ct, 12, 64, 64)
        long_conv(ubuf[0], "ub0", 1 * 4 + ct, C("hyskip", 4 + ct), gbuf, "gbuf", ubuf[1], "ub1w")
        P.dma("sp", lambda e, ct=ct: e.dma_start(out=yaT[ct * 128:(ct + 1) * 128, :], in_=ubuf[1]),
              reads=[("ub1w", g) for g in range(8)], writes=[("yaT", ct)])
    if debug == "hyena":
        return finish()
    P.barrier()
    ar.release(mH)

    hdir = dscr("hdir", [2, L, 512], F32)
    ybT = dscr("ybT", [512, L], BF16)
    k.hdir, k.ybT = hdir, ybT
    NCH = 34
    LT = LC + L
    mM = ar.mark()
    tri = [ar.alloc([128, 128], F32) for _ in range(2)]
    mask4 = [ar.alloc([128, 512], F32) for _ in range(2)]
    gsel = ar.alloc([16, 2], F32)
    for t_, nm in ((tri[0], "tri_f"), (tri[1], "tri_b"), (mask4[0], "mask4_f"), (mask4[1], "mask4_b"), (gsel, "gsel")):
        P.dma("sp", lambda e, t_=t_, nm=nm: e.dma_start(out=t_, in_=cst[nm]), writes=[nm])
    qT = [ar.alloc([128, LT], BF16) for _ in range(4)]
    kT = [ar.alloc([128, LT], BF16) for _ in range(4)]
    vT = [ar.alloc([128, LT], BF16) for _ in range(4)]
    rawm = ar.alloc([128, LT], BF16)
    mG = ar.mark()
    GL = ar.alloc([16, LT], F32)
    GLt = ar.alloc([16, LT], F32)
    GLcol = sb("GLcol", [128, NCH, 16], F32)
    bcol = sb("bcol", [128, NCH, 8], F32)
    gcol = sb("gcol", [128, NCH, 8], F32)
    totrep = sb("totrep", [128, NCH, 8], F32)
    lwe = sb("lwe", [128, NCH, 8], F32)
    mxrep = sb("mxrep", [128, NCH, 8], F32)
    mxT = sb("mxT", [128, 3], F32)
    mxD = sb("mxD", [128, 128], F32)
    P.dma("sp", lambda e: e.dma_start(out=GL[:, 0:LC], in_=gcTf), reads=[("gTf", LC)], writes=["GLa"])
    P.dma("sp", lambda e: e.dma_start(out=GL[:, LC:LT], in_=gTf), reads=[("gTf", L)], writes=["GLb"])
    P.op("act", lambda e: e.activation(out=GLt, in_=GL, func=AF.Exp, scale=-1.0), reads=["GLa", "GLb"], writes=["GLt"])
    P.op("act", lambda e: e.activation(out=GLt, in_=GLt, func=AF.Ln, bias=1.0), reads=["GLt"], writes=["GLt"])
    P.op("dve", lambda e: e.tensor_scalar(out=GL, in0=GL, scalar1=gsel[:, 0:1], scalar2=None, op0=ALU.mult),
         reads=["GLa", "GLb", "gsel"], writes=["GLa", "GLb"])
    P.op("dve", lambda e: e.scalar_tensor_tensor(out=GL, in0=GLt, scalar=gsel[:, 1:2], in1=GL, op0=ALU.mult, op1=ALU.add),
         reads=["GLt", "GLa", "GLb", "gsel"], writes=["GLa", "GLb", "GL"])
    for blk, (c0_, c1_) in enumerate(((0, 32), (32, 34))):
        def trg(e, c0_=c0_, c1_=c1_):
            ins = None
            for c_ in range(c0_, c1_):
                ins = e.transpose(ps_f[0][:, (c_ - c0_) * 16:(c_ - c0_ + 1) * 16], GL[:, c_ * 128:(c_ + 1) * 128], ident_f[0:16, 0:16])
            return ins
        P.op("pe", trg, reads=["GL", "ident_f"], writes=[PSF[0]])
        P.op("dve", lambda e, c0_=c0_, c1_=c1_: e.tensor_copy(
            out=GLcol[:, c0_:c1_, :].rearrange("p a b -> p (a b)"), in_=ps_f[0][:, 0:(c1_ - c0_) * 16]),
            reads=[PSF[0]], writes=[("GLcol", blk), lock(PSF[0])])

    def mm_bt(e):
        ins = None
        for c_ in range(NCH):
            for d_ in range(2):
                lf_ = GLcol[:, c_, 8 * d_ + 4:8 * d_ + 8]
                e.matmul(ps_f[1][:, c_ * 8 + 4 * d_:c_ * 8 + 4 * d_ + 4], lhsT=tri[d_], rhs=lf_, start=True, stop=True)
                ins = e.matmul(ps_f[2][:, c_ * 8 + 4 * d_:c_ * 8 + 4 * d_ + 4], lhsT=ones_f, rhs=lf_, start=True, stop=True)
        return ins
    P.op("pe", mm_bt, reads=[("GLcol", 0), ("GLcol", 1), "tri_f", "tri_b", "ones_f"], writes=[PSF[1], PSF[2]])
    P.op("dve", lambda e: e.tensor_copy(out=bcol.rearrange("p a b -> p (a b)"), in_=ps_f[1][:, 0:NCH * 8]),
         reads=[PSF[1]], writes=["bcol", lock(PSF[1])])
    P.op("dve", lambda e: e.tensor_copy(out=totrep.rearrange("p a b -> p (a b)"), in_=ps_f[2][:, 0:NCH * 8]),
         reads=[PSF[2]], writes=["totrep", lock(PSF[2])])
    for d_ in range(2):
        P.op("dve", lambda e, d_=d_: e.tensor_tensor(out=gcol[:, :, 4 * d_:4 * d_ + 4], in0=GLcol[:, :, 8 * d_:8 * d_ + 4],
                                                     in1=bcol[:, :, 4 * d_:4 * d_ + 4], op=ALU.subtract),
             reads=[("GLcol", 0), ("GLcol", 1), "bcol"], writes=[("gcol", d_)])
    P.op("dve", lambda e: e.tensor_tensor(out=lwe, in0=totrep, in1=gcol, op=ALU.add),
         reads=["totrep", ("gcol", 0), ("gcol", 1)], writes=["lwe"])
    lwe2 = lwe.rearrange("p a b -> p (a b)")
    mx2 = mxrep.rearrange("p a b -> p (a b)")
    for bi_, (a0, a1) in enumerate(((0, 128), (128, 256), (256, NCH * 8))):
        n_ = a1 - a0
        P.op("pe", lambda e, a0=a0, a1=a1, n_=n_: e.transpose(ps_f[3][0:n_, 0:128], lwe2[:, a0:a1], ident_f),
             reads=["lwe", "ident_f"], writes=[PSF[3]])
        P.op("dve", lambda e, n_=n_, bi_=bi_: e.tensor_reduce(out=mxT[0:n_, bi_:bi_ + 1], in_=ps_f[3][0:n_, 0:128], axis=AX.X, op=ALU.max),
             reads=[PSF[3]], writes=[("mxT", bi_), lock(PSF[3])])
        P.op("dve", lambda e, n_=n_, bi_=bi_: e.tensor_scalar(out=mxD[0:n_, 0:n_], in0=ident_f[0:n_, 0:n_], scalar1=mxT[0:n_, bi_:bi_ + 1],
                                                             scalar2=None, op0=ALU.mult), reads=[("mxT", bi_), "ident_f"], writes=["mxD"])
        P.op("pe", lambda e, n_=n_: e.matmul(ps_f[4][:, 0:n_], lhsT=ones_f[0:n_, :], rhs=mxD[0:n_, 0:n_], start=True, stop=True),
             reads=["mxD", "ones_f"], writes=[PSF[4]])
        P.op("dve", lambda e, a0=a0, a1=a1, n_=n_: e.tensor_copy(out=mx2[:, a0:a1], in_=ps_f[4][:, 0:n_]),
             reads=[PSF[4]], writes=[("mxrep", bi_), lock(PSF[4])])
    P.barrier()
    ar.release(mG)

    QS = MLSTM_SCALE = 128 ** -0.5
    for h in range(4):
        for which, dstl, jl, jc in (("q", qT, 12 + h, 0 + h), ("k", kT, 16 + h, 4 + h), ("v", vT, 20 + h, 8 + h)):
            tgt = dstl[h] if which == "v" else rawm
            tk = f"{which}T{h}" if which == "v" else "rawm"
            P.dma("sp", lambda e, tgt=tgt, jc=jc: e.dma_start(out=tgt[:, 0:LC], in_=pcT[jc * 128:(jc + 1) * 128, :]),
                  reads=[("pcT", jc * 128)], writes=[(tk, 0)])
            P.dma("sp", lambda e, tgt=tgt, jl=jl: e.dma_start(out=tgt[:, LC:LT], in_=pT[jl * 128:(jl + 1) * 128, :]),
                  reads=[("pT", jl * 128)], writes=[(tk, 1)])
            if which == "v":
                continue
            widx = h if which == "q" else 4 + h
            dk_ = f"{which}T{h}"
            sconv(dstl[h][:, 0:LC], (dk_, 0), rawm[:, 0:LC], ("rawm", 0), "mlconv", widx, 8, 1, LC, post=AF.Silu)
            sconv(dstl[h][:, LC:LT], (dk_, 1), rawm[:, LC:LT], ("rawm", 1), "mlconv", widx, 8, 64, 64, post=AF.Silu)
            if which == "q":
                P.op("pool", lambda e, h=h: e.tensor_scalar(out=qT[h][:, LC:LT], in0=qT[h][:, LC:LT], scalar1=QS, scalar2=None, op0=ALU.mult),
                     reads=[(dk_, 1)], writes=[(dk_, 1)])

    ST = []
    for d_ in range(2):
        st = K()
        st.C = ar.alloc([128, 4, 128], F32)
        st.n = ar.alloc([128, 4], F32)
        st.m = ar.alloc([128, 4], F32)
        st.Cbf = ar.alloc([128, 4, 128], BF16)
        st.nbf = ar.alloc([128, 4], BF16)
        st.kv = ar.alloc([128, 8, 128], BF16)
        st.dg = ar.alloc([128, 4, 128], F32)
        st.lw = ar.alloc([128, 4, 128], F32)
        st.A = ar.alloc([128, 4, 128], BF16)
        st.AT = ar.alloc([128, 4, 128], BF16)
        st.vw = ar.alloc([128, 4, 128], BF16)
        st.t1 = ar.alloc([128, 4, 128], F32)
        st.t2 = ar.alloc([128, 4, 128], F32)
        st.sm = ar.alloc([128, 16, 4], F32)
        st.ex = ar.alloc([128, 3, 4], F32)
        st.wbf = ar.alloc([128, 4], BF16)
        P.op("pool", lambda e, st=st: e.memset(st.C, 0.0), writes=[f"C{d_}"])
        P.op("pool", lambda e, st=st: e.memset(st.n, 0.0), writes=[f"n{d_}"])
        P.op("pool", lambda e, st=st: e.memset(st.m, 0.0), writes=[f"m{d_}"])
        ST.append(st)
    psL, psS, psN, psQ, psU, psM = ps_f
    kL, kS, kN, kQ, kU, kM = PSF

    def bc4(ap4):
        return ap4.unsqueeze(2).to_broadcast([128, 4, 128])

    def v3(ps):
        return ps.rearrange("p (a b) -> p a b", b=128)

    def chunk_step(c_, d_, with_out):
        st = ST[d_]
        sk = lambda nm: f"{nm}{d_}"
        cs = slice(c_ * 128, (c_ + 1) * 128)
        h4 = slice(4 * d_, 4 * d_ + 4)
        pb, pbk = ps_b[d_], PSB[d_]
        sm = st.sm
        half = 0 if c_ < 2 else 1
        def trkv(e):
            ins = None
            for h in range(4):
                e.transpose(pb[:, h * 128:(h + 1) * 128], kT[h][:, cs], ident_bf)
                ins = e.transpose(pb[:, 512 + h * 128:512 + (h + 1) * 128], vT[h][:, cs], ident_bf)
            return ins
        P.op("pe", trkv, reads=[(f"kT{h}", half) for h in range(4)] + [(f"vT{h}", half) for h in range(4)] + ["ident_bf"], writes=[pbk])
        P.op("act", lambda e: e.copy(out=st.kv.rearrange("p a b -> p (a b)"), in_=pb), reads=[pbk], writes=[sk("kv"), lock(pbk)])
        if with_out:
            P.op("pool", lambda e: e.tensor_tensor(out=st.dg, in0=ident_f.unsqueeze(1).to_broadcast([128, 4, 128]),
                                                  in1=bc4(gcol[:, c_, h4]), op=ALU.mult),
                 reads=[("gcol", d_), "ident_f"], writes=[sk("dg")])

            def mmL(e):
                e.matmul(psL, lhsT=ones_f, rhs=st.dg.rearrange("p a b -> p (a b)"), start=True, stop=False)
                return e.matmul(psL, lhsT=ident_f, rhs=mask4[d_], start=False, stop=True)
            P.op("pe", mmL, reads=[sk("dg"), "ones_f", "ident_f", "mask4_f", "mask4_b"], writes=[kL])
            P.op("dve", lambda e: e.tensor_tensor(out=st.lw, in0=v3(psL), in1=bc4(bcol[:, c_, h4]), op=ALU.add),
                 reads=[kL, "bcol"], writes=[sk("lw"), lock(kL)])
            P.op("dve", lambda e: e.tensor_reduce(out=sm[:, 0, :], in_=st.lw, axis=AX.X, op=ALU.max), reads=[sk("lw")], writes=[sk("sm0")])
            P.op("dve", lambda e: e.tensor_tensor(out=sm[:, 1, :], in0=bcol[:, c_, h4], in1=st.m, op=ALU.add),
                 reads=["bcol", sk("m")], writes=[sk("sm1")])
            P.op("dve", lambda e: e.tensor_tensor(out=sm[:, 2, :], in0=sm[:, 1, :], in1=sm[:, 0, :], op=ALU.max),
                 reads=[sk("sm0"), sk("sm1")], writes=[sk("sm2")])
            P.op("pool", lambda e: e.tensor_tensor(out=st.lw, in0=st.lw, in1=bc4(sm[:, 0, :]), op=ALU.subtract),
                 reads=[sk("lw"), sk("sm0")], writes=[sk("lw")])
            P.op("act", lambda e: e.activation(out=st.lw, in_=st.lw, func=AF.Exp), reads=[sk("lw")], writes=[sk("lw")])

            def mmS(e):
                ins = None
                for h in range(4):
                    ins = e.matmul(psS[:, h * 128:(h + 1) * 128], lhsT=qT[h][:, cs], rhs=kT[h][:, cs], start=True, stop=True)
                return ins
            P.op("pe", mmS, reads=[(f"qT{h}", 1) for h in range(4)] + [(f"kT{h}", 1) for h in range(4)], writes=[kS])
            P.op("dve", lambda e: e.tensor_tensor(out=st.A, in0=v3(psS), in1=st.lw, op=ALU.mult),
                 reads=[kS, sk("lw")], writes=[sk("A"), lock(kS)])
            P.op("dve", lambda e: e.tensor_reduce(out=sm[:, 3, :], in_=st.A, axis=AX.X, op=ALU.add), reads=[sk("A")], writes=[sk("sm3")])

            def trA(e):
                ins = None
                for h in range(4):
                    ins = e.transpose(pb[:, h * 128:(h + 1) * 128], st.A[:, h, :], ident_bf)
                return ins
            P.op("pe", trA, reads=[sk("A"), "ident_bf"], writes=[pbk])
            P.op("act", lambda e: e.copy(out=st.AT.rearrange("p a b -> p (a b)"), in_=pb[:, 0:512]), reads=[pbk], writes=[sk("AT"), lock(pbk)])

            def mmN(e):
                ins = None
                for h in range(4):
                    ins = e.matmul(psN[:, h * 128:(h + 1) * 128], lhsT=st.AT[:, h, :], rhs=st.kv[:, 4 + h, :], start=True, stop=True)
                return ins
            P.op("pe", mmN, reads=[sk("AT"), sk("kv")], writes=[kN])
            P.op("pool", lambda e: e.tensor_copy(out=st.Cbf, in_=st.C), reads=[sk("C")], writes=[sk("Cbf")])
            P.op("pool", lambda e: e.tensor_copy(out=st.nbf, in_=st.n), reads=[sk("n")], writes=[sk("nbf")])

            def mmQ(e):
                ins = None
                for h in range(4):
                    e.matmul(psQ[:, h * 128:(h + 1) * 128], lhsT=qT[h][:, cs], rhs=st.Cbf[:, h, :], start=True, stop=True)
                    ins = e.matmul(psM[:, h:h + 1], lhsT=qT[h][:, cs], rhs=st.nbf[:, h:h + 1], start=True, stop=True)
                return ins
            P.op("pe", mmQ, reads=[(f"qT{h}", 1) for h in range(4)] + [sk("Cbf"), sk("nbf")], writes=[kQ, (kM, "q")])
            P.op("dve", lambda e: e.tensor_tensor(out=st.ex[:, 0, :], in0=sm[:, 0, :], in1=sm[:, 2, :], op=ALU.subtract),
                 reads=[sk("sm0"), sk("sm2")], writes=[sk("ex0")])
            P.op("dve", lambda e: e.tensor_tensor(out=st.ex[:, 1, :], in0=sm[:, 1, :], in1=sm[:, 2, :], op=ALU.subtract),
                 reads=[sk("sm1"), sk("sm2")], writes=[sk("ex1")])
            P.op("dve", lambda e: e.tensor_scalar(out=st.ex[:, 2, :], in0=sm[:, 2, :], scalar1=-1.0, scalar2=None, op0=ALU.mult),
                 reads=[sk("sm2")], writes=[sk("ex2")])
            P.op("act", lambda e: e.activation(out=st.ex, in_=st.ex, func=AF.Exp), reads=[sk("ex0"), sk("ex1"), sk("ex2")], writes=[sk("ex")])
            P.op("dve", lambda e: e.tensor_tensor(out=st.t1, in0=v3(psN), in1=bc4(st.ex[:, 0, :]), op=ALU.mult),
                 reads=[kN, sk("ex")], writes=[sk("t1"), lock(kN)])
            P.op("dve", lambda e: e.tensor_tensor(out=st.t2, in0=v3(psQ), in1=bc4(st.ex[:, 1, :]), op=ALU.mult),
                 reads=[kQ, sk("ex")], writes=[sk("t2"), lock(kQ)])
            P.op("pool", lambda e: e.tensor_tensor(out=st.t1, in0=st.t1, in1=st.t2, op=ALU.add), reads=[sk("t1"), sk("t2")], writes=[sk("t1")])
            P.op("dve", lambda e: e.tensor_tensor(out=sm[:, 4, :], in0=psM[:, 0:4], in1=st.ex[:, 1, :], op=ALU.mult),
                 reads=[(kM, "q"), sk("ex")], writes=[sk("sm4"), lock(kM)])
            P.op("dve", lambda e: e.tensor_tensor(out=sm[:, 5, :], in0=sm[:, 3, :], in1=st.ex[:, 0, :], op=ALU.mult),
                 reads=[sk("sm3"), sk("ex")], writes=[sk("sm5")])
            P.op("dve", lambda e: e.tensor_tensor(out=sm[:, 4, :], in0=sm[:, 4, :], in1=sm[:, 5, :], op=ALU.add),
                 reads=[sk("sm4"), sk("sm5")], writes=[sk("sm4")])
            P.op("dve", lambda e: e.tensor_scalar(out=sm[:, 10, :], in0=sm[:, 4, :], scalar1=-1.0, scalar2=None, op0=ALU.mult),
                 reads=[sk("sm4")], writes=[sk("sm10")])
            P.op("dve", lambda e: e.tensor_tensor(out=sm[:, 4, :], in0=sm[:, 4, :], in1=sm[:, 10, :], op=ALU.max),
                 reads=[sk("sm4"), sk("sm10")], writes=[sk("sm4")])
            P.op("dve", lambda e: e.tensor_tensor(out=sm[:, 4, :], in0=sm[:, 4, :], in1=st.ex[:, 2, :], op=ALU.max),
                 reads=[sk("sm4"), sk("ex")], writes=[sk("sm4")])
            P.op("dve", lambda e: e.reciprocal(out=sm[:, 5, :], in_=sm[:, 4, :]), reads=[sk("sm4")], writes=[sk("sm5")])
            P.op("pool", lambda e: e.tensor_tensor(out=st.t2, in0=st.t1, in1=bc4(sm[:, 5, :]), op=ALU.mult),
                 reads=[sk("t1"), sk("sm5")], writes=[sk("t2")])
            tok0 = (c_ - 2) * 128
            P.dma("sp", lambda e: e.dma_start(out=hdir[d_, tok0:tok0 + 128, :], in_=st.t2.rearrange("p a b -> p (a b)")),
                  reads=[sk("t2")], writes=[("hdir", d_, c_)])
        P.op("dve", lambda e: e.tensor_tensor(out=sm[:, 7, :], in0=totrep[:, c_, h4], in1=st.m, op=ALU.add),
             reads=["totrep", sk("m")], writes=[sk("sm7")])
        P.op("dve", lambda e: e.tensor_tensor(out=sm[:, 6, :], in0=sm[:, 7, :], in1=mxrep[:, c_, h4], op=ALU.max),
             reads=[sk("sm7")] + [("mxrep", b_) for b_ in range(3)], writes=[sk("sm6")])
        P.op("dve", lambda e: e.tensor_tensor(out=sm[:, 8, :], in0=lwe[:, c_, h4], in1=sm[:, 6, :], op=ALU.subtract),
             reads=["lwe", sk("sm6")], writes=[sk("sm8")])
        P.op("dve", lambda e: e.tensor_tensor(out=sm[:, 9, :], in0=sm[:, 7, :], in1=sm[:, 6, :], op=ALU.subtract),
             reads=[sk("sm7"), sk("sm6")], writes=[sk("sm9")])
        P.op("act", lambda e: e.activation(out=sm[:, 8:10, :], in_=sm[:, 8:10, :], func=AF.Exp), reads=[sk("sm8"), sk("sm9")], writes=[sk("sm89")])
        P.op("pool", lambda e: e.tensor_tensor(out=st.vw, in0=st.kv[:, 4:8, :], in1=bc4(sm[:, 8, :]), op=ALU.mult),
             reads=[sk("kv"), sk("sm89")], writes=[sk("vw")])
        P.op("dve", lambda e: e.tensor_copy(out=st.wbf, in_=sm[:, 8, :]), reads=[sk("sm89")], writes=[sk("wbf")])

        def mmU(e):
            ins = None
            for h in range(4):
                e.matmul(psU[:, h * 128:(h + 1) * 128], lhsT=st.kv[:, h, :], rhs=st.vw[:, h, :], start=True, stop=True)
                ins = e.matmul(psM[:, 8 + h:9 + h], lhsT=st.kv[:, h, :], rhs=st.wbf[:, h:h + 1], start=True, stop=True)
            return ins
        P.op("pe", mmU, reads=[sk("kv"), sk("vw"), sk("wbf")], writes=[kU, (kM, "u")])
        cread = [sk("Cbf")] if with_out else []
        P.op("pool", lambda e: e.tensor_tensor(out=st.C, in0=st.C, in1=bc4(sm[:, 9, :]), op=ALU.mult),
             reads=[sk("C"), sk("sm89")] + cread, writes=[sk("C")])
        P.op("dve", lambda e: e.tensor_tensor(out=st.C, in0=st.C, in1=v3(psU), op=ALU.add), reads=[sk("C"), kU], writes=[sk("C"), lock(kU)])
        P.op("dve", lambda e: e.tensor_tensor(out=st.n, in0=st.n, in1=sm[:, 9, :], op=ALU.mult),
             reads=[sk("n"), sk("sm89")] + ([sk("nbf")] if with_out else []), writes=[sk("n")])
        P.op("dve", lambda e: e.tensor_tensor(out=st.n, in0=st.n, in1=psM[:, 8:12], op=ALU.add), reads=[sk("n"), (kM, "u")], writes=[sk("n"), lock(kM)])
        P.op("dve", lambda e: e.tensor_copy(out=st.m, in_=sm[:, 6, :]), reads=[sk("sm6"), sk("sm7")] + ([sk("sm1")] if with_out else []), writes=[sk("m")])

    seq = [[0, 1] + list(range(2, NCH)), [1, 0] + list(range(NCH - 1, 1, -1))]
    nsteps = NCH if debug != "mlstm_ctx" else 2
    for i_ in range(nsteps):
        for d_ in range(2):
            c_ = seq[d_][i_]
            chunk_step(c_, d_, with_out=(c_ >= 2))
        if debug == "mlstm_ctx" and i_ == 1:
            dd = nc.dram_tensor("dbg_C", [2, 128, 512], F32, kind="ExternalOutput").ap()
            dn_ = nc.dram_tensor("dbg_nm", [2, 128, 8], F32, kind="ExternalOutput").ap()
            for d_ in range(2):
                P.dma("sp", lambda e, d_=d_: e.dma_start(out=dd[d_], in_=ST[d_].C.rearrange("p a b -> p (a b)")), reads=[f"C{d_}"], writes=[("dbgC", d_)])
                P.dma("sp", lambda e, d_=d_: e.dma_start(out=dn_[d_, :, 0:4], in_=ST[d_].n), reads=[f"n{d_}"], writes=[("dbgn", d_)])
                P.dma("sp", lambda e, d_=d_: e.dma_start(out=dn_[d_, :, 4:8], in_=ST[d_].m), reads=[f"m{d_}"], writes=[("dbgm", d_)])
            return finish()
    if debug == "mlstm_scan":
        return finish()

    P.barrier()
    ar.release(mM)
    mY = ar.mark()
    ybacc = [ar.alloc([128, L], BF16) for _ in range(4)]
    hsum = ar.alloc([128, NTT, 512], F32)
    hB = [ar.alloc([128, 512], F32) for _ in range(3)]
    hsq = [ar.alloc([128, 512], F32) for _ in range(2)]
    hnb = [ar.alloc([128, 512], BF16) for _ in range(2)]
    ss4 = ar.alloc([128, NTT, 4], F32)
    for i in range(NTT):
        b_, bk = hB[i % 3], f"hB{i % 3}"
        q_, qk = hsq[i % 2], f"hsq{i % 2}"
        P.dma("sp", lambda e, i=i: e.dma_start(out=hsum[:, i, :], in_=hdir[0, i * 128:(i + 1) * 128, :]), reads=[("hdir", 0, i + 2)], writes=[("hsum", i)])
        P.dma("sp", lambda e, b_=b_, i=i: e.dma_start(out=b_, in_=hdir[1, i * 128:(i + 1) * 128, :]), reads=[("hdir", 1, i + 2)], writes=[bk])
        P.op("pool", lambda e, b_=b_, i=i: e.tensor_tensor(out=hsum[:, i, :], in0=hsum[:, i, :], in1=b_, op=ALU.add), reads=[("hsum", i), bk], writes=[("hsum", i)])
        P.op("pool", lambda e, q_=q_, i=i: e.tensor_tensor(out=q_, in0=hsum[:, i, :], in1=hsum[:, i, :], op=ALU.mult), reads=[("hsum", i)], writes=[qk])
        P.op("dve", lambda e, q_=q_, i=i: e.tensor_reduce(out=ss4[:, i, :], in_=q_.rearrange("p (a b) -> p a b", b=128), axis=AX.X, op=ALU.add),
             reads=[qk], writes=[("ss4", i)])
    allss4 = [("ss4", i) for i in range(NTT)]
    P.op("dve", lambda e: e.tensor_scalar(out=ss4, in0=ss4, scalar1=1.0 / 128, scalar2=RMS_EPS, op0=ALU.mult, op1=ALU.add), reads=allss4, writes=["rs4"])
    P.op("act", lambda e: e.activation(out=ss4, in_=ss4, func=AF.Sqrt), reads=["rs4"], writes=["rs4"])
    P.op("dve", lambda e: e.reciprocal(out=ss4, in_=ss4), reads=["rs4"], writes=["rs4"])
    for i in range(NTT):
        hn_, hnk = hnb[i % 2], f"hnb{i % 2}"
        P.op("dve", lambda e, hn_=hn_, i=i: e.tensor_tensor(out=hn_.rearrange("p (a b) -> p a b", b=128), in0=hsum[:, i, :].rearrange("p (a b) -> p a b", b=128),
                                                         in1=bc4(ss4[:, i, :]), op=ALU.mult), reads=[("hsum", i), "rs4"], writes=[hnk])
        pb, pbk = ps_b[i % 2], PSB[i % 2]

        def trh(e, hn_=hn_, pb=pb):
            ins = None
            for h in range(4):
                ins = e.transpose(pb[:, h * 128:(h + 1) * 128], hn_[:, h * 128:(h + 1) * 128], ident_bf)
            return ins
        P.op("pe", trh, reads=[hnk, "ident_bf"], writes=[pbk])
        for h in range(4):
            dst = ybacc[h][:, i * 128:(i + 1) * 128]
            if i % 2 == 0:
                P.op("act", lambda e, dst=dst, pb=pb, h=h: e.activation(out=dst, in_=pb[:, h * 128:(h + 1) * 128], func=AF.Copy, scale=C("mlnorm", h)),
                     reads=[pbk, "colt"], writes=[("ybacc", h, i), lock(pbk)])
            else:
                P.op("dve", lambda e, dst=dst, pb=pb, h=h: e.tensor_scalar(out=dst, in0=pb[:, h * 128:(h + 1) * 128], scalar1=C("mlnorm", h), scalar2=None,
                                                                        op0=ALU.mult), reads=[pbk, "colt"], writes=[("ybacc", h, i), lock(pbk)])
    og = ar.alloc([128, L], BF16)
    for h in range(4):
        P.dma("sp", lambda e, h=h: e.dma_start(out=og, in_=pT[(24 + h) * 128:(25 + h) * 128, :]), reads=[("pT", (24 + h) * 128)], writes=["og"])
        P.op("pool", lambda e, h=h: e.tensor_tensor(out=ybacc[h], in0=ybacc[h], in1=og, op=ALU.mult),
             reads=["og"] + [("ybacc", h, i) for i in range(NTT)], writes=[("ybf", h)])
        P.dma("sp", lambda e, h=h: e.dma_start(out=ybT[h * 128:(h + 1) * 128, :], in_=ybacc[h]), reads=[("ybf", h)], writes=[("ybT", h)])
    if debug == "mlstm":
        return finish()
    P.barrier()
    ar.release(mY)

    x1d = dscr("x1d", [L, D], F32)
    hx2d = dscr("hx2d", [L, D], BF16)
    affd = dscr("affd", [128, NTT * NEXP], F32)
    idx_i = ar.alloc([128, 64], I32)
    idx_f = ar.alloc([128, 64], F32)
    gate_s = ar.alloc([128, 64], F32)
    sc9 = ar.alloc([128, NTT], F32)
    mS6 = ar.mark()
    aff_all = ar.alloc([128, NTT, NEXP], F32)
    m6 = ar.mark()
    wbh = ar.alloc([128, 4, D], BF16)
    wbm = ar.alloc([128, 4, D], BF16)
    wout = ar.alloc([128, 8, D], BF16)
    wrt = ar.alloc([128, 8, NEXP], BF16)
    P.dma("pool", lambda e: e.dma_start(out=wbh, in_=w_bh.rearrange("(c p) n -> p c n", p=128)), writes=["wbh"])
    P.dma("pool", lambda e: e.dma_start(out=wbm, in_=w_bm.rearrange("(c p) n -> p c n", p=128)), writes=["wbm"])
    P.dma("pool", lambda e: e.dma_start(out=wout, in_=w_out.rearrange("(c p) n -> p c n", p=128)), writes=["wout"])
    P.dma("pool", lambda e: e.dma_start(out=wrt, in_=w_rt.rearrange("(c p) n -> p c n", p=128)), writes=["wrt"])
    yab = ar.alloc([128, 4, 512], BF16)
    ybb = ar.alloc([128, 4, 512], BF16)
    gab = ar.alloc([128, 8, 512], BF16)
    gbb = ar.alloc([128, 8, 512], BF16)
    ymT = ar.alloc([128, 8, 512], BF16)
    mt1 = [ar.alloc([128, 512], F32) for _ in range(2)]
    mt2 = [ar.alloc([128, 512], F32) for _ in range(2)]
    xt6 = [ar.alloc([128, D], F32) for _ in range(2)]
    x1t = [ar.alloc([128, D], F32) for _ in range(2)]
    h2f = [ar.alloc([128, D], F32) for _ in range(2)]
    h2b = [ar.alloc([128, D], BF16) for _ in range(2)]
    hx2T = ar.alloc([128, 8, 128], BF16)
    junk6 = ar.alloc([128, D], BF16)
    sc6 = ar.alloc([128, NTT, 4], F32)
    etile = ar.alloc([128, 2, NEXP], F32)
    for tb in range(8):
        ts_ = slice(tb * 512, (tb + 1) * 512)
        P.dma("sp", lambda e, ts_=ts_: e.dma_start(out=yab, in_=yaT[:, ts_].rearrange("(c p) t -> p c t", p=128)),
              reads=[("yaT", ct) for ct in range(4)], writes=["yab"])
        P.dma("sp", lambda e, ts_=ts_: e.dma_start(out=ybb, in_=ybT[:, ts_].rearrange("(c p) t -> p c t", p=128)),
              reads=[("ybT", h) for h in range(4)], writes=["ybb"])
        P.dma("sp", lambda e, ts_=ts_: e.dma_start(out=gab, in_=pT[29 * 128:37 * 128, ts_].rearrange("(c p) t -> p c t", p=128)),
              reads=[("pT", (29 + j) * 128) for j in range(8)], writes=["gab"])
        P.dma("sp", lambda e, ts_=ts_: e.dma_start(out=gbb, in_=pT[37 * 128:45 * 128, ts_].rearrange("(c p) t -> p c t", p=128)),
              reads=[("pT", (37 + j) * 128) for j in range(8)], writes=["gbb"])
        for dm in range(8):
            pa_, pak_ = ps_f[(dm % 2) * 2], PSF[(dm % 2) * 2]
            pb_, pbk_ = ps_f[(dm % 2) * 2 + 1], PSF[(dm % 2) * 2 + 1]

            def mmM(e, dm=dm, pa_=pa_, pb_=pb_):
                ins = None
                for ct in range(4):
                    e.matmul(pa_, lhsT=wbh[:, ct, dm * 128:(dm + 1) * 128], rhs=yab[:, ct, :], start=(ct == 0), stop=(ct == 3))
                for ct in range(4):
                    ins = e.matmul(pb_, lhsT=wbm[:, ct, dm * 128:(dm + 1) * 128], rhs=ybb[:, ct, :], start=(ct == 0), stop=(ct == 3))
                return ins
            P.op("pe", mmM, reads=["wbh", "wbm", "yab", "ybb"], writes=[pak_, pbk_])
            t1_, t1k = mt1[dm % 2], f"mt1{dm % 2}"
            t2_, t2k = mt2[dm % 2], f"mt2{dm % 2}"
            P.op("dve", lambda e, t1_=t1_, pa_=pa_, dm=dm: e.tensor_tensor(out=t1_, in0=pa_, in1=gab[:, dm, :], op=ALU.mult),
                 reads=[pak_, "gab"], writes=[t1k, lock(pak_)])
            P.op("dve", lambda e, t2_=t2_, pb_=pb_, dm=dm: e.tensor_tensor(out=t2_, in0=pb_, in1=gbb[:, dm, :], op=ALU.mult),
                 reads=[pbk_, "gbb"], writes=[t2k, lock(pbk_)])
            P.op("pool", lambda e, t1_=t1_, t2_=t2_, dm=dm: e.tensor_tensor(out=ymT[:, dm, :], in0=t1_, in1=t2_, op=ALU.add),
                 reads=[t1k, t2k], writes=[("ymT", dm)])
        for sub in range(4):
            i = tb * 4 + sub
            xx, xk = xt6[i % 2], f"xt6{i % 2}"
            x1_, x1k = x1t[i % 2], f"x1t{i % 2}"
            hf_, hfk = h2f[i % 2], f"h2f{i % 2}"
            hb_, hbk = h2b[i % 2], f"h2b{i % 2}"
            P.dma("sp", lambda e, xx=xx, i=i: e.dma_start(out=xx, in_=x_t[i]), writes=[xk])
            for half in range(2):
                py, pyk = ps_f[4 + half], PSF[4 + half]
                hs = slice(half * 512, (half + 1) * 512)

                def mmO(e, sub=sub, hs=hs, py=py):
                    ins = None
                    for dm in range(8):
                        ins = e.matmul(py, lhsT=ymT[:, dm, sub * 128:(sub + 1) * 128], rhs=wout[:, dm, hs], start=(dm == 0), stop=(dm == 7))
                    return ins
                P.op("pe", mmO, reads=[("ymT", dm) for dm in range(8)] + ["wout"], writes=[pyk])
                P.op("dve", lambda e, py=py, hs=hs, x1_=x1_: e.tensor_tensor(out=x1_[:, hs], in0=py, in1=bc["ada2"][:, hs], op=ALU.mult),
                     reads=[pyk, ("bc", "ada2", 0), ("bc", "ada2", 1)], writes=[(x1k, half), lock(pyk)])
                P.op("pool", lambda e, hs=hs, x1_=x1_, xx=xx: e.tensor_tensor(out=x1_[:, hs], in0=x1_[:, hs], in1=xx[:, hs], op=ALU.add),
                     reads=[(x1k, half), xk], writes=[(x1k, half)])
            P.dma("sp", lambda e, x1_=x1_, i=i: e.dma_start(out=x1d[i * 128:(i + 1) * 128, :], in_=x1_),
                  reads=[(x1k, 0), (x1k, 1)], writes=[("x1d", i)])
            P.op("act", lambda e, x1_=x1_, i=i: e.activation(out=junk6, in_=x1_, func=AF.Square, accum_out=sc6[:, i, 0:1]),
                 reads=[(x1k, 0), (x1k, 1)], writes=["junk6", ("sc6a", i)])
            P.op("dve", lambda e, i=i: e.tensor_scalar(out=sc6[:, i, 0:1], in0=sc6[:, i, 0:1], scalar1=1.0 / D, scalar2=RMS_EPS, op0=ALU.mult, op1=ALU.add),
                 reads=[("sc6a", i)], writes=[("sc6a", i)])
            P.op("act", lambda e, i=i: e.activation(out=sc6[:, i, 0:1], in_=sc6[:, i, 0:1], func=AF.Sqrt), reads=[("sc6a", i)], writes=[("sc6a", i)])
            P.op("dve", lambda e, i=i: e.reciprocal(out=sc6[:, i, 0:1], in_=sc6[:, i, 0:1]), reads=[("sc6a", i)], writes=[("sc6a", i)])
            P.op("dve", lambda e, i=i, hf_=hf_, x1_=x1_: e.scalar_tensor_tensor(out=hf_, in0=x1_, scalar=sc6[:, i, 0:1], in1=bc["G2"], op0=ALU.mult, op1=ALU.mult),
                 reads=[(x1k, 0), (x1k, 1), ("sc6a", i), ("bc", "G2", 0), ("bc", "G2", 1)], writes=[hfk])
            P.op("pool", lambda e, hf_=hf_, hb_=hb_: e.tensor_tensor(out=hb_, in0=hf_, in1=bc["S2"], op=ALU.add),
                 reads=[hfk, ("bc", "S2", 0), ("bc", "S2", 1)], writes=[hbk])
            P.dma("sp", lambda e, hb_=hb_, i=i: e.dma_start(out=hx2d[i * 128:(i + 1) * 128, :], in_=hb_), reads=[hbk], writes=[("hx2d", i)])
            pb, pbk = ps_b[i % 2], PSB[i % 2]

            def trH(e, hb_=hb_, pb=pb):
                ins = None
                for dc in range(8):
                    ins = e.transpose(pb[:, dc * 128:(dc + 1) * 128], hb_[:, dc * 128:(dc + 1) * 128], ident_bf)
                return ins
            P.op("pe", trH, reads=[hbk, "ident_bf"], writes=[pbk])
            P.op("act", lambda e, pb=pb: e.copy(out=hx2T.rearrange("p a b -> p (a b)"), in_=pb), reads=[pbk], writes=["hx2T", lock(pbk)])
            pr, prk = ps_f[sub % 4], PSF[sub % 4]

            def mmR(e, pr=pr):
                ins = None
                for dc in range(8):
                    ins = e.matmul(pr[:, 0:NEXP], lhsT=hx2T[:, dc, :], rhs=wrt[:, dc, :], start=(dc == 0), stop=(dc == 7))
                return ins
            P.op("pe", mmR, reads=["hx2T", "wrt"], writes=[prk])
            P.op("dve", lambda e, pr=pr, i=i: e.tensor_reduce(out=sc6[:, i, 1:2], in_=pr[:, 0:NEXP], axis=AX.X, op=ALU.max, negate=True),
                 reads=[prk], writes=[("sc6b", i), lock(prk)])
            et = etile[:, i % 2, :]
            P.op("act", lambda e, pr=pr, i=i, et=et: e.activation(out=et, in_=pr[:, 0:NEXP], func=AF.Exp, bias=sc6[:, i, 1:2], accum_out=sc6[:, i, 2:3]),
                 reads=[prk, ("sc6b", i)], writes=[("etile", i % 2), ("sc6c", i), lock(prk)])
            P.op("dve", lambda e, i=i: e.reciprocal(out=sc6[:, i, 2:3], in_=sc6[:, i, 2:3]), reads=[("sc6c", i)], writes=[("sc6c", i)])
            P.op("dve", lambda e, i=i, et=et: e.tensor_scalar(out=aff_all[:, i, :], in0=et, scalar1=sc6[:, i, 2:3], scalar2=None, op0=ALU.mult),
                 reads=[("etile", i % 2), ("sc6c", i)], writes=[("aff", i)])
    if debug == "merge":
        P.dma("sp", lambda e: e.dma_start(out=affd, in_=aff_all.rearrange("p a b -> p (a b)")), reads=[("aff", i) for i in range(NTT)], writes=["affd"])
        return finish()
    P.barrier()
    ar.release(m6)
    w1b = [ar.alloc([128, 8, 512], BF16) for _ in range(2)]
    w3b_ = [ar.alloc([128, 8, 512], BF16) for _ in range(2)]
    w2b = [ar.alloc([128, 16, D], BF16) for _ in range(2)]
    mW = ar.mark()
    n_exp = NEXP if debug != "moe1" else 1
    def load_w2(ex):
        w2_, w2k = w2b[ex % 2], f"w2b{ex % 2}"
        P.dma("pool", lambda e: e.dma_start(out=w2_[:, 0:8, :], in_=w_e2[ex, 0:1024, :].rearrange("(c p) n -> p c n", p=128)), writes=[(w2k, 0)])
        P.dma("pool", lambda e: e.dma_start(out=w2_[:, 8:16, :], in_=w_e2[ex, 1024:2048, :].rearrange("(c p) n -> p c n", p=128)), writes=[(w2k, 1)])

    def load_w13(ex, fb):
        q_ = (ex * 4 + fb) % 2
        fs = slice(fb * 512, (fb + 1) * 512)
        P.dma("pool", lambda e: e.dma_start(out=w1b[q_], in_=w_e1[ex].rearrange("(c p) f -> p c f", p=128)[:, :, fs]), writes=[f"w1b{q_}"])
        P.dma("pool", lambda e: e.dma_start(out=w3b_[q_], in_=w_e3[ex].rearrange("(c p) f -> p c f", p=128)[:, :, fs]), writes=[f"w3b{q_}"])

    if debug in (None, "moe", "moe1", "all"):
        load_w2(0)
        load_w13(0, 0)
        load_w13(0, 1)

    idxd = dscr("idxd", [128, 64], I32)
    gated = dscr("gated", [128, 64], F32)
    m7 = ar.mark()
    lo = ar.alloc([128, NEXP], F32)
    hi = ar.alloc([128, NEXP], F32)
    mid = ar.alloc([128, NEXP], F32)
    cntp = ar.alloc([128, NEXP], F32)
    gef = ar.alloc([128, NEXP], F32)
    dlt = ar.alloc([128, NEXP], F32)
    cmpb = ar.alloc([128, NTT, NEXP], BF16)
    maskf = ar.alloc([128, NTT, NEXP], F32)
    rank = ar.alloc([128, NTT, NEXP], F32)
    offs = ar.alloc([128, NTT, NEXP], F32)
    tris_bf = ar.alloc([128, 128], BF16)
    ones_bf = ar.alloc([128, 128], BF16)
    iot = ar.alloc([128, 512], F32)
    Rv = ar.alloc([128, NTT, NEXP, 4], BF16)
    cpt = ar.alloc([128, NTT, 2], F32)
    alo = ar.alloc([128, NTT, NEXP], F32)
    ahb = ar.alloc([128, NTT, NEXP], BF16)
    oh = [ar.alloc([128, NEXP, 512], BF16) for _ in range(3)]
    P.op("pool", lambda e: e.memset(lo, 0.0), writes=["lo"])
    P.op("pool", lambda e: e.memset(hi, 1.0), writes=["hi"])
    allaff = [("aff", i) for i in range(NTT)]
    for it in range(30):
        P.op("dve", lambda e: e.tensor_scalar(out=mid, in0=lo, scalar1=0.5, scalar2=None, op0=ALU.mult), reads=["lo"], writes=["mid"])
        P.op("dve", lambda e: e.scalar_tensor_tensor(out=mid, in0=hi, scalar=0.5, in1=mid, op0=ALU.mult, op1=ALU.add), reads=["hi", "mid"], writes=["mid"])
        P.op("dve", lambda e: e.tensor_tensor(out=cmpb, in0=aff_all, in1=mid.unsqueeze(1).to_broadcast([128, NTT, NEXP]), op=ALU.is_ge),
             reads=allaff + ["mid"], writes=["cmpb"])
        P.op("dve", lambda e: e.tensor_reduce(out=cntp, in_=cmpb.rearrange("p c e -> p e c"), axis=AX.X, op=ALU.add), reads=["cmpb"], writes=["cntp"])
        P.op("pe", lambda e: e.matmul(ps_f[0][:, 0:NEXP], lhsT=ones_f, rhs=cntp, start=True, stop=True), reads=["cntp", "ones_f"], writes=[PSF[0]])
        P.op("dve", lambda e: e.tensor_single_scalar(out=gef, in_=ps_f[0][:, 0:NEXP], scalar=float(CAP), op=ALU.is_ge), reads=[PSF[0]], writes=["gef", lock(PSF[0])])
        P.op("dve", lambda e: e.tensor_tensor(out=dlt, in0=mid, in1=lo, op=ALU.subtract), reads=["mid", "lo"], writes=["dlt"])
        P.op("dve", lambda e: e.tensor_tensor(out=dlt, in0=dlt, in1=gef, op=ALU.mult), reads=["dlt", "gef"], writes=["dlt"])
        P.op("dve", lambda e: e.tensor_tensor(out=lo, in0=lo, in1=dlt, op=ALU.add), reads=["lo", "dlt"], writes=["lo"])
        P.op("dve", lambda e: e.tensor_tensor(out=dlt, in0=hi, in1=mid, op=ALU.subtract), reads=["hi", "mid"], writes=["dlt"])
        P.op("dve", lambda e: e.tensor_tensor(out=dlt, in0=dlt, in1=gef, op=ALU.mult), reads=["dlt", "gef"], writes=["dlt"])
        P.op("dve", lambda e: e.tensor_tensor(out=hi, in0=mid, in1=dlt, op=ALU.add), reads=["mid", "dlt"], writes=["hi"])
    P.op("dve", lambda e: e.tensor_tensor(out=maskf, in0=aff_all, in1=lo.unsqueeze(1).to_broadcast([128, NTT, NEXP]), op=ALU.is_ge),
         reads=allaff + ["lo"], writes=["maskf"])
    P.op("dve", lambda e: e.tensor_copy(out=cmpb, in_=maskf), reads=["maskf"], writes=["cmpb"])
    P.op("pool", lambda e: e.tensor_copy(out=ones_bf, in_=ones_f), reads=["ones_f"], writes=["ones_bf"])
    P.dma("sp", lambda e: e.dma_start(out=rank[:, 0:8, :].rearrange("p a b -> p (a b)"), in_=cst["tri_f"]), writes=["rank"])
    P.op("dve", lambda e: e.tensor_tensor(out=tris_bf, in0=rank[:, 0:8, :].rearrange("p a b -> p (a b)"), in1=ident_f, op=ALU.subtract),
         reads=["rank", "ident_f"], writes=["tris_bf"])
    cm2 = cmpb.rearrange("p a b -> p (a b)")
    P.op("pe", lambda e: e.matmul(ps_f[1], lhsT=tris_bf, rhs=cm2, start=True, stop=True), reads=["tris_bf", "cmpb"], writes=[PSF[1]])
    P.op("pe", lambda e: e.matmul(ps_f[2], lhsT=ones_bf, rhs=cm2, start=True, stop=True), reads=["ones_bf", "cmpb"], writes=[PSF[2]])
    P.op("dve", lambda e: e.tensor_copy(out=rank.rearrange("p a b -> p (a b)"), in_=ps_f[1]), reads=[PSF[1], "tris_bf"], writes=["rank", lock(PSF[1])])
    P.op("dve", lambda e: e.tensor_copy(out=offs.rearrange("p a b -> p (a b)"), in_=ps_f[2]),
         reads=[PSF[2]], writes=["tot7", lock(PSF[2])])
    P.op("pool", lambda e: e.memset(cntp, 0.0), reads=["cntp"], writes=["cntp"])
    for c_ in range(NTT):
        P.op("dve", lambda e, c_=c_: e.tensor_tensor(out=rank[:, c_, :], in0=rank[:, c_, :], in1=cntp, op=ALU.add), reads=["rank", "cntp"], writes=["rank"])
        P.op("dve", lambda e, c_=c_: e.tensor_tensor(out=cntp, in0=cntp, in1=offs[:, c_, :], op=ALU.add), reads=["cntp", "tot7"], writes=["cntp"])
    P.op("dve", lambda e: e.scalar_tensor_tensor(out=rank, in0=rank, scalar=1.0, in1=maskf, op0=ALU.add, op1=ALU.mult), reads=["rank", "maskf"], writes=["rank"])
    P.op("dve", lambda e: e.tensor_scalar(out=rank, in0=rank, scalar1=-1.0, scalar2=None, op0=ALU.add), reads=["rank"], writes=["rank"])
    P.op("pool", lambda e: e.iota(iot, pattern=[[1, 512]], base=0, channel_multiplier=0, allow_small_or_imprecise_dtypes=True), writes=["iot"])
    P.op("pool", lambda e: e.iota(cpt[:, :, 0], pattern=[[1, NTT]], base=0, channel_multiplier=0, allow_small_or_imprecise_dtypes=True), writes=["cpt0"])
    P.op("pool", lambda e: e.iota(cpt[:, :, 1], pattern=[[0, NTT]], base=0, channel_multiplier=1, allow_small_or_imprecise_dtypes=True), writes=["cpt1"])
    P.op("dve", lambda e: e.tensor_copy(out=ahb, in_=aff_all), reads=allaff, writes=["ahb"])
    P.op("dve", lambda e: e.tensor_tensor(out=alo, in0=aff_all, in1=ahb, op=ALU.subtract), reads=allaff + ["ahb"], writes=["alo"])
    P.op("dve", lambda e: e.tensor_copy(out=Rv[:, :, :, 0:2], in_=cpt.unsqueeze(2).to_broadcast([128, NTT, NEXP, 2])), reads=["cpt0", "cpt1"], writes=["Rv01"])
    P.op("dve", lambda e: e.tensor_copy(out=Rv[:, :, :, 2], in_=ahb), reads=["ahb"], writes=["Rv2"])
    P.op("dve", lambda e: e.tensor_copy(out=Rv[:, :, :, 3], in_=alo), reads=["alo"], writes=["Rv3"])
    psI = ps_f[3]
    for c_ in range(NTT):
        oh_, ohk = oh[c_ % 3], f"oh{c_ % 3}"
        P.op("dve", lambda e, oh_=oh_, c_=c_: e.tensor_tensor(
            out=oh_, in0=iot.unsqueeze(1).to_broadcast([128, NEXP, 512]),
            in1=rank[:, c_, :].unsqueeze(2).to_broadcast([128, NEXP, 512]), op=ALU.is_equal),
            reads=["iot", "rank"], writes=[ohk])

        def mmI(e, oh_=oh_, c_=c_):
            ins = None
            for ex in range(NEXP):
                for j in range(4):
                    q_ = (ex * 4 + j) * 4
                    ins = e.matmul(psI[:, q_:q_ + 4], lhsT=oh_[:, ex, j * 128:(j + 1) * 128], rhs=Rv[:, c_, ex, :],
                                   start=(c_ == 0 and ex == 0 and j == 0), stop=(c_ == NTT - 1), skip_group_check=True)
            return ins
        P.op("pe", mmI, reads=[ohk, "Rv01", "Rv2", "Rv3"], writes=[PSF[3]])
    pIs = ar.alloc([128, 256], F32)
    P.op("dve", lambda e: e.tensor_copy(out=pIs, in_=psI[:, 0:256]), reads=[PSF[3]], writes=["pIs", lock(PSF[3])])
    pI = pIs.rearrange("p (q f) -> p q f", f=4)
    P.op("dve", lambda e: e.scalar_tensor_tensor(out=idx_f, in0=pI[:, :, 0], scalar=128.0, in1=pI[:, :, 1], op0=ALU.mult, op1=ALU.add),
         reads=["pIs"], writes=["idx_f"])
    P.op("dve", lambda e: e.tensor_tensor(out=gate_s, in0=pI[:, :, 2], in1=pI[:, :, 3], op=ALU.add), reads=["pIs"], writes=["gate_s"])
    P.op("dve", lambda e: e.tensor_copy(out=idx_i, in_=idx_f), reads=["idx_f"], writes=["idx_i"])
    if debug == "route":
        P.dma("sp", lambda e: e.dma_start(out=idxd, in_=idx_i), reads=["idx_i"], writes=["idxd"])
        P.dma("sp", lambda e: e.dma_start(out=gated, in_=gate_s), reads=["gate_s"], writes=["gated"])
        return finish()
    P.barrier()
    ar.release(mW)

    m8 = ar.mark()
    xg = [ar.alloc([128, D], BF16) for _ in range(2)]
    xgT2 = [ar.alloc([128, 8, CAP], BF16) for _ in range(2)]
    actT = ar.alloc([128, 16, CAP], BF16)
    sil = [ar.alloc([128, CAP], F32) for _ in range(2)]
    yg = [ar.alloc([128, D], F32) for _ in range(2)]
    n_exp = NEXP if debug != "moe1" else 1

    def gather(ex):
        xgT = xgT2[ex % 2]
        for j in range(4):
            xg_, xgk = xg[j % 2], f"xg{j % 2}"
            col = ex * 4 + j
            P.dma("pool", lambda e, xg_=xg_, col=col: e.indirect_dma_start(
                out=xg_, out_offset=None, in_=hx2d, in_offset=bass.IndirectOffsetOnAxis(ap=idx_i[:, col:col + 1], axis=0)),
                reads=["idx_i"] + [("hx2d", i) for i in range(NTT)], writes=[xgk])
            pb, pbk = ps_b[j % 2], PSB[j % 2]

            def trX(e, xg_=xg_, pb=pb):
                ins = None
                for dc in range(8):
                    ins = e.transpose(pb[:, dc * 128:(dc + 1) * 128], xg_[:, dc * 128:(dc + 1) * 128], ident_bf)
                return ins
            P.op("pe", trX, reads=[xgk, "ident_bf"], writes=[pbk])
            dst = xgT[:, :, j * 128:(j + 1) * 128]
            src = pb.rearrange("p (a b) -> p a b", b=128)
            if j % 2 == 0:
                P.op("act", lambda e, dst=dst, src=src: e.copy(out=dst, in_=src), reads=[pbk], writes=[("xgT", ex % 2, j), lock(pbk)])
            else:
                P.op("dve", lambda e, dst=dst, src=src: e.tensor_copy(out=dst, in_=src), reads=[pbk], writes=[("xgT", ex % 2, j), lock(pbk)])

    gather(0)
    for ex in range(n_exp):
        xgT = xgT2[ex % 2]
        w2_, w2k = w2b[ex % 2], f"w2b{ex % 2}"
        for fb in range(4):
            q_ = (ex * 4 + fb) % 2
            w1_, w1k = w1b[q_], f"w1b{q_}"
            w3_, w3k = w3b_[q_], f"w3b{q_}"
            for fc in range(4):
                p1, p1k = ps_f[(fc % 2) * 2], PSF[(fc % 2) * 2]
                p3, p3k = ps_f[(fc % 2) * 2 + 1], PSF[(fc % 2) * 2 + 1]

                def mmH(e, w1_=w1_, w3_=w3_, fc=fc, p1=p1, p3=p3, xgT=xgT):
                    ins = None
                    for dc in range(8):
                        e.matmul(p1, lhsT=w1_[:, dc, fc * 128:(fc + 1) * 128], rhs=xgT[:, dc, :], start=(dc == 0), stop=(dc == 7))
                    for dc in range(8):
                        ins = e.matmul(p3, lhsT=w3_[:, dc, fc * 128:(fc + 1) * 128], rhs=xgT[:, dc, :], start=(dc == 0), stop=(dc == 7))
                    return ins
                P.op("pe", mmH, reads=[w1k, w3k] + [("xgT", ex % 2, j) for j in range(4)], writes=[p1k, p3k])
                sl_, slk = sil[fc % 2], f"sil{fc % 2}"
                P.op("act", lambda e, sl_=sl_, p1=p1: e.activation(out=sl_, in_=p1, func=AF.Silu), reads=[p1k], writes=[slk, lock(p1k)])
                P.op("dve", lambda e, sl_=sl_, p3=p3, fb=fb, fc=fc: e.tensor_tensor(out=actT[:, fb * 4 + fc, :], in0=p3, in1=sl_, op=ALU.mult),
                     reads=[p3k, slk], writes=[("actT", fb * 4 + fc), lock(p3k)])
            if fb + 2 < 4:
                load_w13(ex, fb + 2)
        if ex + 1 < n_exp:
            load_w2(ex + 1)
            gather(ex + 1)
            load_w13(ex + 1, 0)
            load_w13(ex + 1, 1)
        for j in range(4):
            yg_, ygk = yg[j % 2], f"yg{j % 2}"
            col = ex * 4 + j
            for half in range(2):
                py, pyk = ps_f[4 + half], PSF[4 + half]
                hs = slice(half * 512, (half + 1) * 512)

                def mmY(e, j=j, hs=hs, py=py, w2_=w2_):
                    ins = None
                    for fc in range(16):
                        ins = e.matmul(py, lhsT=actT[:, fc, j * 128:(j + 1) * 128], rhs=w2_[:, fc, hs], start=(fc == 0), stop=(fc == 15))
                    return ins
                P.op("pe", mmY, reads=[("actT", fc) for fc in range(16)] + [(w2k, 0), (w2k, 1)], writes=[pyk])
                P.op("dve", lambda e, yg_=yg_, py=py, hs=hs, col=col: e.scalar_tensor_tensor(
                    out=yg_[:, hs], in0=py, scalar=gate_s[:, col:col + 1], in1=bc["ada5"][:, hs], op0=ALU.mult, op1=ALU.mult),
                    reads=[pyk, "gate_s", ("bc", "ada5", 0), ("bc", "ada5", 1)], writes=[(ygk, half), lock(pyk)])
            P.dma("pool", lambda e, yg_=yg_, col=col: e.indirect_dma_start(
                out=x1d, out_offset=bass.IndirectOffsetOnAxis(ap=idx_i[:, col:col + 1], axis=0), in_=yg_, in_offset=None,
                compute_op=ALU.add), reads=[(ygk, 0), (ygk, 1), "idx_i"], writes=["x1d_sc"])
    if debug in ("moe", "moe1"):
        return finish()
    P.barrier()
    ar.release(m8)

    xf = [ar.alloc([128, D], F32) for _ in range(3)]
    of = [ar.alloc([128, D], F32) for _ in range(2)]
    junk9 = ar.alloc([128, D], BF16)
    out_t = out.rearrange("(n p) d -> n p d", p=128)
    for i in range(NTT):
        xx, xk = xf[i % 3], f"xf{i % 3}"
        oo, ok_ = of[i % 2], f"of{i % 2}"
        P.dma("sp", lambda e, xx=xx, i=i: e.dma_start(out=xx, in_=x1d[i * 128:(i + 1) * 128, :]), reads=["x1d_sc", ("x1d", i)], writes=[xk])
        P.op("act", lambda e, xx=xx, i=i: e.activation(out=junk9, in_=xx, func=AF.Square, accum_out=sc9[:, i:i + 1]), reads=[xk], writes=["junk9", ("sc9", i)])
        P.op("dve", lambda e, i=i: e.tensor_scalar(out=sc9[:, i:i + 1], in0=sc9[:, i:i + 1], scalar1=1.0 / D, scalar2=RMS_EPS, op0=ALU.mult, op1=ALU.add),
             reads=[("sc9", i)], writes=[("sc9", i)])
        P.op("act", lambda e, i=i: e.activation(out=sc9[:, i:i + 1], in_=sc9[:, i:i + 1], func=AF.Sqrt), reads=[("sc9", i)], writes=[("sc9", i)])
        P.op("dve", lambda e, i=i: e.reciprocal(out=sc9[:, i:i + 1], in_=sc9[:, i:i + 1]), reads=[("sc9", i)], writes=[("sc9", i)])
        P.op("dve", lambda e, i=i, xx=xx, oo=oo: e.scalar_tensor_tensor(out=oo, in0=xx, scalar=sc9[:, i:i + 1], in1=bc["fing"], op0=ALU.mult, op1=ALU.mult),
             reads=[xk, ("sc9", i), ("bc", "fing", 0), ("bc", "fing", 1)], writes=[ok_])
        P.dma("sp", lambda e, oo=oo, i=i: e.dma_start(out=out_t[i], in_=oo), reads=[ok_], writes=[("out", i)])

    P.final_wait("sp")
    P.run()
    return nc


def _in_maps(inp, ncores=8):
    hc = _host_consts()
    f = lambda a: np.ascontiguousarray(np.asarray(a, np.float32))
    b_in = f(inp["b_in"][0])
    cols = np.zeros((128, NCOLS), np.float32)
    cols[:, COLS["gmix"]:COLS["gmix"] + 8] = _col(inp["norm_mix_g"][0])
    cols[:, COLS["bada"]:COLS["bada"] + 48] = _col(inp["b_ada"][0])
    for j, (c0, n) in enumerate(JOBS):
        cols[0:n, COLS["bin"] + j] = b_in[c0:c0 + n]
    hyc = f(inp["hy_conv"][0])
    for t in range(3):
        cols[:, COLS["hyconv"] + t * 12:COLS["hyconv"] + (t + 1) * 12] = _col(hyc[t])
    mlc = f(inp["ml_conv"][0])
    for t in range(3):
        cols[:, COLS["mlconv"] + t * 8:COLS["mlconv"] + (t + 1) * 8] = _col(mlc[t])
    sk = f(inp["hy_skip"][0])
    for o in range(2):
        cols[:, COLS["hyskip"] + o * 4:COLS["hyskip"] + (o + 1) * 4] = _col(sk[o])
    cols[:, COLS["mlnorm"]:COLS["mlnorm"] + 4] = _col(inp["ml_norm_g"][0])
    cols[:, COLS["delta"]:COLS["delta"] + 4] = _delta_col()
    cols[0:64, COLS["fb1"]] = f(inp["hy_f_b1"][0])
    cols[0:64, COLS["fb2"]] = f(inp["hy_f_b2"][0])
    rows = np.concatenate([f(inp["norm_ffn_g"][0]), f(inp["final_norm_g"])])[None, :]
    shared = {
        "c_ctx": f(inp["c_ctx"]).reshape(128, 8),
        "w_ada": f(inp["w_ada"][0]),
        "bada_row": np.ascontiguousarray(np.broadcast_to(f(inp["b_ada"][0])[None, :], (2, 6 * D))),
        "cols": cols, "rows": np.ascontiguousarray(rows),
        "w_in": f(inp["w_in"][0]),
        "f_w1": f(inp["hy_f_w1"][0]), "f_w2": f(inp["hy_f_w2"][0]), "f_w3": f(inp["hy_f_w3"][0]),
        "w_bh": f(inp["w_branch_hy"][0]), "w_bm": f(inp["w_branch_ml"][0]), "w_out": f(inp["w_out"][0]),
        "w_rt": f(inp["w_router"][0]),
        "w_e1": f(inp["w_exp1"][0]), "w_e3": f(inp["w_exp3"][0]), "w_e2": f(inp["w_exp2"][0]),
    }
    for name, arr in hc.items():
        shared["k_" + name] = arr
    maps = []
    for b in range(ncores):
        m = dict(shared)
        m["x"] = f(inp["x"][b])
        m["c"] = f(inp["c"][b]).reshape(128, 8)
        m["ctx"] = f(inp["ctx"][b])
        maps.append(m)
    return maps


def kernel(**inputs):
    nc = build()
    maps = _in_maps(inputs)
    res = run_bass_kernel_spmd(nc, maps, core_ids=list(range(8)))
    return np.stack([np.asarray(r["out"], np.float32) for r in res.results], axis=0)
```

```python
import math
import numpy as np
import ml_dtypes
import concourse.bass as bass
import concourse.mybir as mybir
from concourse.bass_utils import run_bass_kernel_spmd

F32 = mybir.dt.float32
BF16 = mybir.dt.bfloat16
I32 = mybir.dt.int32
U32 = mybir.dt.uint32
AF = mybir.ActivationFunctionType
ALU = mybir.AluOpType
AX = mybir.AxisListType

D = 1024
L = 4096
LC = 256
NTT = L // 128
DIN = 5648
HY0, ML0, GT0, MG0 = 0, 1536, 3584, 3600
NEXP = 16
CAP = 512
DFF = 2048
RMS_EPS = 1e-6
NFFT = 8192

ENGS = ("pe", "act", "dve", "pool", "sp")
N_DMA_SEMS = 12


class _Op:
    __slots__ = ("eng", "emit", "waits", "inc", "is_dma")


class Prog:
    def __init__(self, nc):
        self.nc = nc
        self.ops = {e: [] for e in ENGS}
        self.cnt = {e: 0 for e in ENGS}
        self.known = {e: {} for e in ENGS}
        self.last_w = {}
        self.readers = {}
        self.dma_cnt = {}
        self.dma_rr = {e: 0 for e in ("sp", "act", "pool")}
        self.semobj = {}

    def _need(self, eng, tok, waits):
        if tok is None:
            return
        k, v = tok
        if self.known[eng].get(k, 0) >= v:
            return
        if waits.get(k, 0) < v:
            waits[k] = v

    def _deps(self, eng, reads, writes):
        waits = {}
        for k in reads:
            self._need(eng, self.last_w.get(k), waits)
        for k in writes:
            self._need(eng, self.last_w.get(k), waits)
            for t in self.readers.get(k, ()):
                self._need(eng, t, waits)
        for k, v in waits.items():
            self.known[eng][k] = v
        return waits

    def _commit(self, tok, reads, writes):
        for k in reads:
            self.readers.setdefault(k, []).append(tok)
        for k in writes:
            self.last_w[k] = tok
            self.readers[k] = []

    def op(self, eng, emit, reads=(), writes=()):
        o = _Op()
        o.eng, o.emit, o.is_dma = eng, emit, False
        o.waits = self._deps(eng, reads, writes)
        self.cnt[eng] += 1
        tok = (("c", eng), self.cnt[eng])
        o.inc = tok
        self.ops[eng].append(o)
        self._commit(tok, reads, writes)
        return tok

    def dma(self, eng, emit, reads=(), writes=()):
        o = _Op()
        o.eng, o.emit, o.is_dma = eng, emit, True
        i = self.dma_rr[eng]
        self.dma_rr[eng] = (i + 1) % N_DMA_SEMS
        k = ("d", eng, i)
        waits = self._deps(eng, reads, writes)
        prev = self.dma_cnt.get(k, 0)
        if prev:
            self._need(eng, (k, 16 * prev), waits)
            self.known[eng][k] = max(self.known[eng].get(k, 0), 16 * prev)
        o.waits = waits
        self.dma_cnt[k] = prev + 1
        tok = (k, 16 * (prev + 1))
        o.inc = tok
        self.ops[eng].append(o)
        self._commit(tok, reads, writes)
        return tok

    def barrier(self):
        toks = [(("c", e), self.cnt[e]) for e in ENGS if self.cnt[e]]
        toks += [(k, 16 * n) for k, n in self.dma_cnt.items()]
        for eng in ENGS:
            waits = {}
            for t in toks:
                self._need(eng, t, waits)
            for k, v in waits.items():
                self.known[eng][k] = v
            if waits:
                o = _Op()
                o.eng, o.emit, o.is_dma, o.waits, o.inc = eng, None, False, waits, None
                self.ops[eng].append(o)

    def final_wait(self, eng):
        toks = [(("c", e), self.cnt[e]) for e in ENGS if self.cnt[e]]
        toks += [(k, 16 * n) for k, n in self.dma_cnt.items()]
        waits = {}
        for t in toks:
            self._need(eng, t, waits)
        o = _Op()
        o.eng, o.emit, o.is_dma, o.waits, o.inc = eng, None, False, waits, None
        self.ops[eng].append(o)

    def run(self):
        import contextlib
        nc = self.nc
        keys = [("c", e) for e in ENGS if self.cnt[e]] + list(self.dma_cnt.keys())
        with contextlib.ExitStack() as st:
            for k in keys:
                self.semobj[k] = st.enter_context(nc.semaphore("s_" + "_".join(map(str, k))))
            block = st.enter_context(nc.Block())

            def body(ename):
                def f(eng):
                    for o in self.ops[ename]:
                        for k, v in o.waits.items():
                            eng.wait_ge(self.semobj[k], v)
                        if o.emit is None:
                            continue
                        ins = o.emit(eng)
                        ins.then_inc(self.semobj[o.inc[0]], 16 if o.is_dma else 1)
                return f

            if self.ops["pe"]:
                block.tensor(body("pe"))
            if self.ops["act"]:
                block.scalar(body("act"))
            if self.ops["dve"]:
                block.vector(body("dve"))
            if self.ops["pool"]:
                block.gpsimd(body("pool"))
            if self.ops["sp"]:
                block.sync(body("sp"))


def _inproj_jobs():
    jobs = [(128 * t, 128) for t in range(28)]
    jobs.append((GT0, 16))
    jobs += [(MG0 + 128 * t, 128) for t in range(16)]
    return jobs


JOBS = _inproj_jobs()
NJ = len(JOBS)

COLS = {}
_off = 0
for _name, _n in [("gmix", 8), ("bada", 48), ("bin", NJ), ("hyconv", 36), ("mlconv", 24), ("hyskip", 8),
                  ("mlnorm", 4), ("delta", 4), ("fb1", 1), ("fb2", 1)]:
    COLS[_name] = _off
    _off += _n
NCOLS = _off


def _col(vec, n=None):
    v = np.asarray(vec, np.float32).reshape(-1)
    if v.size % 128:
        v = np.concatenate([v, np.zeros(128 - v.size % 128, np.float32)])
    return np.ascontiguousarray(v.reshape(-1, 128).T)


def _host_consts():
    c = {}
    c["ident_bf"] = np.eye(128, dtype=np.float32).astype(ml_dtypes.bfloat16)
    c["ident_f"] = np.eye(128, dtype=np.float32)
    c["ones_f"] = np.ones((128, 128), np.float32)
    n1 = np.arange(128)[:, None]
    f1 = np.arange(128)[None, :]
    FA = np.exp(-2j * np.pi * (n1 * f1 / 128 + n1 / 256))
    c["fa_re"] = FA.real.astype(np.float32).astype(ml_dtypes.bfloat16)
    c["fa_im"] = FA.imag.astype(np.float32).astype(ml_dtypes.bfloat16)
    f1c = np.arange(128)[:, None]
    n2r = np.arange(64)[None, :]
    Tw = np.exp(-2j * np.pi * (n2r * f1c / 8192 + n2r / 16384))
    c["tw"] = np.concatenate([Tw.real, Tw.imag, -Tw.imag], axis=1).astype(np.float32)
    n2c = np.arange(64)[:, None]
    f2r = np.arange(32)[None, :]
    FB = np.exp(-2j * np.pi * n2c * f2r / 64)
    FBr, FBi = FB.real, FB.imag

    def bd(a, b):
        m = np.zeros((128, 128))
        blk = np.concatenate([a, b], axis=1)
        m[0:64, 0:64] = blk
        m[64:128, 64:128] = blk
        return m

    lb = [bd(FBr, FBi), bd(-FBi, FBr),
          bd(-FBi, FBr), bd(-FBr, -FBi),
          bd(FBr, FBr), bd(-FBi, -FBi),
          bd(FBi, FBi), bd(FBr, FBr)]
    Cm = np.exp(2j * np.pi * np.arange(32)[:, None] * np.arange(64)[None, :] / 64)

    def bdi(top, bot):
        m = np.zeros((128, 128))
        blk = np.concatenate([top, bot], axis=0)
        m[0:64, 0:64] = blk
        m[64:128, 64:128] = blk
        return m

    lb += [bdi(Cm.real, -Cm.imag), bdi(Cm.imag, Cm.real)]
    c["lb"] = np.stack(lb, axis=1).astype(np.float32).astype(ml_dtypes.bfloat16)
    t1 = np.arange(64)[None, None, :]
    f1_ = np.arange(128)[:, None, None]
    t2 = np.arange(64)[None, :, None]
    E = np.exp(2j * np.pi * (t1 * f1_ / 128 + t1 / 256 + t2 * f1_ / 8192 + t2 / 16384)) * (2.0 / NFFT)
    c["ma_re"] = E.real.astype(np.float32).astype(ml_dtypes.bfloat16)
    c["ma_imn"] = (-E.imag).astype(np.float32).astype(ml_dtypes.bfloat16)
    ii = np.arange(128)
    c["tri_f"] = (ii[:, None] <= ii[None, :]).astype(np.float32)
    c["tri_b"] = (ii[:, None] >= ii[None, :]).astype(np.float32)
    NEG = -1.0e30
    mf_ = np.where(ii[None, :] <= ii[:, None], 0.0, NEG).astype(np.float32)
    mb_ = np.where(ii[None, :] >= ii[:, None], 0.0, NEG).astype(np.float32)
    c["mask4_f"] = np.tile(mf_, (1, 4))
    c["mask4_b"] = np.tile(mb_, (1, 4))
    gsel = np.zeros((16, 2), np.float32)
    gsel[[0, 1, 2, 3, 8, 9, 10, 11], 0] = 1.0
    gsel[[4, 5, 6, 7, 12, 13, 14, 15], 1] = -1.0
    c["gsel"] = gsel
    pos = np.arange(NFFT)
    pos = np.where(pos < L, pos, NFFT - pos)
    pos[L] = 0
    tlin = np.linspace(0.0, 1.0, L, dtype=np.float32)
    bands = np.linspace(1e-4, 15, 16, dtype=np.float32)[None, :]
    ang = (np.float32(2.0 * math.pi / L) * np.arange(L, dtype=np.float32)[:, None]) * bands
    z = np.concatenate([tlin[:, None], np.cos(ang), np.sin(ang)], axis=-1).astype(np.float32)
    c["zext"] = np.ascontiguousarray(z[pos].T)
    c["text"] = np.ascontiguousarray(np.broadcast_to(tlin[pos][None, :], (128, NFFT))).astype(np.float32)
    return c


def _delta_col():
    mn = math.log(1e-2) / 1.5
    mx = math.log(1e-2) / 0.3
    d = np.abs(np.linspace(mn, mx, 512, dtype=np.float32))
    return _col(-d)


class K:
    pass


def _esz(dt):
    return 2 if dt == BF16 else 4


class Arena:
    def __init__(self, nc, nbytes):
        self.t = nc.alloc_sbuf_tensor("arena", [128, nbytes // 2], BF16)[:]
        self.off = 0
        self.cap = nbytes
        self.peak = 0

    def mark(self):
        return self.off

    def release(self, m):
        self.off = m

    def alloc(self, shape, dt):
        n = int(np.prod(shape[1:]))
        nb = (n * _esz(dt) + 31) // 32 * 32
        assert self.off + nb <= self.cap, f"arena overflow: need {nb} at {self.off} cap {self.cap}"
        v = self.t[:, self.off // 2:(self.off + nb) // 2]
        self.off += nb
        self.peak = max(self.peak, self.off)
        if dt != BF16:
            v = v.bitcast(dt)
        v = v[:, 0:n]
        if len(shape) > 2:
            names = [f"a{i}" for i in range(len(shape) - 1)]
            kw = {nm: int(sz) for nm, sz in zip(names[:-1], shape[1:-1])}
            v = v.rearrange("p (" + " ".join(names) + ") -> p " + " ".join(names), **kw)
        return v[0:shape[0]]


def build(debug=None, cut=99):
    nc = bass.Bass("TRN2", target_bir_lowering=False)
    P = Prog(nc)
    k = K()
    k.nc, k.P, k.debug = nc, P, debug
    dbg_kind = "ExternalOutput" if debug else "Internal"

    in_names = []
    k.in_names = in_names
    nc._k = k

    def din(name, shape, dt=F32):
        in_names.append(name)
        return nc.dram_tensor(name, list(shape), dt, kind="ExternalInput").ap()

    DBG_OUT = {"inproj": ("pT", "pcT"), "hyena": ("yaT",), "mlstm": ("yaT", "ybT"), "merge": ("x1d", "hx2d", "affd"),
               "route": ("idxd", "gated"), "moe": ("x1d",), "moe1": ("x1d",)}

    def dscr(name, shape, dt):
        kind = "ExternalOutput" if (debug and name in DBG_OUT.get(debug, ())) else "Internal"
        return nc.dram_tensor(name, list(shape), dt, kind=kind).ap()

    x = din("x", [L, D])
    cc = din("c", [128, 8])
    cctx = din("c_ctx", [128, 8])
    ctx = din("ctx", [LC, D])
    w_ada = din("w_ada", [D, 6 * D])
    bada_row = din("bada_row", [2, 6 * D])
    cols_d = din("cols", [128, NCOLS])
    rows_d = din("rows", [1, 2 * D])
    w_in = din("w_in", [D, DIN])
    f_w1 = din("f_w1", [33, 64])
    f_w2 = din("f_w2", [64, 64])
    f_w3 = din("f_w3", [64, 2048])
    w_bh = din("w_bh", [512, D])
    w_bm = din("w_bm", [512, D])
    w_out = din("w_out", [D, D])
    w_rt = din("w_rt", [D, NEXP])
    if debug in (None, "moe", "moe1", "all"):
        w_e1 = din("w_e1", [NEXP, D, DFF])
        w_e3 = din("w_e3", [NEXP, D, DFF])
        w_e2 = din("w_e2", [NEXP, DFF, D])
    cst = {}
    hc = _host_consts()
    k.hc = hc
    for name, arr in hc.items():
        cst[name] = din("k_" + name, arr.shape, BF16 if arr.dtype == ml_dtypes.bfloat16 else F32)
    out = nc.dram_tensor("out", [L, D], F32, kind="ExternalOutput").ap()

    pT = dscr("pT", [NJ * 128, L], BF16)
    pcT = dscr("pcT", [17 * 128, LC], BF16)
    gTf = dscr("gTf", [16, L], F32)
    gcTf = dscr("gcTf", [16, LC], F32)

    def sb(name, shape, dt):
        return nc.alloc_sbuf_tensor(name, list(shape), dt)[:]

    ar = Arena(nc, 196 * 1024)
    k.ar = ar
    ps_f = [nc.alloc_psum_tensor(f"psf{i}", [128, 512], F32)[:] for i in range(6)]
    ps_b = [nc.alloc_psum_tensor(f"psb{i}", [128, 1024], BF16)[:] for i in range(2)]
    PSF = [f"psf{i}" for i in range(6)]
    PSB = [f"psb{i}" for i in range(2)]
    k.ps_f, k.ps_b, k.PSF, k.PSB = ps_f, ps_b, PSF, PSB

    ident_bf = sb("ident_bf", [128, 128], BF16)
    ident_f = sb("ident_f", [128, 128], F32)
    ones_f = sb("ones_f", [128, 128], F32)
    colt = sb("colt", [128, NCOLS], F32)
    adaT = sb("adaT", [128, 48, 2], F32)
    G1 = sb("G1", [128, 8, 2], F32)
    ssq = sb("ssq", [128, NTT + 2], F32)
    rstd = sb("rstd", [128, NTT + 2], F32)
    P.dma("sp", lambda e: e.dma_start(out=ident_bf, in_=cst["ident_bf"]), writes=["ident_bf"])
    P.dma("sp", lambda e: e.dma_start(out=ident_f, in_=cst["ident_f"]), writes=["ident_f"])
    P.dma("sp", lambda e: e.dma_start(out=ones_f, in_=cst["ones_f"]), writes=["ones_f"])
    P.dma("sp", lambda e: e.dma_start(out=colt, in_=cols_d), writes=["colt"])
    k.ident_bf, k.ident_f, k.ones_f, k.colt = ident_bf, ident_f, ones_f, colt

    def C(name, j=0, n=1):
        o = COLS[name] + j
        return colt[:, o:o + n]
    k.C = C

    def finish(dump=None, shape=None, dt=F32):
        print("arena peak", ar.peak, {e_: len(v_) for e_, v_ in P.ops.items()})
        P.barrier()
        if dump is not None:
            dd = nc.dram_tensor("dbg_dump", list(shape), dt, kind="ExternalOutput").ap()
            P.dma("sp", lambda e: e.dma_start(out=dd, in_=dump), writes=["dbg_dump"])
        P.final_wait("sp")
        P.run()
        return nc
    k.finish = finish

    bc = {}
    for name in ("ada2", "ada5", "S2", "G2", "fing"):
        bc[name] = ar.alloc([128, D], F32)
    k.bc = bc

    mA = ar.mark()
    cA = ar.alloc([128, 8], F32)
    cB = ar.alloc([128, 8], F32)
    s2 = ar.alloc([128, 8, 2], BF16)
    R = ar.alloc([2, 6 * D], F32)
    brow = ar.alloc([2, 6 * D], F32)
    wab = [ar.alloc([128, 8, 1024], BF16) for i in range(2)]
    rowt = ar.alloc([1, 2 * D], F32)
    g2row = ar.alloc([1, D], F32)
    P.dma("sp", lambda e: e.dma_start(out=cA, in_=cc), writes=["cA"])
    P.dma("sp", lambda e: e.dma_start(out=cB, in_=cctx), writes=["cB"])
    P.op("act", lambda e: e.activation(out=s2[:, :, 0], in_=cA, func=AF.Silu), reads=["cA"], writes=["s2a"])
    P.op("act", lambda e: e.activation(out=s2[:, :, 1], in_=cB, func=AF.Silu), reads=["cB"], writes=["s2b"])
    P.dma("sp", lambda e: e.dma_start(out=brow, in_=bada_row), writes=["brow"])
    P.dma("sp", lambda e: e.dma_start(out=rowt, in_=rows_d), writes=["rowt"])
    def _cut_here():
        dbgc = nc.dram_tensor("dbg_cut", [128, 16], BF16, kind="ExternalOutput").ap()
        P.barrier()
        P.dma("sp", lambda e: e.dma_start(out=dbgc, in_=s2.rearrange("p a b -> p (a b)")), writes=["dbgc"])
        P.final_wait("sp")
        P.run()
        return nc
    if cut == 1:
        return _cut_here()
    wada_v = w_ada.rearrange("(p dc) n -> p dc n", dc=8)
    for i in range(6 if cut > 2 else 1):
        wb = wab[i % 2]
        wk = f"wab{i % 2}"
        P.dma("pool", lambda e, wb=wb, i=i: e.dma_start(out=wb, in_=wada_v[:, :, i * 1024:(i + 1) * 1024]),
              writes=[wk])
        for h in range(2):
            pst, pk = ps_f[h], PSF[h]

            def mm_row(e, wb=wb, h=h, pst=pst):
                ins = None
                for dc in range(8):
                    ins = e.matmul(pst[0:2, :], lhsT=s2[:, dc, :], rhs=wb[:, dc, h * 512:(h + 1) * 512],
                                   start=(dc == 0), stop=(dc == 7))
                return ins
            P.op("pe", mm_row, reads=[wk, "s2a", "s2b"], writes=[pk])
            sl = slice(i * 1024 + h * 512, i * 1024 + (h + 1) * 512)
            P.op("dve", lambda e, pst=pst, sl=sl: e.tensor_tensor(out=R[:, sl], in0=pst[0:2, :], in1=brow[:, sl],
                                                                 op=ALU.add),
                 reads=[pk, "brow"], writes=[("R", i, h)])
        pst, pk = ps_f[2 + (i % 2)], PSF[2 + (i % 2)]

        def mm_col(e, wb=wb, pst=pst):
            ins = None
            for dco in range(8):
                for dc in range(8):
                    ins = e.matmul(pst[:, 2 * dco:2 * dco + 2], lhsT=wb[:, dc, dco * 128:(dco + 1) * 128],
                                   rhs=s2[:, dc, :], start=(dc == 0), stop=(dc == 7))
            return ins
        P.op("pe", mm_col, reads=[wk, "s2a", "s2b"], writes=[pk])
        P.op("dve", lambda e, pst=pst, i=i: e.tensor_tensor(
            out=adaT[:, i * 8:(i + 1) * 8, :], in0=pst[:, 0:16].rearrange("p (a b) -> p a b", b=2),
            in1=C("bada", i * 8, 8).unsqueeze(2).to_broadcast([128, 8, 2]), op=ALU.add),
            reads=[pk, "colt"], writes=[("adaT", i)])

    if cut in (2, 3):
        return _cut_here()
    P.op("dve", lambda e: e.tensor_scalar(out=G1, in0=adaT[:, 8:16, :], scalar1=1.0, scalar2=None, op0=ALU.add),
         reads=[("adaT", 1)], writes=["G1"])
    P.op("dve", lambda e: e.tensor_tensor(out=G1, in0=G1, in1=C("gmix", 0, 8).unsqueeze(2).to_broadcast([128, 8, 2]),
                                          op=ALU.mult), reads=["G1", "colt"], writes=["G1"])
    k.adaT, k.G1 = adaT, G1

    if cut == 4:
        return _cut_here()
    P.op("dve", lambda e: e.tensor_scalar(out=g2row, in0=R[0:1, 4 * D:5 * D], scalar1=1.0, scalar2=None, op0=ALU.add),
         reads=[("R", 4, 0), ("R", 4, 1)], writes=["g2row"])
    P.op("dve", lambda e: e.tensor_tensor(out=g2row, in0=g2row, in1=rowt[:, 0:D], op=ALU.mult),
         reads=["g2row", "rowt"], writes=["g2row"])
    srcs = {"ada2": (R[0:1, 2 * D:3 * D], [("R", 2, 0), ("R", 2, 1)]),
            "ada5": (R[0:1, 5 * D:6 * D], [("R", 5, 0), ("R", 5, 1)]),
            "S2": (R[0:1, 3 * D:4 * D], [("R", 3, 0), ("R", 3, 1)]),
            "G2": (g2row[:, :], ["g2row"]),
            "fing": (rowt[:, D:2 * D], ["rowt"])}
    for bi, (name, (src, rk)) in enumerate(srcs.items()):
        for h in range(2):
            pst, pk = ps_f[(2 * bi + h) % 4], PSF[(2 * bi + h) % 4]
            P.op("pe", lambda e, pst=pst, src=src, h=h: e.matmul(pst, lhsT=ones_f[0:1, :], rhs=src[:, h * 512:(h + 1) * 512],
                                                                start=True, stop=True),
                 reads=rk + ["ones_f"], writes=[pk])
            P.op("act", lambda e, pst=pst, name=name, h=h: e.copy(out=bc[name][:, h * 512:(h + 1) * 512], in_=pst),
                 reads=[pk], writes=[("bc", name, h)])
    if debug == "ada":
        dbg = nc.dram_tensor("dbg_adaT", [128, 96], F32, kind="ExternalOutput").ap()
        P.dma("sp", lambda e: e.dma_start(out=dbg, in_=adaT.rearrange("p a b -> p (a b)")),
              reads=[("adaT", i) for i in range(6)], writes=["dbg"])
        dbg3 = nc.dram_tensor("dbg_bc", [128, D], F32, kind="ExternalOutput").ap()
        P.dma("sp", lambda e: e.dma_start(out=dbg3, in_=bc["G2"]), reads=[("bc", "G2", 0), ("bc", "G2", 1)], writes=["dbg3"])
        P.final_wait("sp")
        P.run()
        return nc
    P.barrier()
    ar.release(mA)

    mB = ar.mark()
    hT = ar.alloc([128, 8, L], BF16)
    hcT = ar.alloc([128, 8, LC], BF16)
    xt = [ar.alloc([128, D], F32) for i in range(3)]
    junk = ar.alloc([128, D], BF16)
    ybf = [ar.alloc([128, D], BF16) for i in range(2)]
    x_t = x.rearrange("(n p) d -> n p d", p=128)
    ctx_t = ctx.rearrange("(n p) d -> n p d", p=128)
    srcs1 = [x_t[i] for i in range(NTT)] + [ctx_t[i] for i in range(2)]
    for i, src in enumerate(srcs1):
        xb_, xk = xt[i % 3], f"xt{i % 3}"
        P.dma("sp", lambda e, xb_=xb_, src=src: e.dma_start(out=xb_, in_=src), writes=[xk])
        P.op("act", lambda e, xb_=xb_, i=i: e.activation(out=junk, in_=xb_, func=AF.Square, accum_out=ssq[:, i:i + 1]),
             reads=[xk], writes=["junk", ("ssq", i)])
    if cut == 10:
        return finish(ssq, [128, NTT + 2])
    allss = [("ssq", i) for i in range(NTT + 2)]
    P.op("dve", lambda e: e.tensor_scalar(out=rstd, in0=ssq, scalar1=1.0 / D, scalar2=RMS_EPS, op0=ALU.mult, op1=ALU.add),
         reads=allss, writes=["rstd"])
    P.op("act", lambda e: e.activation(out=rstd, in_=rstd, func=AF.Sqrt), reads=["rstd"], writes=["rstd"])
    P.op("dve", lambda e: e.reciprocal(out=rstd, in_=rstd), reads=["rstd"], writes=["rstd"])
    if cut == 11:
        return finish(rstd, [128, NTT + 2])
    for i, src in enumerate(srcs1):
        if cut == 12 and i >= 2:
            break
        xb_, xk = xt[i % 3], f"xt{i % 3}"
        yb_, yk = ybf[i % 2], f"ybf{i % 2}"
        pb, pbk = ps_b[i % 2], PSB[i % 2]
        P.dma("sp", lambda e, xb_=xb_, src=src: e.dma_start(out=xb_, in_=src), writes=[xk])
        P.op("dve", lambda e, xb_=xb_, yb_=yb_, i=i: e.tensor_scalar(out=yb_, in0=xb_, scalar1=rstd[:, i:i + 1], scalar2=None,
                                                                  op0=ALU.mult), reads=[xk, "rstd"], writes=[yk])

        def tr8(e, yb_=yb_, pb=pb):
            ins = None
            for dc in range(8):
                ins = e.transpose(pb[:, dc * 128:(dc + 1) * 128], yb_[:, dc * 128:(dc + 1) * 128], ident_bf)
            return ins
        P.op("pe", tr8, reads=[yk, "ident_bf"], writes=[pbk])
        lat = i < NTT
        for dc in range(8):
            if lat:
                dst = hT[:, dc, i * 128:(i + 1) * 128]
                wkey = ("hT", i // 4, dc, i % 4)
            else:
                dst = hcT[:, dc, (i - NTT) * 128:(i - NTT + 1) * 128]
                wkey = ("hcT", 0, dc, i - NTT)
            col = 0 if lat else 1
            if i % 2 == 0:
                P.op("act", lambda e, dst=dst, pb=pb, dc=dc, col=col: e.activation(
                    out=dst, in_=pb[:, dc * 128:(dc + 1) * 128], func=AF.Identity,
                    scale=G1[:, dc, col:col + 1], bias=adaT[:, dc, col:col + 1]),
                    reads=[pbk, "G1", ("adaT", 0)], writes=[wkey])
            else:
                P.op("dve", lambda e, dst=dst, pb=pb, dc=dc, col=col: e.tensor_scalar(
                    out=dst, in0=pb[:, dc * 128:(dc + 1) * 128], scalar1=G1[:, dc, col:col + 1],
                    scalar2=adaT[:, dc, col:col + 1], op0=ALU.mult, op1=ALU.add),
                    reads=[pbk, "G1", ("adaT", 0)], writes=[wkey])

    if cut in (12, 13):
        return finish(hT[:, 0, 0:256], [128, 256], BF16)
    win_v = w_in.rearrange("(dc p) n -> p dc n", p=128)
    wib = [ar.alloc([128, 8, 512], BF16) for i in range(2)]
    pacc = [ar.alloc([128, L], BF16) for i in range(2)]
    gacc = ar.alloc([16, L], F32)
    groups_lat = [list(range(4 * g, 4 * g + 4)) for g in range(7)] + [[28]] + \
                 [list(range(29 + 4 * g, 29 + 4 * g + 4)) for g in range(4)]
    groups_ctx = [list(range(12 + 4 * g, 12 + 4 * g + 4)) for g in range(4)] + [[28]]
    cnt = {"g": 0, "t": 0}

    def inproj(groups, src_T, src_key, ntok, dst_dram, row_of):
        nblk = max(1, ntok // 512)
        bw = min(512, ntok)
        nsub = bw // 128
        for grp in groups:
            gc0 = JOBS[grp[0]][0]
            gn = sum(JOBS[j][1] for j in grp)
            wb, wk = wib[cnt["g"] % 2], f"wib{cnt['g'] % 2}"
            cnt["g"] += 1
            P.dma("pool", lambda e, wb=wb, gc0=gc0, gn=gn: e.dma_start(out=wb[:, :, 0:gn], in_=win_v[:, :, gc0:gc0 + gn]),
                  writes=[wk])
            for j in grp:
                c0, n = JOBS[j]
                o = c0 - gc0
                pa, pak = pacc[cnt["t"] % 2], f"pacc{cnt['t'] % 2}"
                cnt["t"] += 1
                is_sig = (c0 >= MG0) or (ML0 + 1536 <= c0 < GT0)
                for tb in range(nblk):
                    pst, pk = ps_f[tb % 4], PSF[tb % 4]

                    def mm(e, wb=wb, n=n, o=o, tb=tb, pst=pst):
                        ins = None
                        for dc in range(8):
                            ins = e.matmul(pst[0:n, 0:bw], lhsT=wb[:, dc, o:o + n], rhs=src_T[:, dc, tb * bw:(tb + 1) * bw],
                                           start=(dc == 0), stop=(dc == 7))
                        return ins
                    rk = [(src_key, tb, dc, q) for dc in range(8) for q in range(nsub)]
                    P.op("pe", mm, reads=[wk] + rk, writes=[pk])
                    if j == 28:
                        P.op("act", lambda e, pst=pst, n=n, tb=tb, j=j: e.activation(
                            out=gacc[0:n, tb * bw:(tb + 1) * bw], in_=pst[0:n, 0:bw], func=AF.Identity, bias=C("bin", j)[0:n, :]),
                            reads=[pk, "colt"], writes=[("gacc", tb)])
                        continue
                    P.op("act", lambda e, pa=pa, pst=pst, n=n, tb=tb, j=j, is_sig=is_sig: e.activation(
                        out=pa[0:n, tb * bw:(tb + 1) * bw], in_=pst[0:n, 0:bw],
                        func=(AF.Sigmoid if is_sig else AF.Identity), bias=C("bin", j)[0:n, :]),
                        reads=[pk, "colt"], writes=[(pak, tb)])
                if j == 28:
                    gd = gTf if dst_dram is pT else gcTf
                    P.dma("sp", lambda e, gd=gd: e.dma_start(out=gd, in_=gacc[:, 0:ntok]),
                          reads=[("gacc", tb) for tb in range(nblk)], writes=[("gTf", ntok)])
                    continue
                r0 = row_of(j)
                P.dma("sp", lambda e, pa=pa, n=n, r0=r0: e.dma_start(out=dst_dram[r0:r0 + n, :], in_=pa[0:n, 0:ntok]),
                      reads=[(pak, tb) for tb in range(nblk)], writes=[("pT" if dst_dram is pT else "pcT", r0)])

    if cut == 14:
        inproj(groups_ctx[:1], hcT, "hcT", LC, pcT, lambda j: (j - 12) * 128)
        return finish(pacc[0][:, 0:256], [128, 256], BF16)
    if cut == 15:
        inproj(groups_ctx, hcT, "hcT", LC, pcT, lambda j: (j - 12) * 128)
        return finish(pacc[0][:, 0:256], [128, 256], BF16)
    if cut == 16:
        inproj(groups_lat[:1], hT, "hT", L, pT, lambda j: j * 128)
        return finish(pacc[0][:, 0:256], [128, 256], BF16)
    inproj(groups_ctx, hcT, "hcT", LC, pcT, lambda j: (j - 12) * 128)
    inproj(groups_lat, hT, "hT", L, pT, lambda j: j * 128)
    k.pT, k.pcT = pT, pcT

    if debug == "inproj":
        return finish()
    P.barrier()
    ar.release(mB)

    TWO_PI = 2.0 * math.pi
    MAGIC = 12582912.0
    lock = lambda pk: ("lock", pk)

    KF = dscr("KF", [8, 2, 128, NFFT], BF16)
    yaT = dscr("yaT", [512, L], BF16)
    k.KF, k.yaT = KF, yaT
    mH = ar.mark()
    fa_re = ar.alloc([128, 128], BF16)
    fa_im = ar.alloc([128, 128], BF16)
    tw = ar.alloc([128, 192], F32)
    lb = ar.alloc([128, 10, 128], BF16)
    ma_re = ar.alloc([128, 64, 64], BF16)
    ma_imn = ar.alloc([128, 64, 64], BF16)
    for t_, nm in ((fa_re, "fa_re"), (fa_im, "fa_im"), (tw, "tw"), (lb, "lb"), (ma_re, "ma_re"), (ma_imn, "ma_imn")):
        P.dma("sp", lambda e, t_=t_, nm=nm: e.dma_start(out=t_, in_=cst[nm]), writes=[nm])
    R1 = ar.alloc([128, 8192], BF16)
    R2 = ar.alloc([128, 8192], BF16)
    BA = ar.alloc([128, 16384], BF16)
    BB = ar.alloc([128, 16384], BF16)
    mH1 = ar.mark()
    h2T = ar.alloc([64, NFFT], BF16)
    w1f = ar.alloc([33, 64], F32)
    w2f = ar.alloc([64, 64], F32)
    w3b = ar.alloc([64, 2048], BF16)
    w3n = ar.alloc([64, 2048], BF16)
    dsc = ar.alloc([128, 4], F32)
    zb = [ar.alloc([33, 512], F32) for _ in range(2)]
    sx = [ar.alloc([64, 512], F32) for _ in range(3)]
    itile = [ar.alloc([128, 512], F32) for _ in range(2)]
    dtile = [ar.alloc([128, 512], F32) for _ in range(2)]
    tmpf = [ar.alloc([128, 512], F32) for _ in range(4)]
    kst = [ar.alloc([128, 2, 512], BF16) for _ in range(2)]
    P.dma("sp", lambda e: e.dma_start(out=w1f, in_=f_w1), writes=["w1f"])
    P.dma("sp", lambda e: e.dma_start(out=w2f, in_=f_w2), writes=["w2f"])
    P.dma("pool", lambda e: e.dma_start(out=w3b, in_=f_w3), writes=["w3b"])
    P.op("dve", lambda e: e.tensor_scalar(out=w3n, in0=w3b, scalar1=-1.0, scalar2=None, op0=ALU.mult),
         reads=["w3b"], writes=["w3n"])
    P.op("dve", lambda e: e.tensor_scalar(out=dsc, in0=C("delta", 0, 4), scalar1=1.0 / (L - 1), scalar2=None, op0=ALU.mult),
         reads=["colt"], writes=["dsc"])

    def sin_chain(pst, pk, bias_col, out_ap, okey, q):
        a, b_, c_ = sx[0], sx[1], sx[2]
        P.op("dve", lambda e: e.tensor_scalar(out=a, in0=pst[0:64, :], scalar1=bias_col, scalar2=None, op0=ALU.add),
             reads=[pk, "colt"], writes=["sx0", lock(pk)])
        P.op("dve", lambda e: e.tensor_scalar(out=b_, in0=a, scalar1=1.0 / TWO_PI, scalar2=MAGIC, op0=ALU.mult, op1=ALU.add),
             reads=["sx0"], writes=["sx1"])
        P.op("dve", lambda e: e.tensor_scalar(out=c_, in0=b_, scalar1=MAGIC, scalar2=None, op0=ALU.subtract),
             reads=["sx1"], writes=["sx2"])
        P.op("dve", lambda e: e.scalar_tensor_tensor(out=b_, in0=c_, scalar=-TWO_PI, in1=a, op0=ALU.mult, op1=ALU.add),
             reads=["sx2", "sx0"], writes=["sx1"])
        P.op("dve", lambda e: e.tensor_scalar(out=b_, in0=b_, scalar1=-math.pi, scalar2=math.pi, op0=ALU.max, op1=ALU.min),
             reads=["sx1"], writes=["sx1"])
        P.op("act", lambda e: e.activation(out=out_ap, in_=b_, func=AF.Sin), reads=["sx1"], writes=[okey])

    h1t = ar.alloc([64, 512], F32)
    for blk in range(16):
        zt, zk = zb[blk % 2], f"zb{blk % 2}"
        P.dma("sp", lambda e, zt=zt, blk=blk: e.dma_start(out=zt, in_=cst["zext"][:, blk * 512:(blk + 1) * 512]), writes=[zk])
        P.op("pe", lambda e, zt=zt: e.matmul(ps_f[0][0:64, :], lhsT=w1f, rhs=zt, start=True, stop=True),
             reads=["w1f", zk], writes=[PSF[0]])
        sin_chain(ps_f[0], PSF[0], C("fb1")[0:64, :], h1t, "h1t", 0)
        P.op("pe", lambda e: e.matmul(ps_f[1][0:64, :], lhsT=w2f, rhs=h1t, start=True, stop=True),
             reads=["w2f", "h1t"], writes=[PSF[1]])
        sin_chain(ps_f[1], PSF[1], C("fb2")[0:64, :], h2T[:, blk * 512:(blk + 1) * 512], ("h2T", blk), 1)

    ktil = R1
    Uf = R2.rearrange("p (a b) -> p a b", b=128)
    Aa = BA.rearrange("p (r c n) -> p r c n", r=2, n=64)
    At = BB.rearrange("p (r q f) -> p r q f", r=2, f=128)

    def fft_A(U_, ukey, K_):
        for g in range(16):
            pre, prk = ps_f[(g % 2) * 2], PSF[(g % 2) * 2]
            pim, pik = ps_f[(g % 2) * 2 + 1], PSF[(g % 2) * 2 + 1]

            def mmA(e, g=g, pre=pre, pim=pim):
                ins = None
                for j in range(4):
                    e.matmul(pre[:, j * 128:(j + 1) * 128], lhsT=fa_re[0:K_, :], rhs=U_[0:K_, 4 * g + j, :], start=True, stop=True)
                    ins = e.matmul(pim[:, j * 128:(j + 1) * 128], lhsT=fa_im[0:K_, :], rhs=U_[0:K_, 4 * g + j, :], start=True, stop=True)
                return ins
            P.op("pe", mmA, reads=[(ukey, g), "fa_re", "fa_im"], writes=[prk, pik])
            n0 = 4 * g
            Trb = tw[:, n0:n0 + 4].unsqueeze(2).to_broadcast([128, 4, 128])
            Tib = tw[:, 64 + n0:64 + n0 + 4].unsqueeze(2).to_broadcast([128, 4, 128])
            pre3 = pre.rearrange("p (a b) -> p a b", b=128)
            pim3 = pim.rearrange("p (a b) -> p a b", b=128)
            tb_ = (g % 2) * 4 if len(tmpf) >= 8 else 0
            ms = [tmpf[tb_ + q_] for q_ in range(4)]
            mk = [f"tmpf{tb_ + q_}" for q_ in range(4)]
            m3v = [m_.rearrange("p (a b) -> p a b", b=128) for m_ in ms]
            P.op("dve", lambda e, m3v=m3v, pre3=pre3, Trb=Trb: e.tensor_tensor(out=m3v[0], in0=pre3, in1=Trb, op=ALU.mult),
                 reads=[prk, "tw"], writes=[mk[0], lock(prk)])
            P.op("dve", lambda e, m3v=m3v, pim3=pim3, Tib=Tib: e.tensor_tensor(out=m3v[1], in0=pim3, in1=Tib, op=ALU.mult),
                 reads=[pik, "tw"], writes=[mk[1], lock(pik)])
            P.op("dve", lambda e, m3v=m3v, pre3=pre3, Tib=Tib: e.tensor_tensor(out=m3v[2], in0=pre3, in1=Tib, op=ALU.mult),
                 reads=[prk, "tw"], writes=[mk[2], lock(prk)])
            P.op("dve", lambda e, m3v=m3v, pim3=pim3, Trb=Trb: e.tensor_tensor(out=m3v[3], in0=pim3, in1=Trb, op=ALU.mult),
                 reads=[pik, "tw"], writes=[mk[3], lock(pik)])
            mp = [m_.rearrange("p (a b) -> p b a", b=128) for m_ in ms]
            P.op("pool", lambda e, mp=mp, n0=n0: e.tensor_tensor(out=Aa[:, 0, :, n0:n0 + 4], in0=mp[0], in1=mp[1], op=ALU.subtract),
                 reads=[mk[0], mk[1]], writes=[("Aa", 0, n0 + q_) for q_ in range(4)])
            P.op("pool", lambda e, mp=mp, n0=n0: e.tensor_tensor(out=Aa[:, 1, :, n0:n0 + 4], in0=mp[2], in1=mp[3], op=ALU.add),
                 reads=[mk[2], mk[3]], writes=[("Aa", 1, n0 + q_) for q_ in range(4)])

    def fft_T1():
        bi = 0
        for ri in range(2):
            for q in range(8):
                pb, pbk = ps_b[bi % 2], PSB[bi % 2]

                def trT(e, ri=ri, q=q, pb=pb):
                    ins = None
                    for j in range(8):
                        p_ = 8 * q + j
                        ins = e.transpose(pb[:, j * 128:(j + 1) * 128],
                                          Aa[:, ri, 2 * p_:2 * p_ + 2, :].rearrange("p a b -> p (a b)"), ident_bf)
                    return ins
                P.op("pe", trT, reads=[("Aa", ri, n2) for n2 in range(64)] + ["ident_bf"], writes=[pbk])
                dst = At[:, ri, 8 * q:8 * q + 8, :].rearrange("p a b -> p (a b)")
                if bi % 2 == 0:
                    P.op("act", lambda e, dst=dst, pb=pb: e.copy(out=dst, in_=pb), reads=[pbk], writes=[("At", ri, q), lock(pbk)])
                else:
                    P.op("dve", lambda e, dst=dst, pb=pb: e.tensor_copy(out=dst, in_=pb), reads=[pbk], writes=[("At", ri, q), lock(pbk)])
                bi += 1

    for o in range(2):
        for ct in range(4):
            fidx = o * 4 + ct
            for blk in range(16):
                side = blk // 8
                col0 = o * 1024 + side * 512 + ct * 128
                w3x, w3k = (w3b, "w3b") if side == 0 else (w3n, "w3n")
                pst, pk = ps_f[4 + blk % 2], PSF[4 + blk % 2]
                it_, itk = itile[blk % 2], f"it{blk % 2}"
                dt_, dtk = dtile[blk % 2], f"dt{blk % 2}"
                P.op("pe", lambda e, pst=pst, w3x=w3x, col0=col0, blk=blk: e.matmul(
                    pst, lhsT=w3x[:, col0:col0 + 128], rhs=h2T[:, blk * 512:(blk + 1) * 512], start=True, stop=True),
                    reads=[w3k, ("h2T", blk)], writes=[pk])
                if side == 0:
                    P.op("pool", lambda e, it_=it_, blk=blk: e.iota(it_, pattern=[[1, 512]], base=blk * 512, channel_multiplier=0,
                                                                    allow_small_or_imprecise_dtypes=True), writes=[itk])
                else:
                    P.op("pool", lambda e, it_=it_, blk=blk: e.iota(it_, pattern=[[-1, 512]], base=NFFT - blk * 512, channel_multiplier=0,
                                                                    allow_small_or_imprecise_dtypes=True), writes=[itk])
                P.op("act", lambda e, it_=it_, dt_=dt_, ct=ct: e.activation(out=dt_, in_=it_, func=AF.Exp, scale=dsc[:, ct:ct + 1]),
                     reads=[itk, "dsc"], writes=[dtk])
                P.op("dve", lambda e, pst=pst, dt_=dt_, blk=blk: e.tensor_tensor(
                    out=ktil[:, blk * 512:(blk + 1) * 512], in0=pst, in1=dt_, op=ALU.mult),
                    reads=[pk, dtk], writes=[("ktil", blk), lock(pk)])
            P.op("dve", lambda e: e.memset(ktil[:, L:L + 1], 0.0), reads=[("ktil", 8)], writes=[("ktil", 8)])
            kv = ktil.rearrange("p (a b) -> p a b", b=64)
            for g in range(16):
                pb, pbk = ps_b[g % 2], PSB[g % 2]

                def tr0(e, g=g, pb=pb):
                    ins = None
                    for j in range(4):
                        ins = e.transpose(pb[:, j * 128:(j + 1) * 128], kv[:, :, 4 * g + j], ident_bf)
                    return ins
                P.op("pe", tr0, reads=[("ktil", b_) for b_ in range(16)] + ["ident_bf"], writes=[pbk])
                dst = Uf[:, 4 * g:4 * g + 4, :].rearrange("p a b -> p (a b)")
                if g % 2 == 0:
                    P.op("act", lambda e, dst=dst, pb=pb: e.copy(out=dst, in_=pb[:, 0:512]), reads=[pbk], writes=[("Uf", g), lock(pbk)])
                else:
                    P.op("dve", lambda e, dst=dst, pb=pb: e.tensor_copy(out=dst, in_=pb[:, 0:512]), reads=[pbk], writes=[("Uf", g), lock(pbk)])
            fft_A(Uf, "Uf", 128)
            fft_T1()
            for g in range(16):
                pa_, pak_ = ps_f[(g % 2) * 2], PSF[(g % 2) * 2]
                pb_, pbk_ = ps_f[(g % 2) * 2 + 1], PSF[(g % 2) * 2 + 1]
                rr = At[:, 0, 4 * g:4 * g + 4, :].rearrange("p a b -> p (a b)")
                ri_ = At[:, 1, 4 * g:4 * g + 4, :].rearrange("p a b -> p (a b)")

                def mmB(e, pa_=pa_, pb_=pb_, rr=rr, ri_=ri_):
                    e.matmul(pa_, lhsT=lb[:, 4, :], rhs=rr, start=True, stop=False)
                    e.matmul(pa_, lhsT=lb[:, 5, :], rhs=ri_, start=False, stop=True)
                    e.matmul(pb_, lhsT=lb[:, 6, :], rhs=rr, start=True, stop=False)
                    return e.matmul(pb_, lhsT=lb[:, 7, :], rhs=ri_, start=False, stop=True)
                rkeys = [("At", r_, g // 2) for r_ in range(2)]
                P.op("pe", mmB, reads=rkeys + ["lb"], writes=[pak_, pbk_])
                ks_, ksk = kst[g % 2], f"kst{g % 2}"
                P.op("act", lambda e, ks_=ks_, pa_=pa_: e.copy(out=ks_[:, 0, :], in_=pa_), reads=[pak_], writes=[(ksk, 0), lock(pak_)])
                P.op("dve", lambda e, ks_=ks_, pb_=pb_: e.tensor_copy(out=ks_[:, 1, :], in_=pb_), reads=[pbk_], writes=[(ksk, 1), lock(pbk_)])
                for r_ in range(2):
                    P.dma("sp", lambda e, ks_=ks_, r_=r_, g=g, fidx=fidx: e.dma_start(
                        out=KF[fidx, r_, :, g * 512:(g + 1) * 512], in_=ks_[:, r_, :]),
                        reads=[(ksk, r_)], writes=[("KF", fidx, r_, g)])
    if debug == "filt":
        return finish()
    P.barrier()
    ar.release(mH1)

    raw = ar.alloc([128, L], BF16)
    ubuf = [ar.alloc([128, L], BF16) for _ in range(2)]
    gbuf = ar.alloc([128, L], BF16)
    kfb = [ar.alloc([128, 2, 512], BF16) for _ in range(2)]
    tmpf = [ar.alloc([128, 512], F32) for _ in range(8)]
    U_ = R1.rearrange("p (a b) -> p a b", b=128)
    Zs = R2.rearrange("p (q f) -> p q f", f=128)
    Vs = BA.rearrange("p (r q f) -> p r q f", r=2, f=128)
    Vt = BB.rearrange("p (r t c) -> p r t c", r=2, c=128)

    def sconv(dst, dkey, src, skey, wname, widx, ntile, rows, rowlen, post=None):
        w0, w1_, w2_ = (C(wname, t * ntile + widx) for t in range(3))
        n_ = rows * rowlen
        d3 = dst[:, 0:n_].rearrange("p (r j) -> p r j", j=rowlen)
        s3 = src[:, 0:n_].rearrange("p (r j) -> p r j", j=rowlen)
        P.op("dve", lambda e: e.tensor_scalar(out=dst[:, 0:n_], in0=src[:, 0:n_], scalar1=w1_, scalar2=None, op0=ALU.mult),
             reads=[skey, "colt"], writes=[dkey])
        P.op("dve", lambda e: e.scalar_tensor_tensor(out=d3[:, :, 1:rowlen], in0=s3[:, :, 0:rowlen - 1], scalar=w0,
                                                     in1=d3[:, :, 1:rowlen], op0=ALU.mult, op1=ALU.add),
             reads=[skey, dkey, "colt"], writes=[dkey])
        P.op("dve", lambda e: e.scalar_tensor_tensor(out=d3[:, :, 0:rowlen - 1], in0=s3[:, :, 1:rowlen], scalar=w2_,
                                                     in1=d3[:, :, 0:rowlen - 1], op0=ALU.mult, op1=ALU.add),
             reads=[skey, dkey, "colt"], writes=[dkey])
        if post is not None:
            P.op("act", lambda e: e.activation(out=dst[:, 0:n_], in_=dst[:, 0:n_], func=post), reads=[dkey], writes=[dkey])
    k.sconv = sconv

    def long_conv(u, ukeys, fidx, skipcol, gate, gkey, zout, zkeys):
        uv = u.rearrange("p (a b) -> p a b", b=64)
        for g in range(16):
            pb, pbk = ps_b[g % 2], PSB[g % 2]

            def tr0(e, g=g, pb=pb):
                ins = None
                for j in range(4):
                    ins = e.transpose(pb[0:64, j * 128:(j + 1) * 128], uv[:, :, 4 * g + j], ident_bf)
                return ins
            P.op("pe", tr0, reads=list(ukeys) + ["ident_bf"], writes=[pbk])
            dst = U_[0:64, 4 * g:4 * g + 4, :].rearrange("p a b -> p (a b)")
            if g % 2 == 0:
                P.op("act", lambda e, dst=dst, pb=pb: e.copy(out=dst, in_=pb[0:64, 0:512]), reads=[pbk], writes=[("U", g), lock(pbk)])
            else:
                P.op("dve", lambda e, dst=dst, pb=pb: e.tensor_copy(out=dst, in_=pb[0:64, 0:512]), reads=[pbk], writes=[("U", g), lock(pbk)])
        fft_A(U_, "U", 64)
        fft_T1()
        for g in range(16):
            pa_, pak_ = ps_f[(g % 2) * 2], PSF[(g % 2) * 2]
            pb_, pbk_ = ps_f[(g % 2) * 2 + 1], PSF[(g % 2) * 2 + 1]
            rr = At[:, 0, 4 * g:4 * g + 4, :].rearrange("p a b -> p (a b)")
            ri_ = At[:, 1, 4 * g:4 * g + 4, :].rearrange("p a b -> p (a b)")
            kf_, kfk = kfb[g % 2], f"kfb{g % 2}"
            P.dma("sp", lambda e, kf_=kf_, g=g: e.dma_start(out=kf_, in_=KF[fidx, :, :, g * 512:(g + 1) * 512].rearrange("r p f -> p r f")),
                  reads=[("KF", fidx, r_, g) for r_ in range(2)], writes=[kfk])

            def mmB(e, pa_=pa_, pb_=pb_, rr=rr, ri_=ri_):
                e.matmul(pa_, lhsT=lb[:, 0, :], rhs=rr, start=True, stop=False)
                e.matmul(pa_, lhsT=lb[:, 1, :], rhs=ri_, start=False, stop=True)
                e.matmul(pb_, lhsT=lb[:, 2, :], rhs=rr, start=True, stop=False)
                return e.matmul(pb_, lhsT=lb[:, 3, :], rhs=ri_, start=False, stop=True)
            P.op("pe", mmB, reads=[("At", r_, g // 2) for r_ in range(2)] + ["lb"], writes=[pak_, pbk_])
            ta, tak = tmpf[(g % 2) * 2], f"tmpf{(g % 2) * 2}"
            tb_, tbk = tmpf[(g % 2) * 2 + 1], f"tmpf{(g % 2) * 2 + 1}"
            P.op("dve", lambda e, ta=ta, pa_=pa_, kf_=kf_: e.tensor_tensor(out=ta, in0=pa_, in1=kf_[:, 0, :], op=ALU.mult),
                 reads=[pak_, kfk], writes=[tak, lock(pak_)])
            P.op("dve", lambda e, tb_=tb_, pb_=pb_, kf_=kf_: e.tensor_tensor(out=tb_, in0=pb_, in1=kf_[:, 1, :], op=ALU.mult),
                 reads=[pbk_, kfk], writes=[tbk, lock(pbk_)])
            zd = Zs[:, 4 * g:4 * g + 4, :].rearrange("p a b -> p (a b)")
            P.op("pool", lambda e, zd=zd, ta=ta, tb_=tb_: e.tensor_tensor(out=zd, in0=ta, in1=tb_, op=ALU.add),
                 reads=[tak, tbk], writes=[("Zs", g)])
        for g in range(16):
            pa_, pak_ = ps_f[(g % 2) * 2], PSF[(g % 2) * 2]
            pb_, pbk_ = ps_f[(g % 2) * 2 + 1], PSF[(g % 2) * 2 + 1]
            zr = Zs[:, 4 * g:4 * g + 4, :].rearrange("p a b -> p (a b)")

            def mmBi(e, pa_=pa_, pb_=pb_, zr=zr):
                e.matmul(pa_, lhsT=lb[:, 8, :], rhs=zr, start=True, stop=True)
                return e.matmul(pb_, lhsT=lb[:, 9, :], rhs=zr, start=True, stop=True)
            P.op("pe", mmBi, reads=[("Zs", g), "lb"], writes=[pak_, pbk_])
            P.op("act", lambda e, pa_=pa_, g=g: e.copy(out=Vs[:, 0, 4 * g:4 * g + 4, :].rearrange("p a b -> p (a b)"), in_=pa_),
                 reads=[pak_], writes=[("Vs", 0, g), lock(pak_)])
            P.op("dve", lambda e, pb_=pb_, g=g: e.tensor_copy(out=Vs[:, 1, 4 * g:4 * g + 4, :].rearrange("p a b -> p (a b)"), in_=pb_),
                 reads=[pbk_], writes=[("Vs", 1, g), lock(pbk_)])
        bi = 0
        for ri in range(2):
            for q in range(8):
                pb, pbk = ps_b[bi % 2], PSB[bi % 2]

                def trV(e, ri=ri, q=q, pb=pb):
                    ins = None
                    for j in range(8):
                        ins = e.transpose(pb[:, j * 128:(j + 1) * 128], Vs[:, ri, 8 * q + j, :], ident_bf)
                    return ins
                P.op("pe", trV, reads=[("Vs", ri, 2 * q), ("Vs", ri, 2 * q + 1), "ident_bf"], writes=[pbk])
                dst = Vt[:, ri, :, 16 * q:16 * q + 16]
                src = pb.rearrange("p (c t) -> p t c", t=64)
                if bi % 2 == 0:
                    P.op("act", lambda e, dst=dst, src=src: e.copy(out=dst, in_=src), reads=[pbk], writes=[("Vt", ri, q), lock(pbk)])
                else:
                    P.op("dve", lambda e, dst=dst, src=src: e.tensor_copy(out=dst, in_=src), reads=[pbk], writes=[("Vt", ri, q), lock(pbk)])
                bi += 1
        u3 = u.rearrange("p (a b) -> p a b", b=64)
        g3 = gate.rearrange("p (a b) -> p a b", b=64)
        z3 = zout.rearrange("p (a b) -> p a b", b=64)
        for g in range(8):
            pst, pk = ps_f[g % 4], PSF[g % 4]

            def mmAi(e, g=g, pst=pst):
                ins = None
                for j in range(8):
                    t2 = 8 * g + j
                    e.matmul(pst[:, j * 64:(j + 1) * 64], lhsT=Vt[:, 0, t2, :], rhs=ma_re[:, t2, :], start=True, stop=False)
                    ins = e.matmul(pst[:, j * 64:(j + 1) * 64], lhsT=Vt[:, 1, t2, :], rhs=ma_imn[:, t2, :], start=False, stop=True)
                return ins
            P.op("pe", mmAi, reads=[("Vt", r_, q) for r_ in range(2) for q in range(8)] + ["ma_re", "ma_imn"], writes=[pk])
            tf, tfk = tmpf[g % 4], f"tmpf{g % 4}"
            tf3 = tf.rearrange("p (a b) -> p a b", b=8)
            psv = pst.rearrange("p (j t) -> p t j", t=64)
            P.op("dve", lambda e, tf3=tf3, psv=psv, g=g: e.scalar_tensor_tensor(
                out=tf3, in0=u3[:, :, 8 * g:8 * g + 8], scalar=skipcol, in1=psv, op0=ALU.mult, op1=ALU.add),
                reads=[pk, "colt"] + list(ukeys), writes=[tfk, lock(pk)])
            P.op("pool", lambda e, tf3=tf3, g=g: e.tensor_tensor(out=z3[:, :, 8 * g:8 * g + 8], in0=tf3, in1=g3[:, :, 8 * g:8 * g + 8], op=ALU.mult),
                 reads=[tfk, gkey], writes=zkeys(g))

    dbg_cv = None
    for ct in range(4):
        P.dma("sp", lambda e, ct=ct: e.dma_start(out=raw, in_=pT[ct * 128:(ct + 1) * 128, :]), reads=[("pT", ct * 128)], writes=["raw"])
        sconv(ubuf[0], "ub0", raw, "raw", "hyconv", ct, 12, 64, 64)
        P.dma("sp", lambda e, ct=ct: e.dma_start(out=raw, in_=pT[(4 + ct) * 128:(5 + ct) * 128, :]), reads=[("pT", (4 + ct) * 128)], writes=["raw"])
        sconv(gbuf, "gbuf", raw, "raw", "hyconv", 4 + ct, 12, 64, 64)
        long_conv(ubuf[0], ["ub0"], 0 * 4 + ct, C("hyskip", ct), gbuf, "gbuf", ubuf[1], lambda g: [("ub1w", g)])
        if debug == "hy1" and ct == 0:
            dd = nc.dram_tensor("dbg_z1", [128, L], BF16, kind="ExternalOutput").ap()
            P.dma("sp", lambda e: e.dma_start(out=dd, in_=ubuf[1]), reads=[("ub1w", g) for g in range(8)], writes=["dbgz1"])
            return finish()
        P.dma("sp", lambda e, ct=ct: e.dma_start(out=raw, in_=pT[(8 + ct) * 128:(9 + ct) * 128, :]), reads=[("pT", (8 + ct) * 128)], writes=["raw"])
        sconv(gbuf, "gbuf", raw, "raw", "hyconv", 8 + ct, 12, 64, 64)
        long_conv(ubuf[1], [("ub1w", g) for g in range(8)], 1 * 4 + ct, C("hyskip", 4 + ct), gbuf, "gbuf", ubuf[0], lambda g: ["ub0"])
        P.dma("sp", lambda e, ct=ct: e.dma_start(out=yaT[ct * 128:(ct + 1) * 128, :], in_=ubuf[0]),
              reads=["ub0"], writes=[("yaT", ct)])
    if debug == "hyena":
        return finish()
    P.barrier()
    ar.release(mH)

    hdir = dscr("hdir", [2, L, 512], F32)
    ybT = dscr("ybT", [512, L], BF16)
    k.hdir, k.ybT = hdir, ybT
    NCH = 34
    LT = LC + L
    mM = ar.mark()
    tri = [ar.alloc([128, 128], F32) for _ in range(2)]
    mask4 = [ar.alloc([128, 512], F32) for _ in range(2)]
    gsel = ar.alloc([16, 2], F32)
    for t_, nm in ((tri[0], "tri_f"), (tri[1], "tri_b"), (mask4[0], "mask4_f"), (mask4[1], "mask4_b"), (gsel, "gsel")):
        P.dma("sp", lambda e, t_=t_, nm=nm: e.dma_start(out=t_, in_=cst[nm]), writes=[nm])
    qT = [ar.alloc([128, LT], BF16) for _ in range(4)]
    kT = [ar.alloc([128, LT], BF16) for _ in range(4)]
    vT = [ar.alloc([128, LT], BF16) for _ in range(4)]
    rawm = ar.alloc([128, LT], BF16)
    mG = ar.mark()
    GL = ar.alloc([16, LT], F32)
    GLt = ar.alloc([16, LT], F32)
    GLcol = sb("GLcol", [128, NCH, 16], F32)
    bcol = sb("bcol", [128, NCH, 8], F32)
    gcol = sb("gcol", [128, NCH, 8], F32)
    totrep = sb("totrep", [128, NCH, 8], F32)
    lwe = sb("lwe", [128, NCH, 8], F32)
    mxrep = sb("mxrep", [128, NCH, 8], F32)
    mxT = sb("mxT", [128, 3], F32)
    mxD = sb("mxD", [128, 128], F32)
    P.dma("sp", lambda e: e.dma_start(out=GL[:, 0:LC], in_=gcTf), reads=[("gTf", LC)], writes=["GLa"])
    P.dma("sp", lambda e: e.dma_start(out=GL[:, LC:LT], in_=gTf), reads=[("gTf", L)], writes=["GLb"])
    P.op("act", lambda e: e.activation(out=GLt, in_=GL, func=AF.Exp, scale=-1.0), reads=["GLa", "GLb"], writes=["GLt"])
    P.op("act", lambda e: e.activation(out=GLt, in_=GLt, func=AF.Ln, bias=1.0), reads=["GLt"], writes=["GLt"])
    P.op("dve", lambda e: e.tensor_scalar(out=GL, in0=GL, scalar1=gsel[:, 0:1], scalar2=None, op0=ALU.mult),
         reads=["GLa", "GLb", "gsel"], writes=["GLa", "GLb"])
    P.op("dve", lambda e: e.scalar_tensor_tensor(out=GL, in0=GLt, scalar=gsel[:, 1:2], in1=GL, op0=ALU.mult, op1=ALU.add),
         reads=["GLt", "GLa", "GLb", "gsel"], writes=["GLa", "GLb", "GL"])
    for blk, (c0_, c1_) in enumerate(((0, 32), (32, 34))):
        def trg(e, c0_=c0_, c1_=c1_):
            ins = None
            for c_ in range(c0_, c1_):
                ins = e.transpose(ps_f[0][:, (c_ - c0_) * 16:(c_ - c0_ + 1) * 16], GL[:, c_ * 128:(c_ + 1) * 128], ident_f[0:16, 0:16])
            return ins
        P.op("pe", trg, reads=["GL", "ident_f"], writes=[PSF[0]])
        P.op("dve", lambda e, c0_=c0_, c1_=c1_: e.tensor_copy(
            out=GLcol[:, c0_:c1_, :].rearrange("p a b -> p (a b)"), in_=ps_f[0][:, 0:(c1_ - c0_) * 16]),
            reads=[PSF[0]], writes=[("GLcol", blk), lock(PSF[0])])

    def mm_bt(e):
        ins = None
        for c_ in range(NCH):
            for d_ in range(2):
                lf_ = GLcol[:, c_, 8 * d_ + 4:8 * d_ + 8]
                e.matmul(ps_f[1][:, c_ * 8 + 4 * d_:c_ * 8 + 4 * d_ + 4], lhsT=tri[d_], rhs=lf_, start=True, stop=True)
                ins = e.matmul(ps_f[2][:, c_ * 8 + 4 * d_:c_ * 8 + 4 * d_ + 4], lhsT=ones_f, rhs=lf_, start=True, stop=True)
        return ins
    P.op("pe", mm_bt, reads=[("GLcol", 0), ("GLcol", 1), "tri_f", "tri_b", "ones_f"], writes=[PSF[1], PSF[2]])
    P.op("dve", lambda e: e.tensor_copy(out=bcol.rearrange("p a b -> p (a b)"), in_=ps_f[1][:, 0:NCH * 8]),
         reads=[PSF[1]], writes=["bcol", lock(PSF[1])])
    P.op("dve", lambda e: e.tensor_copy(out=totrep.rearrange("p a b -> p (a b)"), in_=ps_f[2][:, 0:NCH * 8]),
         reads=[PSF[2]], writes=["totrep", lock(PSF[2])])
    for d_ in range(2):
        P.op("dve", lambda e, d_=d_: e.tensor_tensor(out=gcol[:, :, 4 * d_:4 * d_ + 4], in0=GLcol[:, :, 8 * d_:8 * d_ + 4],
                                                     in1=bcol[:, :, 4 * d_:4 * d_ + 4], op=ALU.subtract),
             reads=[("GLcol", 0), ("GLcol", 1), "bcol"], writes=[("gcol", d_)])
    P.op("dve", lambda e: e.tensor_tensor(out=lwe, in0=totrep, in1=gcol, op=ALU.add),
         reads=["totrep", ("gcol", 0), ("gcol", 1)], writes=["lwe"])
    lwe2 = lwe.rearrange("p a b -> p (a b)")
    mx2 = mxrep.rearrange("p a b -> p (a b)")
    for bi_, (a0, a1) in enumerate(((0, 128), (128, 256), (256, NCH * 8))):
        n_ = a1 - a0
        P.op("pe", lambda e, a0=a0, a1=a1, n_=n_: e.transpose(ps_f[3][0:n_, 0:128], lwe2[:, a0:a1], ident_f),
             reads=["lwe", "ident_f"], writes=[PSF[3]])
        P.op("dve", lambda e, n_=n_, bi_=bi_: e.tensor_reduce(out=mxT[0:n_, bi_:bi_ + 1], in_=ps_f[3][0:n_, 0:128], axis=AX.X, op=ALU.max),
             reads=[PSF[3]], writes=[("mxT", bi_), lock(PSF[3])])
        P.op("dve", lambda e, n_=n_, bi_=bi_: e.tensor_scalar(out=mxD[0:n_, 0:n_], in0=ident_f[0:n_, 0:n_], scalar1=mxT[0:n_, bi_:bi_ + 1],
                                                             scalar2=None, op0=ALU.mult), reads=[("mxT", bi_), "ident_f"], writes=["mxD"])
        P.op("pe", lambda e, n_=n_: e.matmul(ps_f[4][:, 0:n_], lhsT=ones_f[0:n_, :], rhs=mxD[0:n_, 0:n_], start=True, stop=True),
             reads=["mxD", "ones_f"], writes=[PSF[4]])
        P.op("dve", lambda e, a0=a0, a1=a1, n_=n_: e.tensor_copy(out=mx2[:, a0:a1], in_=ps_f[4][:, 0:n_]),
             reads=[PSF[4]], writes=[("mxrep", bi_), lock(PSF[4])])
    P.barrier()
    ar.release(mG)

    QS = MLSTM_SCALE = 128 ** -0.5
    for h in range(4):
        for which, dstl, jl, jc in (("q", qT, 12 + h, 0 + h), ("k", kT, 16 + h, 4 + h), ("v", vT, 20 + h, 8 + h)):
            tgt = dstl[h] if which == "v" else rawm
            tk = f"{which}T{h}" if which == "v" else "rawm"
            P.dma("sp", lambda e, tgt=tgt, jc=jc: e.dma_start(out=tgt[:, 0:LC], in_=pcT[jc * 128:(jc + 1) * 128, :]),
                  reads=[("pcT", jc * 128)], writes=[(tk, 0)])
            P.dma("sp", lambda e, tgt=tgt, jl=jl: e.dma_start(out=tgt[:, LC:LT], in_=pT[jl * 128:(jl + 1) * 128, :]),
                  reads=[("pT", jl * 128)], writes=[(tk, 1)])
            if which == "v":
                continue
            widx = h if which == "q" else 4 + h
            dk_ = f"{which}T{h}"
            sconv(dstl[h][:, 0:LC], (dk_, 0), rawm[:, 0:LC], ("rawm", 0), "mlconv", widx, 8, 1, LC, post=AF.Silu)
            sconv(dstl[h][:, LC:LT], (dk_, 1), rawm[:, LC:LT], ("rawm", 1), "mlconv", widx, 8, 64, 64, post=AF.Silu)
            if which == "q":
                P.op("act", lambda e, h=h: e.mul(out=qT[h][:, LC:LT], in_=qT[h][:, LC:LT], mul=QS),
                     reads=[(dk_, 1)], writes=[(dk_, 1)])

    ST = []
    for d_ in range(2):
        st = K()
        st.C = ar.alloc([128, 4, 128], F32)
        st.n = ar.alloc([128, 4], F32)
        st.m = ar.alloc([128, 4], F32)
        st.Cbf = ar.alloc([128, 4, 128], BF16)
        st.nbf = ar.alloc([128, 4], BF16)
        st.kv = ar.alloc([128, 8, 128], BF16)
        st.dg = ar.alloc([128, 4, 128], F32)
        st.lw = ar.alloc([128, 4, 128], F32)
        st.A = ar.alloc([128, 4, 128], BF16)
        st.AT = ar.alloc([128, 4, 128], BF16)
        st.vw = ar.alloc([128, 4, 128], BF16)
        st.t1 = ar.alloc([128, 4, 128], F32)
        st.t2 = ar.alloc([128, 4, 128], F32)
        st.sm = ar.alloc([128, 16, 4], F32)
        st.ex = ar.alloc([128, 3, 4], F32)
        st.wbf = ar.alloc([128, 4], BF16)
        P.op("pool", lambda e, st=st: e.memset(st.C, 0.0), writes=[f"C{d_}"])
        P.op("pool", lambda e, st=st: e.memset(st.n, 0.0), writes=[f"n{d_}"])
        P.op("pool", lambda e, st=st: e.memset(st.m, 0.0), writes=[f"m{d_}"])
        ST.append(st)
    psL, psS, psN, psQ, psU, psM = ps_f
    kL, kS, kN, kQ, kU, kM = PSF

    def bc4(ap4):
        return ap4.unsqueeze(2).to_broadcast([128, 4, 128])

    def v3(ps):
        return ps.rearrange("p (a b) -> p a b", b=128)

    def chunk_step(c_, d_, with_out):
        st = ST[d_]
        sk = lambda nm: f"{nm}{d_}"
        cs = slice(c_ * 128, (c_ + 1) * 128)
        h4 = slice(4 * d_, 4 * d_ + 4)
        pb, pbk = ps_b[d_], PSB[d_]
        sm = st.sm
        half = 0 if c_ < 2 else 1
        def trkv(e):
            ins = None
            for h in range(4):
                e.transpose(pb[:, h * 128:(h + 1) * 128], kT[h][:, cs], ident_bf)
                ins = e.transpose(pb[:, 512 + h * 128:512 + (h + 1) * 128], vT[h][:, cs], ident_bf)
            return ins
        P.op("pe", trkv, reads=[(f"kT{h}", half) for h in range(4)] + [(f"vT{h}", half) for h in range(4)] + ["ident_bf"], writes=[pbk])
        P.op("act", lambda e: e.copy(out=st.kv.rearrange("p a b -> p (a b)"), in_=pb), reads=[pbk], writes=[sk("kv"), lock(pbk)])
        if with_out:
            P.op("pool", lambda e: e.tensor_tensor(out=st.dg, in0=ident_f.unsqueeze(1).to_broadcast([128, 4, 128]),
                                                  in1=bc4(gcol[:, c_, h4]), op=ALU.mult),
                 reads=[("gcol", d_), "ident_f"], writes=[sk("dg")])

            def mmL(e):
                e.matmul(psL, lhsT=ones_f, rhs=st.dg.rearrange("p a b -> p (a b)"), start=True, stop=False)
                return e.matmul(psL, lhsT=ident_f, rhs=mask4[d_], start=False, stop=True)
            P.op("pe", mmL, reads=[sk("dg"), "ones_f", "ident_f", "mask4_f", "mask4_b"], writes=[kL])
            P.op("dve", lambda e: e.tensor_tensor(out=st.lw, in0=v3(psL), in1=bc4(bcol[:, c_, h4]), op=ALU.add),
                 reads=[kL, "bcol"], writes=[sk("lw"), lock(kL)])
            P.op("dve", lambda e: e.tensor_reduce(out=sm[:, 0, :], in_=st.lw, axis=AX.X, op=ALU.max), reads=[sk("lw")], writes=[sk("sm0")])
            P.op("dve", lambda e: e.tensor_tensor(out=sm[:, 1, :], in0=bcol[:, c_, h4], in1=st.m, op=ALU.add),
                 reads=["bcol", sk("m")], writes=[sk("sm1")])
            P.op("dve", lambda e: e.tensor_tensor(out=sm[:, 2, :], in0=sm[:, 1, :], in1=sm[:, 0, :], op=ALU.max),
                 reads=[sk("sm0"), sk("sm1")], writes=[sk("sm2")])
            P.op("pool", lambda e: e.tensor_tensor(out=st.lw, in0=st.lw, in1=bc4(sm[:, 0, :]), op=ALU.subtract),
                 reads=[sk("lw"), sk("sm0")], writes=[sk("lw")])
            P.op("act", lambda e: e.activation(out=st.lw, in_=st.lw, func=AF.Exp), reads=[sk("lw")], writes=[sk("lw")])

            def mmS(e):
                ins = None
                for h in range(4):
                    ins = e.matmul(psS[:, h * 128:(h + 1) * 128], lhsT=qT[h][:, cs], rhs=kT[h][:, cs], start=True, stop=True)
                return ins
            P.op("pe", mmS, reads=[(f"qT{h}", 1) for h in range(4)] + [(f"kT{h}", 1) for h in range(4)], writes=[kS])
            P.op("dve", lambda e: e.tensor_tensor(out=st.A, in0=v3(psS), in1=st.lw, op=ALU.mult),
                 reads=[kS, sk("lw")], writes=[sk("A"), lock(kS)])
            P.op("dve", lambda e: e.tensor_reduce(out=sm[:, 3, :], in_=st.A, axis=AX.X, op=ALU.add), reads=[sk("A")], writes=[sk("sm3")])

            def trA(e):
                ins = None
                for h in range(4):
                    ins = e.transpose(pb[:, h * 128:(h + 1) * 128], st.A[:, h, :], ident_bf)
                return ins
            P.op("pe", trA, reads=[sk("A"), "ident_bf"], writes=[pbk])
            P.op("act", lambda e: e.copy(out=st.AT.rearrange("p a b -> p (a b)"), in_=pb[:, 0:512]), reads=[pbk], writes=[sk("AT"), lock(pbk)])

            def mmN(e):
                ins = None
                for h in range(4):
                    ins = e.matmul(psN[:, h * 128:(h + 1) * 128], lhsT=st.AT[:, h, :], rhs=st.kv[:, 4 + h, :], start=True, stop=True)
                return ins
            P.op("pe", mmN, reads=[sk("AT"), sk("kv")], writes=[kN])
            P.op("pool", lambda e: e.tensor_copy(out=st.Cbf, in_=st.C), reads=[sk("C")], writes=[sk("Cbf")])
            P.op("pool", lambda e: e.tensor_copy(out=st.nbf, in_=st.n), reads=[sk("n")], writes=[sk("nbf")])

            def mmQ(e):
                ins = None
                for h in range(4):
                    e.matmul(psQ[:, h * 128:(h + 1) * 128], lhsT=qT[h][:, cs], rhs=st.Cbf[:, h, :], start=True, stop=True)
                    ins = e.matmul(psM[:, h:h + 1], lhsT=qT[h][:, cs], rhs=st.nbf[:, h:h + 1], start=True, stop=True)
                return ins
            P.op("pe", mmQ, reads=[(f"qT{h}", 1) for h in range(4)] + [sk("Cbf"), sk("nbf")], writes=[kQ, (kM, "q")])
            P.op("dve", lambda e: e.tensor_tensor(out=st.ex[:, 0, :], in0=sm[:, 0, :], in1=sm[:, 2, :], op=ALU.subtract),
                 reads=[sk("sm0"), sk("sm2")], writes=[sk("ex0")])
            P.op("dve", lambda e: e.tensor_tensor(out=st.ex[:, 1, :], in0=sm[:, 1, :], in1=sm[:, 2, :], op=ALU.subtract),
                 reads=[sk("sm1"), sk("sm2")], writes=[sk("ex1")])
            P.op("dve", lambda e: e.tensor_scalar(out=st.ex[:, 2, :], in0=sm[:, 2, :], scalar1=-1.0, scalar2=None, op0=ALU.mult),
                 reads=[sk("sm2")], writes=[sk("ex2")])
            P.op("act", lambda e: e.activation(out=st.ex, in_=st.ex, func=AF.Exp), reads=[sk("ex0"), sk("ex1"), sk("ex2")], writes=[sk("ex")])
            P.op("dve", lambda e: e.tensor_tensor(out=st.t1, in0=v3(psN), in1=bc4(st.ex[:, 0, :]), op=ALU.mult),
                 reads=[kN, sk("ex")], writes=[sk("t1"), lock(kN)])
            P.op("dve", lambda e: e.tensor_tensor(out=st.t2, in0=v3(psQ), in1=bc4(st.ex[:, 1, :]), op=ALU.mult),
                 reads=[kQ, sk("ex")], writes=[sk("t2"), lock(kQ)])
            P.op("pool", lambda e: e.tensor_tensor(out=st.t1, in0=st.t1, in1=st.t2, op=ALU.add), reads=[sk("t1"), sk("t2")], writes=[sk("t1")])
            P.op("dve", lambda e: e.tensor_tensor(out=sm[:, 4, :], in0=psM[:, 0:4], in1=st.ex[:, 1, :], op=ALU.mult),
                 reads=[(kM, "q"), sk("ex")], writes=[sk("sm4"), lock(kM)])
            P.op("dve", lambda e: e.tensor_tensor(out=sm[:, 5, :], in0=sm[:, 3, :], in1=st.ex[:, 0, :], op=ALU.mult),
                 reads=[sk("sm3"), sk("ex")], writes=[sk("sm5")])
            P.op("dve", lambda e: e.tensor_tensor(out=sm[:, 4, :], in0=sm[:, 4, :], in1=sm[:, 5, :], op=ALU.add),
                 reads=[sk("sm4"), sk("sm5")], writes=[sk("sm4")])
            P.op("dve", lambda e: e.tensor_scalar(out=sm[:, 10, :], in0=sm[:, 4, :], scalar1=-1.0, scalar2=None, op0=ALU.mult),
                 reads=[sk("sm4")], writes=[sk("sm10")])
            P.op("dve", lambda e: e.tensor_tensor(out=sm[:, 4, :], in0=sm[:, 4, :], in1=sm[:, 10, :], op=ALU.max),
                 reads=[sk("sm4"), sk("sm10")], writes=[sk("sm4")])
            P.op("dve", lambda e: e.tensor_tensor(out=sm[:, 4, :], in0=sm[:, 4, :], in1=st.ex[:, 2, :], op=ALU.max),
                 reads=[sk("sm4"), sk("ex")], writes=[sk("sm4")])
            P.op("dve", lambda e: e.reciprocal(out=sm[:, 5, :], in_=sm[:, 4, :]), reads=[sk("sm4")], writes=[sk("sm5")])
            P.op("pool", lambda e: e.tensor_tensor(out=st.t2, in0=st.t1, in1=bc4(sm[:, 5, :]), op=ALU.mult),
                 reads=[sk("t1"), sk("sm5")], writes=[sk("t2")])
            tok0 = (c_ - 2) * 128
            P.dma("sp", lambda e: e.dma_start(out=hdir[d_, tok0:tok0 + 128, :], in_=st.t2.rearrange("p a b -> p (a b)")),
                  reads=[sk("t2")], writes=[("hdir", d_, c_)])
        P.op("dve", lambda e: e.tensor_tensor(out=sm[:, 7, :], in0=totrep[:, c_, h4], in1=st.m, op=ALU.add),
             reads=["totrep", sk("m")], writes=[sk("sm7")])
        P.op("dve", lambda e: e.tensor_tensor(out=sm[:, 6, :], in0=sm[:, 7, :], in1=mxrep[:, c_, h4], op=ALU.max),
             reads=[sk("sm7")] + [("mxrep", b_) for b_ in range(3)], writes=[sk("sm6")])
        P.op("dve", lambda e: e.tensor_tensor(out=sm[:, 8, :], in0=lwe[:, c_, h4], in1=sm[:, 6, :], op=ALU.subtract),
             reads=["lwe", sk("sm6")], writes=[sk("sm8")])
        P.op("dve", lambda e: e.tensor_tensor(out=sm[:, 9, :], in0=sm[:, 7, :], in1=sm[:, 6, :], op=ALU.subtract),
             reads=[sk("sm7"), sk("sm6")], writes=[sk("sm9")])
        P.op("act", lambda e: e.activation(out=sm[:, 8:10, :], in_=sm[:, 8:10, :], func=AF.Exp), reads=[sk("sm8"), sk("sm9")], writes=[sk("sm89")])
        P.op("pool", lambda e: e.tensor_tensor(out=st.vw, in0=st.kv[:, 4:8, :], in1=bc4(sm[:, 8, :]), op=ALU.mult),
             reads=[sk("kv"), sk("sm89")], writes=[sk("vw")])
        P.op("dve", lambda e: e.tensor_copy(out=st.wbf, in_=sm[:, 8, :]), reads=[sk("sm89")], writes=[sk("wbf")])

        def mmU(e):
            ins = None
            for h in range(4):
                e.matmul(psU[:, h * 128:(h + 1) * 128], lhsT=st.kv[:, h, :], rhs=st.vw[:, h, :], start=True, stop=True)
                ins = e.matmul(psM[:, 8 + h:9 + h], lhsT=st.kv[:, h, :], rhs=st.wbf[:, h:h + 1], start=True, stop=True)
            return ins
        P.op("pe", mmU, reads=[sk("kv"), sk("vw"), sk("wbf")], writes=[kU, (kM, "u")])
        cread = [sk("Cbf")] if with_out else []
        P.op("pool", lambda e: e.tensor_tensor(out=st.C, in0=st.C, in1=bc4(sm[:, 9, :]), op=ALU.mult),
             reads=[sk("C"), sk("sm89")] + cread, writes=[sk("C")])
        P.op("dve", lambda e: e.tensor_tensor(out=st.C, in0=st.C, in1=v3(psU), op=ALU.add), reads=[sk("C"), kU], writes=[sk("C"), lock(kU)])
        P.op("dve", lambda e: e.tensor_tensor(out=st.n, in0=st.n, in1=sm[:, 9, :], op=ALU.mult),
             reads=[sk("n"), sk("sm89")] + ([sk("nbf")] if with_out else []), writes=[sk("n")])
        P.op("dve", lambda e: e.tensor_tensor(out=st.n, in0=st.n, in1=psM[:, 8:12], op=ALU.add), reads=[sk("n"), (kM, "u")], writes=[sk("n"), lock(kM)])
        P.op("dve", lambda e: e.tensor_copy(out=st.m, in_=sm[:, 6, :]), reads=[sk("sm6"), sk("sm7")] + ([sk("sm1")] if with_out else []), writes=[sk("m")])

    seq = [[0, 1] + list(range(2, NCH)), [1, 0] + list(range(NCH - 1, 1, -1))]
    nsteps = NCH if debug != "mlstm_ctx" else 2
    for i_ in range(nsteps):
        for d_ in range(2):
            c_ = seq[d_][i_]
            chunk_step(c_, d_, with_out=(c_ >= 2))
        if debug == "mlstm_ctx" and i_ == 1:
            dd = nc.dram_tensor("dbg_C", [2, 128, 512], F32, kind="ExternalOutput").ap()
            dn_ = nc.dram_tensor("dbg_nm", [2, 128, 8], F32, kind="ExternalOutput").ap()
            for d_ in range(2):
                P.dma("sp", lambda e, d_=d_: e.dma_start(out=dd[d_], in_=ST[d_].C.rearrange("p a b -> p (a b)")), reads=[f"C{d_}"], writes=[("dbgC", d_)])
                P.dma("sp", lambda e, d_=d_: e.dma_start(out=dn_[d_, :, 0:4], in_=ST[d_].n), reads=[f"n{d_}"], writes=[("dbgn", d_)])
                P.dma("sp", lambda e, d_=d_: e.dma_start(out=dn_[d_, :, 4:8], in_=ST[d_].m), reads=[f"m{d_}"], writes=[("dbgm", d_)])
            return finish()
    if debug == "mlstm_scan":
        return finish()

    P.barrier()
    ar.release(mM)
    mY = ar.mark()
    ybacc = [ar.alloc([128, L], BF16) for _ in range(4)]
    hsum = ar.alloc([128, NTT, 512], F32)
    hB = [ar.alloc([128, 512], F32) for _ in range(3)]
    hsq = [ar.alloc([128, 512], F32) for _ in range(2)]
    hnb = [ar.alloc([128, 512], BF16) for _ in range(2)]
    ss4 = ar.alloc([128, NTT, 4], F32)
    for i in range(NTT):
        b_, bk = hB[i % 3], f"hB{i % 3}"
        q_, qk = hsq[i % 2], f"hsq{i % 2}"
        P.dma("sp", lambda e, i=i: e.dma_start(out=hsum[:, i, :], in_=hdir[0, i * 128:(i + 1) * 128, :]), reads=[("hdir", 0, i + 2)], writes=[("hsum", i)])
        P.dma("sp", lambda e, b_=b_, i=i: e.dma_start(out=b_, in_=hdir[1, i * 128:(i + 1) * 128, :]), reads=[("hdir", 1, i + 2)], writes=[bk])
        P.op("pool", lambda e, b_=b_, i=i: e.tensor_tensor(out=hsum[:, i, :], in0=hsum[:, i, :], in1=b_, op=ALU.add), reads=[("hsum", i), bk], writes=[("hsum", i)])
        P.op("pool", lambda e, q_=q_, i=i: e.tensor_tensor(out=q_, in0=hsum[:, i, :], in1=hsum[:, i, :], op=ALU.mult), reads=[("hsum", i)], writes=[qk])
        P.op("dve", lambda e, q_=q_, i=i: e.tensor_reduce(out=ss4[:, i, :], in_=q_.rearrange("p (a b) -> p a b", b=128), axis=AX.X, op=ALU.add),
             reads=[qk], writes=[("ss4", i)])
    allss4 = [("ss4", i) for i in range(NTT)]
    P.op("dve", lambda e: e.tensor_scalar(out=ss4, in0=ss4, scalar1=1.0 / 128, scalar2=RMS_EPS, op0=ALU.mult, op1=ALU.add), reads=allss4, writes=["rs4"])
    P.op("act", lambda e: e.activation(out=ss4, in_=ss4, func=AF.Sqrt), reads=["rs4"], writes=["rs4"])
    P.op("dve", lambda e: e.reciprocal(out=ss4, in_=ss4), reads=["rs4"], writes=["rs4"])
    for i in range(NTT):
        hn_, hnk = hnb[i % 2], f"hnb{i % 2}"
        P.op("dve", lambda e, hn_=hn_, i=i: e.tensor_tensor(out=hn_.rearrange("p (a b) -> p a b", b=128), in0=hsum[:, i, :].rearrange("p (a b) -> p a b", b=128),
                                                         in1=bc4(ss4[:, i, :]), op=ALU.mult), reads=[("hsum", i), "rs4"], writes=[hnk])
        pb, pbk = ps_b[i % 2], PSB[i % 2]

        def trh(e, hn_=hn_, pb=pb):
            ins = None
            for h in range(4):
                ins = e.transpose(pb[:, h * 128:(h + 1) * 128], hn_[:, h * 128:(h + 1) * 128], ident_bf)
            return ins
        P.op("pe", trh, reads=[hnk, "ident_bf"], writes=[pbk])
        for h in range(4):
            dst = ybacc[h][:, i * 128:(i + 1) * 128]
            if i % 2 == 0:
                P.op("act", lambda e, dst=dst, pb=pb, h=h: e.activation(out=dst, in_=pb[:, h * 128:(h + 1) * 128], func=AF.Copy, scale=C("mlnorm", h)),
                     reads=[pbk, "colt"], writes=[("ybacc", h, i), lock(pbk)])
            else:
                P.op("dve", lambda e, dst=dst, pb=pb, h=h: e.tensor_scalar(out=dst, in0=pb[:, h * 128:(h + 1) * 128], scalar1=C("mlnorm", h), scalar2=None,
                                                                        op0=ALU.mult), reads=[pbk, "colt"], writes=[("ybacc", h, i), lock(pbk)])
    og = ar.alloc([128, L], BF16)
    for h in range(4):
        P.dma("sp", lambda e, h=h: e.dma_start(out=og, in_=pT[(24 + h) * 128:(25 + h) * 128, :]), reads=[("pT", (24 + h) * 128)], writes=["og"])
        P.op("pool", lambda e, h=h: e.tensor_tensor(out=ybacc[h], in0=ybacc[h], in1=og, op=ALU.mult),
             reads=["og"] + [("ybacc", h, i) for i in range(NTT)], writes=[("ybf", h)])
        P.dma("sp", lambda e, h=h: e.dma_start(out=ybT[h * 128:(h + 1) * 128, :], in_=ybacc[h]), reads=[("ybf", h)], writes=[("ybT", h)])
    if debug == "mlstm":
        return finish()
    P.barrier()
    ar.release(mY)

    x1d = dscr("x1d", [L, D], F32)
    hx2d = dscr("hx2d", [L, D], BF16)
    affd = dscr("affd", [128, NTT * NEXP], F32)
    idx_i = ar.alloc([128, 64], I32)
    idx_f = ar.alloc([128, 64], F32)
    gate_s = ar.alloc([128, 64], F32)
    sc9 = ar.alloc([128, NTT], F32)
    mS6 = ar.mark()
    aff_all = ar.alloc([128, NTT, NEXP], F32)
    m6 = ar.mark()
    wbh = ar.alloc([128, 4, D], BF16)
    wbm = ar.alloc([128, 4, D], BF16)
    wout = ar.alloc([128, 8, D], BF16)
    wrt = ar.alloc([128, 8, NEXP], BF16)
    P.dma("pool", lambda e: e.dma_start(out=wbh, in_=w_bh.rearrange("(c p) n -> p c n", p=128)), writes=["wbh"])
    P.dma("pool", lambda e: e.dma_start(out=wbm, in_=w_bm.rearrange("(c p) n -> p c n", p=128)), writes=["wbm"])
    P.dma("pool", lambda e: e.dma_start(out=wout, in_=w_out.rearrange("(c p) n -> p c n", p=128)), writes=["wout"])
    P.dma("pool", lambda e: e.dma_start(out=wrt, in_=w_rt.rearrange("(c p) n -> p c n", p=128)), writes=["wrt"])
    yab = ar.alloc([128, 4, 512], BF16)
    ybb = ar.alloc([128, 4, 512], BF16)
    gab = ar.alloc([128, 8, 512], BF16)
    gbb = ar.alloc([128, 8, 512], BF16)
    ymT = ar.alloc([128, 8, 512], BF16)
    mt1 = [ar.alloc([128, 512], F32) for _ in range(2)]
    mt2 = [ar.alloc([128, 512], F32) for _ in range(2)]
    xt6 = [ar.alloc([128, D], F32) for _ in range(2)]
    x1t = [ar.alloc([128, D], F32) for _ in range(2)]
    h2f = [ar.alloc([128, D], F32) for _ in range(2)]
    h2b = [ar.alloc([128, D], BF16) for _ in range(2)]
    hx2T = ar.alloc([128, 8, 128], BF16)
    junk6 = ar.alloc([128, D], BF16)
    sc6 = ar.alloc([128, NTT, 4], F32)
    etile = ar.alloc([128, 2, NEXP], F32)
    for tb in range(8):
        ts_ = slice(tb * 512, (tb + 1) * 512)
        P.dma("sp", lambda e, ts_=ts_: e.dma_start(out=yab, in_=yaT[:, ts_].rearrange("(c p) t -> p c t", p=128)),
              reads=[("yaT", ct) for ct in range(4)], writes=["yab"])
        P.dma("sp", lambda e, ts_=ts_: e.dma_start(out=ybb, in_=ybT[:, ts_].rearrange("(c p) t -> p c t", p=128)),
              reads=[("ybT", h) for h in range(4)], writes=["ybb"])
        P.dma("sp", lambda e, ts_=ts_: e.dma_start(out=gab, in_=pT[29 * 128:37 * 128, ts_].rearrange("(c p) t -> p c t", p=128)),
              reads=[("pT", (29 + j) * 128) for j in range(8)], writes=["gab"])
        P.dma("sp", lambda e, ts_=ts_: e.dma_start(out=gbb, in_=pT[37 * 128:45 * 128, ts_].rearrange("(c p) t -> p c t", p=128)),
              reads=[("pT", (37 + j) * 128) for j in range(8)], writes=["gbb"])
        for dm in range(8):
            pa_, pak_ = ps_f[(dm % 2) * 2], PSF[(dm % 2) * 2]
            pb_, pbk_ = ps_f[(dm % 2) * 2 + 1], PSF[(dm % 2) * 2 + 1]

            def mmM(e, dm=dm, pa_=pa_, pb_=pb_):
                ins = None
                for ct in range(4):
                    e.matmul(pa_, lhsT=wbh[:, ct, dm * 128:(dm + 1) * 128], rhs=yab[:, ct, :], start=(ct == 0), stop=(ct == 3))
                for ct in range(4):
                    ins = e.matmul(pb_, lhsT=wbm[:, ct, dm * 128:(dm + 1) * 128], rhs=ybb[:, ct, :], start=(ct == 0), stop=(ct == 3))
                return ins
            P.op("pe", mmM, reads=["wbh", "wbm", "yab", "ybb"], writes=[pak_, pbk_])
            t1_, t1k = mt1[dm % 2], f"mt1{dm % 2}"
            t2_, t2k = mt2[dm % 2], f"mt2{dm % 2}"
            P.op("dve", lambda e, t1_=t1_, pa_=pa_, dm=dm: e.tensor_tensor(out=t1_, in0=pa_, in1=gab[:, dm, :], op=ALU.mult),
                 reads=[pak_, "gab"], writes=[t1k, lock(pak_)])
            P.op("dve", lambda e, t2_=t2_, pb_=pb_, dm=dm: e.tensor_tensor(out=t2_, in0=pb_, in1=gbb[:, dm, :], op=ALU.mult),
                 reads=[pbk_, "gbb"], writes=[t2k, lock(pbk_)])
            P.op("pool", lambda e, t1_=t1_, t2_=t2_, dm=dm: e.tensor_tensor(out=ymT[:, dm, :], in0=t1_, in1=t2_, op=ALU.add),
                 reads=[t1k, t2k], writes=[("ymT", dm)])
        for sub in range(4):
            i = tb * 4 + sub
            xx, xk = xt6[i % 2], f"xt6{i % 2}"
            x1_, x1k = x1t[i % 2], f"x1t{i % 2}"
            hf_, hfk = h2f[i % 2], f"h2f{i % 2}"
            hb_, hbk = h2b[i % 2], f"h2b{i % 2}"
            P.dma("sp", lambda e, xx=xx, i=i: e.dma_start(out=xx, in_=x_t[i]), writes=[xk])
            for half in range(2):
                py, pyk = ps_f[4 + half], PSF[4 + half]
                hs = slice(half * 512, (half + 1) * 512)

                def mmO(e, sub=sub, hs=hs, py=py):
                    ins = None
                    for dm in range(8):
                        ins = e.matmul(py, lhsT=ymT[:, dm, sub * 128:(sub + 1) * 128], rhs=wout[:, dm, hs], start=(dm == 0), stop=(dm == 7))
                    return ins
                P.op("pe", mmO, reads=[("ymT", dm) for dm in range(8)] + ["wout"], writes=[pyk])
                P.op("dve", lambda e, py=py, hs=hs, x1_=x1_: e.tensor_tensor(out=x1_[:, hs], in0=py, in1=bc["ada2"][:, hs], op=ALU.mult),
                     reads=[pyk, ("bc", "ada2", 0), ("bc", "ada2", 1)], writes=[(x1k, half), lock(pyk)])
                P.op("pool", lambda e, hs=hs, x1_=x1_, xx=xx: e.tensor_tensor(out=x1_[:, hs], in0=x1_[:, hs], in1=xx[:, hs], op=ALU.add),
                     reads=[(x1k, half), xk], writes=[(x1k, half)])
            P.dma("sp", lambda e, x1_=x1_, i=i: e.dma_start(out=x1d[i * 128:(i + 1) * 128, :], in_=x1_),
                  reads=[(x1k, 0), (x1k, 1)], writes=[("x1d", i)])
            P.op("act", lambda e, x1_=x1_, i=i: e.activation(out=junk6, in_=x1_, func=AF.Square, accum_out=sc6[:, i, 0:1]),
                 reads=[(x1k, 0), (x1k, 1)], writes=["junk6", ("sc6a", i)])
            P.op("dve", lambda e, i=i: e.tensor_scalar(out=sc6[:, i, 0:1], in0=sc6[:, i, 0:1], scalar1=1.0 / D, scalar2=RMS_EPS, op0=ALU.mult, op1=ALU.add),
                 reads=[("sc6a", i)], writes=[("sc6a", i)])
            P.op("act", lambda e, i=i: e.activation(out=sc6[:, i, 0:1], in_=sc6[:, i, 0:1], func=AF.Sqrt), reads=[("sc6a", i)], writes=[("sc6a", i)])
            P.op("dve", lambda e, i=i: e.reciprocal(out=sc6[:, i, 0:1], in_=sc6[:, i, 0:1]), reads=[("sc6a", i)], writes=[("sc6a", i)])
            P.op("dve", lambda e, i=i, hf_=hf_, x1_=x1_: e.scalar_tensor_tensor(out=hf_, in0=x1_, scalar=sc6[:, i, 0:1], in1=bc["G2"], op0=ALU.mult, op1=ALU.mult),
                 reads=[(x1k, 0), (x1k, 1), ("sc6a", i), ("bc", "G2", 0), ("bc", "G2", 1)], writes=[hfk])
            P.op("pool", lambda e, hf_=hf_, hb_=hb_: e.tensor_tensor(out=hb_, in0=hf_, in1=bc["S2"], op=ALU.add),
                 reads=[hfk, ("bc", "S2", 0), ("bc", "S2", 1)], writes=[hbk])
            P.dma("sp", lambda e, hb_=hb_, i=i: e.dma_start(out=hx2d[i * 128:(i + 1) * 128, :], in_=hb_), reads=[hbk], writes=[("hx2d", i)])
            pb, pbk = ps_b[i % 2], PSB[i % 2]

            def trH(e, hb_=hb_, pb=pb):
                ins = None
                for dc in range(8):
                    ins = e.transpose(pb[:, dc * 128:(dc + 1) * 128], hb_[:, dc * 128:(dc + 1) * 128], ident_bf)
                return ins
            P.op("pe", trH, reads=[hbk, "ident_bf"], writes=[pbk])
            P.op("act", lambda e, pb=pb: e.copy(out=hx2T.rearrange("p a b -> p (a b)"), in_=pb), reads=[pbk], writes=["hx2T", lock(pbk)])
            pr, prk = ps_f[sub % 4], PSF[sub % 4]

            def mmR(e, pr=pr):
                ins = None
                for dc in range(8):
                    ins = e.matmul(pr[:, 0:NEXP], lhsT=hx2T[:, dc, :], rhs=wrt[:, dc, :], start=(dc == 0), stop=(dc == 7))
                return ins
            P.op("pe", mmR, reads=["hx2T", "wrt"], writes=[prk])
            P.op("dve", lambda e, pr=pr, i=i: e.tensor_reduce(out=sc6[:, i, 1:2], in_=pr[:, 0:NEXP], axis=AX.X, op=ALU.max, negate=True),
                 reads=[prk], writes=[("sc6b", i), lock(prk)])
            et = etile[:, i % 2, :]
            P.op("act", lambda e, pr=pr, i=i, et=et: e.activation(out=et, in_=pr[:, 0:NEXP], func=AF.Exp, bias=sc6[:, i, 1:2], accum_out=sc6[:, i, 2:3]),
                 reads=[prk, ("sc6b", i)], writes=[("etile", i % 2), ("sc6c", i), lock(prk)])
            P.op("dve", lambda e, i=i: e.reciprocal(out=sc6[:, i, 2:3], in_=sc6[:, i, 2:3]), reads=[("sc6c", i)], writes=[("sc6c", i)])
            P.op("dve", lambda e, i=i, et=et: e.tensor_scalar(out=aff_all[:, i, :], in0=et, scalar1=sc6[:, i, 2:3], scalar2=None, op0=ALU.mult),
                 reads=[("etile", i % 2), ("sc6c", i)], writes=[("aff", i)])
    if debug == "merge":
        P.dma("sp", lambda e: e.dma_start(out=affd, in_=aff_all.rearrange("p a b -> p (a b)")), reads=[("aff", i) for i in range(NTT)], writes=["affd"])
        return finish()
    P.barrier()
    ar.release(m6)
    w1b = [ar.alloc([128, 8, 512], BF16) for _ in range(2)]
    w3b_ = [ar.alloc([128, 8, 512], BF16) for _ in range(2)]
    w2b = [ar.alloc([128, 16, D], BF16) for _ in range(2)]
    mW = ar.mark()
    n_exp = NEXP if debug != "moe1" else 1
    def load_w2(ex):
        w2_, w2k = w2b[ex % 2], f"w2b{ex % 2}"
        P.dma("pool", lambda e: e.dma_start(out=w2_[:, 0:8, :], in_=w_e2[ex, 0:1024, :].rearrange("(c p) n -> p c n", p=128)), writes=[(w2k, 0)])
        P.dma("pool", lambda e: e.dma_start(out=w2_[:, 8:16, :], in_=w_e2[ex, 1024:2048, :].rearrange("(c p) n -> p c n", p=128)), writes=[(w2k, 1)])

    def load_w13(ex, fb):
        q_ = (ex * 4 + fb) % 2
        fs = slice(fb * 512, (fb + 1) * 512)
        P.dma("pool", lambda e: e.dma_start(out=w1b[q_], in_=w_e1[ex].rearrange("(c p) f -> p c f", p=128)[:, :, fs]), writes=[f"w1b{q_}"])
        P.dma("pool", lambda e: e.dma_start(out=w3b_[q_], in_=w_e3[ex].rearrange("(c p) f -> p c f", p=128)[:, :, fs]), writes=[f"w3b{q_}"])

    if debug in (None, "moe", "moe1", "all"):
        load_w2(0)
        load_w13(0, 0)
        load_w13(0, 1)

    idxd = dscr("idxd", [128, 64], I32)
    gated = dscr("gated", [128, 64], F32)
    m7 = ar.mark()
    lo = ar.alloc([128, NEXP], F32)
    hi = ar.alloc([128, NEXP], F32)
    mid = ar.alloc([128, NEXP], F32)
    cntp = ar.alloc([128, NEXP], F32)
    gef = ar.alloc([128, NEXP], F32)
    dlt = ar.alloc([128, NEXP], F32)
    cmpb = ar.alloc([128, NTT, NEXP], BF16)
    maskf = ar.alloc([128, NTT, NEXP], F32)
    rank = ar.alloc([128, NTT, NEXP], F32)
    offs = ar.alloc([128, NTT, NEXP], F32)
    tris_bf = ar.alloc([128, 128], BF16)
    ones_bf = ar.alloc([128, 128], BF16)
    iot = ar.alloc([128, 512], F32)
    Rv = ar.alloc([128, NTT, NEXP, 4], BF16)
    cpt = ar.alloc([128, NTT, 2], F32)
    alo = ar.alloc([128, NTT, NEXP], F32)
    ahb = ar.alloc([128, NTT, NEXP], BF16)
    oh = [ar.alloc([128, NEXP, 512], BF16) for _ in range(3)]
    P.op("pool", lambda e: e.memset(lo, 0.0), writes=["lo"])
    P.op("pool", lambda e: e.memset(hi, 1.0), writes=["hi"])
    allaff = [("aff", i) for i in range(NTT)]
    for it in range(30):
        P.op("dve", lambda e: e.tensor_scalar(out=mid, in0=lo, scalar1=0.5, scalar2=None, op0=ALU.mult), reads=["lo"], writes=["mid"])
        P.op("dve", lambda e: e.scalar_tensor_tensor(out=mid, in0=hi, scalar=0.5, in1=mid, op0=ALU.mult, op1=ALU.add), reads=["hi", "mid"], writes=["mid"])
        P.op("dve", lambda e: e.tensor_tensor(out=cmpb, in0=aff_all, in1=mid.unsqueeze(1).to_broadcast([128, NTT, NEXP]), op=ALU.is_ge),
             reads=allaff + ["mid"], writes=["cmpb"])
        P.op("dve", lambda e: e.tensor_reduce(out=cntp, in_=cmpb.rearrange("p c e -> p e c"), axis=AX.X, op=ALU.add), reads=["cmpb"], writes=["cntp"])
        P.op("pe", lambda e: e.matmul(ps_f[0][:, 0:NEXP], lhsT=ones_f, rhs=cntp, start=True, stop=True), reads=["cntp", "ones_f"], writes=[PSF[0]])
        P.op("dve", lambda e: e.tensor_single_scalar(out=gef, in_=ps_f[0][:, 0:NEXP], scalar=float(CAP), op=ALU.is_ge), reads=[PSF[0]], writes=["gef", lock(PSF[0])])
        P.op("dve", lambda e: e.tensor_tensor(out=dlt, in0=mid, in1=lo, op=ALU.subtract), reads=["mid", "lo"], writes=["dlt"])
        P.op("dve", lambda e: e.tensor_tensor(out=dlt, in0=dlt, in1=gef, op=ALU.mult), reads=["dlt", "gef"], writes=["dlt"])
        P.op("dve", lambda e: e.tensor_tensor(out=lo, in0=lo, in1=dlt, op=ALU.add), reads=["lo", "dlt"], writes=["lo"])
        P.op("dve", lambda e: e.tensor_tensor(out=dlt, in0=hi, in1=mid, op=ALU.subtract), reads=["hi", "mid"], writes=["dlt"])
        P.op("dve", lambda e: e.tensor_tensor(out=dlt, in0=dlt, in1=gef, op=ALU.mult), reads=["dlt", "gef"], writes=["dlt"])
        P.op("dve", lambda e: e.tensor_tensor(out=hi, in0=mid, in1=dlt, op=ALU.add), reads=["mid", "dlt"], writes=["hi"])
    P.op("dve", lambda e: e.tensor_tensor(out=maskf, in0=aff_all, in1=lo.unsqueeze(1).to_broadcast([128, NTT, NEXP]), op=ALU.is_ge),
         reads=allaff + ["lo"], writes=["maskf"])
    P.op("dve", lambda e: e.tensor_copy(out=cmpb, in_=maskf), reads=["maskf"], writes=["cmpb"])
    P.op("pool", lambda e: e.tensor_copy(out=ones_bf, in_=ones_f), reads=["ones_f"], writes=["ones_bf"])
    P.dma("sp", lambda e: e.dma_start(out=rank[:, 0:8, :].rearrange("p a b -> p (a b)"), in_=cst["tri_f"]), writes=["rank"])
    P.op("dve", lambda e: e.tensor_tensor(out=tris_bf, in0=rank[:, 0:8, :].rearrange("p a b -> p (a b)"), in1=ident_f, op=ALU.subtract),
         reads=["rank", "ident_f"], writes=["tris_bf"])
    cm2 = cmpb.rearrange("p a b -> p (a b)")
    P.op("pe", lambda e: e.matmul(ps_f[1], lhsT=tris_bf, rhs=cm2, start=True, stop=True), reads=["tris_bf", "cmpb"], writes=[PSF[1]])
    P.op("pe", lambda e: e.matmul(ps_f[2], lhsT=ones_bf, rhs=cm2, start=True, stop=True), reads=["ones_bf", "cmpb"], writes=[PSF[2]])
    P.op("dve", lambda e: e.tensor_copy(out=rank.rearrange("p a b -> p (a b)"), in_=ps_f[1]), reads=[PSF[1], "tris_bf"], writes=["rank", lock(PSF[1])])
    P.op("dve", lambda e: e.tensor_copy(out=offs.rearrange("p a b -> p (a b)"), in_=ps_f[2]),
         reads=[PSF[2]], writes=["tot7", lock(PSF[2])])
    P.op("pool", lambda e: e.memset(cntp, 0.0), reads=["cntp"], writes=["cntp"])
    for c_ in range(NTT):
        P.op("dve", lambda e, c_=c_: e.tensor_tensor(out=rank[:, c_, :], in0=rank[:, c_, :], in1=cntp, op=ALU.add), reads=["rank", "cntp"], writes=["rank"])
        P.op("dve", lambda e, c_=c_: e.tensor_tensor(out=cntp, in0=cntp, in1=offs[:, c_, :], op=ALU.add), reads=["cntp", "tot7"], writes=["cntp"])
    P.op("dve", lambda e: e.scalar_tensor_tensor(out=rank, in0=rank, scalar=1.0, in1=maskf, op0=ALU.add, op1=ALU.mult), reads=["rank", "maskf"], writes=["rank"])
    P.op("dve", lambda e: e.tensor_scalar(out=rank, in0=rank, scalar1=-1.0, scalar2=None, op0=ALU.add), reads=["rank"], writes=["rank"])
    P.op("pool", lambda e: e.iota(iot, pattern=[[1, 512]], base=0, channel_multiplier=0, allow_small_or_imprecise_dtypes=True), writes=["iot"])
    P.op("pool", lambda e: e.iota(cpt[:, :, 0], pattern=[[1, NTT]], base=0, channel_multiplier=0, allow_small_or_imprecise_dtypes=True), writes=["cpt0"])
    P.op("pool", lambda e: e.iota(cpt[:, :, 1], pattern=[[0, NTT]], base=0, channel_multiplier=1, allow_small_or_imprecise_dtypes=True), writes=["cpt1"])
    P.op("dve", lambda e: e.tensor_copy(out=ahb, in_=aff_all), reads=allaff, writes=["ahb"])
    P.op("dve", lambda e: e.tensor_tensor(out=alo, in0=aff_all, in1=ahb, op=ALU.subtract), reads=allaff + ["ahb"], writes=["alo"])
    P.op("dve", lambda e: e.tensor_copy(out=Rv[:, :, :, 0:2], in_=cpt.unsqueeze(2).to_broadcast([128, NTT, NEXP, 2])), reads=["cpt0", "cpt1"], writes=["Rv01"])
    P.op("dve", lambda e: e.tensor_copy(out=Rv[:, :, :, 2], in_=ahb), reads=["ahb"], writes=["Rv2"])
    P.op("dve", lambda e: e.tensor_copy(out=Rv[:, :, :, 3], in_=alo), reads=["alo"], writes=["Rv3"])
    psI = ps_f[3]
    for c_ in range(NTT):
        oh_, ohk = oh[c_ % 3], f"oh{c_ % 3}"
        P.op("dve", lambda e, oh_=oh_, c_=c_: e.tensor_tensor(
            out=oh_, in0=iot.unsqueeze(1).to_broadcast([128, NEXP, 512]),
            in1=rank[:, c_, :].unsqueeze(2).to_broadcast([128, NEXP, 512]), op=ALU.is_equal),
            reads=["iot", "rank"], writes=[ohk])

        def mmI(e, oh_=oh_, c_=c_):
            ins = None
            for ex in range(NEXP):
                for j in range(4):
                    q_ = (ex * 4 + j) * 4
                    ins = e.matmul(psI[:, q_:q_ + 4], lhsT=oh_[:, ex, j * 128:(j + 1) * 128], rhs=Rv[:, c_, ex, :],
                                   start=(c_ == 0 and ex == 0 and j == 0), stop=(c_ == NTT - 1), skip_group_check=True)
            return ins
        P.op("pe", mmI, reads=[ohk, "Rv01", "Rv2", "Rv3"], writes=[PSF[3]])
    pIs = ar.alloc([128, 256], F32)
    P.op("dve", lambda e: e.tensor_copy(out=pIs, in_=psI[:, 0:256]), reads=[PSF[3]], writes=["pIs", lock(PSF[3])])
    pI = pIs.rearrange("p (q f) -> p q f", f=4)
    P.op("dve", lambda e: e.scalar_tensor_tensor(out=idx_f, in0=pI[:, :, 0], scalar=128.0, in1=pI[:, :, 1], op0=ALU.mult, op1=ALU.add),
         reads=["pIs"], writes=["idx_f"])
    P.op("dve", lambda e: e.tensor_tensor(out=gate_s, in0=pI[:, :, 2], in1=pI[:, :, 3], op=ALU.add), reads=["pIs"], writes=["gate_s"])
    P.op("dve", lambda e: e.tensor_copy(out=idx_i, in_=idx_f), reads=["idx_f"], writes=["idx_i"])
    if debug == "route":
        P.dma("sp", lambda e: e.dma_start(out=idxd, in_=idx_i), reads=["idx_i"], writes=["idxd"])
        P.dma("sp", lambda e: e.dma_start(out=gated, in_=gate_s), reads=["gate_s"], writes=["gated"])
        return finish()
    P.barrier()
    ar.release(mW)

    m8 = ar.mark()
    xg = [ar.alloc([128, D], BF16) for _ in range(2)]
    xgT2 = [ar.alloc([128, 8, CAP], BF16) for _ in range(2)]
    actT = ar.alloc([128, 16, CAP], BF16)
    sil = [ar.alloc([128, CAP], F32) for _ in range(2)]
    yg = [ar.alloc([128, D], F32) for _ in range(2)]
    n_exp = NEXP if debug != "moe1" else 1

    def gather(ex):
        xgT = xgT2[ex % 2]
        for j in range(4):
            xg_, xgk = xg[j % 2], f"xg{j % 2}"
            col = ex * 4 + j
            P.dma("pool", lambda e, xg_=xg_, col=col: e.indirect_dma_start(
                out=xg_, out_offset=None, in_=hx2d, in_offset=bass.IndirectOffsetOnAxis(ap=idx_i[:, col:col + 1], axis=0)),
                reads=["idx_i"] + [("hx2d", i) for i in range(NTT)], writes=[xgk])
            pb, pbk = ps_b[j % 2], PSB[j % 2]

            def trX(e, xg_=xg_, pb=pb):
                ins = None
                for dc in range(8):
                    ins = e.transpose(pb[:, dc * 128:(dc + 1) * 128], xg_[:, dc * 128:(dc + 1) * 128], ident_bf)
                return ins
            P.op("pe", trX, reads=[xgk, "ident_bf"], writes=[pbk])
            dst = xgT[:, :, j * 128:(j + 1) * 128]
            src = pb.rearrange("p (a b) -> p a b", b=128)
            if j % 2 == 0:
                P.op("act", lambda e, dst=dst, src=src: e.copy(out=dst, in_=src), reads=[pbk], writes=[("xgT", ex % 2, j), lock(pbk)])
            else:
                P.op("dve", lambda e, dst=dst, src=src: e.tensor_copy(out=dst, in_=src), reads=[pbk], writes=[("xgT", ex % 2, j), lock(pbk)])

    gather(0)
    for ex in range(n_exp):
        xgT = xgT2[ex % 2]
        w2_, w2k = w2b[ex % 2], f"w2b{ex % 2}"
        for fb in range(4):
            q_ = (ex * 4 + fb) % 2
            w1_, w1k = w1b[q_], f"w1b{q_}"
            w3_, w3k = w3b_[q_], f"w3b{q_}"
            for fc in range(4):
                p1, p1k = ps_f[(fc % 2) * 2], PSF[(fc % 2) * 2]
                p3, p3k = ps_f[(fc % 2) * 2 + 1], PSF[(fc % 2) * 2 + 1]

                def mmH(e, w1_=w1_, w3_=w3_, fc=fc, p1=p1, p3=p3, xgT=xgT):
                    ins = None
                    for dc in range(8):
                        e.matmul(p1, lhsT=w1_[:, dc, fc * 128:(fc + 1) * 128], rhs=xgT[:, dc, :], start=(dc == 0), stop=(dc == 7))
                    for dc in range(8):
                        ins = e.matmul(p3, lhsT=w3_[:, dc, fc * 128:(fc + 1) * 128], rhs=xgT[:, dc, :], start=(dc == 0), stop=(dc == 7))
                    return ins
                P.op("pe", mmH, reads=[w1k, w3k] + [("xgT", ex % 2, j) for j in range(4)], writes=[p1k, p3k])
                sl_, slk = sil[fc % 2], f"sil{fc % 2}"
                P.op("act", lambda e, sl_=sl_, p1=p1: e.activation(out=sl_, in_=p1, func=AF.Silu), reads=[p1k], writes=[slk, lock(p1k)])
                P.op("dve", lambda e, sl_=sl_, p3=p3, fb=fb, fc=fc: e.tensor_tensor(out=actT[:, fb * 4 + fc, :], in0=p3, in1=sl_, op=ALU.mult),
                     reads=[p3k, slk], writes=[("actT", fb * 4 + fc), lock(p3k)])
            if fb + 2 < 4:
                load_w13(ex, fb + 2)
        if ex + 1 < n_exp:
            load_w2(ex + 1)
            gather(ex + 1)
            load_w13(ex + 1, 0)
            load_w13(ex + 1, 1)
        for j in range(4):
            yg_, ygk = yg[j % 2], f"yg{j % 2}"
            col = ex * 4 + j
            for half in range(2):
                py, pyk = ps_f[4 + half], PSF[4 + half]
                hs = slice(half * 512, (half + 1) * 512)

                def mmY(e, j=j, hs=hs, py=py, w2_=w2_):
                    ins = None
                    for fc in range(16):
                        ins = e.matmul(py, lhsT=actT[:, fc, j * 128:(j + 1) * 128], rhs=w2_[:, fc, hs], start=(fc == 0), stop=(fc == 15))
                    return ins
                P.op("pe", mmY, reads=[("actT", fc) for fc in range(16)] + [(w2k, 0), (w2k, 1)], writes=[pyk])
                P.op("dve", lambda e, yg_=yg_, py=py, hs=hs, col=col: e.scalar_tensor_tensor(
                    out=yg_[:, hs], in0=py, scalar=gate_s[:, col:col + 1], in1=bc["ada5"][:, hs], op0=ALU.mult, op1=ALU.mult),
                    reads=[pyk, "gate_s", ("bc", "ada5", 0), ("bc", "ada5", 1)], writes=[(ygk, half), lock(pyk)])
            P.dma("pool", lambda e, yg_=yg_, col=col: e.indirect_dma_start(
                out=x1d, out_offset=bass.IndirectOffsetOnAxis(ap=idx_i[:, col:col + 1], axis=0), in_=yg_, in_offset=None,
                compute_op=ALU.add), reads=[(ygk, 0), (ygk, 1), "idx_i"], writes=["x1d_sc"])
    if debug in ("moe", "moe1"):
        return finish()
    P.barrier()
    ar.release(m8)

    xf = [ar.alloc([128, D], F32) for _ in range(3)]
    of = [ar.alloc([128, D], F32) for _ in range(2)]
    junk9 = ar.alloc([128, D], BF16)
    out_t = out.rearrange("(n p) d -> n p d", p=128)
    for i in range(NTT):
        xx, xk = xf[i % 3], f"xf{i % 3}"
        oo, ok_ = of[i % 2], f"of{i % 2}"
        P.dma("sp", lambda e, xx=xx, i=i: e.dma_start(out=xx, in_=x1d[i * 128:(i + 1) * 128, :]), reads=["x1d_sc", ("x1d", i)], writes=[xk])
        P.op("act", lambda e, xx=xx, i=i: e.activation(out=junk9, in_=xx, func=AF.Square, accum_out=sc9[:, i:i + 1]), reads=[xk], writes=["junk9", ("sc9", i)])
        P.op("dve", lambda e, i=i: e.tensor_scalar(out=sc9[:, i:i + 1], in0=sc9[:, i:i + 1], scalar1=1.0 / D, scalar2=RMS_EPS, op0=ALU.mult, op1=ALU.add),
             reads=[("sc9", i)], writes=[("sc9", i)])
        P.op("act", lambda e, i=i: e.activation(out=sc9[:, i:i + 1], in_=sc9[:, i:i + 1], func=AF.Sqrt), reads=[("sc9", i)], writes=[("sc9", i)])
        P.op("dve", lambda e, i=i: e.reciprocal(out=sc9[:, i:i + 1], in_=sc9[:, i:i + 1]), reads=[("sc9", i)], writes=[("sc9", i)])
        P.op("dve", lambda e, i=i, xx=xx, oo=oo: e.scalar_tensor_tensor(out=oo, in0=xx, scalar=sc9[:, i:i + 1], in1=bc["fing"], op0=ALU.mult, op1=ALU.mult),
             reads=[xk, ("sc9", i), ("bc", "fing", 0), ("bc", "fing", 1)], writes=[ok_])
        P.dma("sp", lambda e, oo=oo, i=i: e.dma_start(out=out_t[i], in_=oo), reads=[ok_], writes=[("out", i)])

    P.final_wait("sp")
    P.run()
    return nc


def _in_maps(inp, ncores=8):
    hc = _host_consts()
    f = lambda a: np.ascontiguousarray(np.asarray(a, np.float32))
    b_in = f(inp["b_in"][0])
    cols = np.zeros((128, NCOLS), np.float32)
    cols[:, COLS["gmix"]:COLS["gmix"] + 8] = _col(inp["norm_mix_g"][0])
    cols[:, COLS["bada"]:COLS["bada"] + 48] = _col(inp["b_ada"][0])
    for j, (c0, n) in enumerate(JOBS):
        cols[0:n, COLS["bin"] + j] = b_in[c0:c0 + n]
    hyc = f(inp["hy_conv"][0])
    for t in range(3):
        cols[:, COLS["hyconv"] + t * 12:COLS["hyconv"] + (t + 1) * 12] = _col(hyc[t])
    mlc = f(inp["ml_conv"][0])
    for t in range(3):
        cols[:, COLS["mlconv"] + t * 8:COLS["mlconv"] + (t + 1) * 8] = _col(mlc[t])
    sk = f(inp["hy_skip"][0])
    for o in range(2):
        cols[:, COLS["hyskip"] + o * 4:COLS["hyskip"] + (o + 1) * 4] = _col(sk[o])
    cols[:, COLS["mlnorm"]:COLS["mlnorm"] + 4] = _col(inp["ml_norm_g"][0])
    cols[:, COLS["delta"]:COLS["delta"] + 4] = _delta_col()
    cols[0:64, COLS["fb1"]] = f(inp["hy_f_b1"][0])
    cols[0:64, COLS["fb2"]] = f(inp["hy_f_b2"][0])
    rows = np.concatenate([f(inp["norm_ffn_g"][0]), f(inp["final_norm_g"])])[None, :]
    shared = {
        "c_ctx": f(inp["c_ctx"]).reshape(128, 8),
        "w_ada": f(inp["w_ada"][0]),
        "bada_row": np.ascontiguousarray(np.broadcast_to(f(inp["b_ada"][0])[None, :], (2, 6 * D))),
        "cols": cols, "rows": np.ascontiguousarray(rows),
        "w_in": f(inp["w_in"][0]),
        "f_w1": f(inp["hy_f_w1"][0]), "f_w2": f(inp["hy_f_w2"][0]), "f_w3": f(inp["hy_f_w3"][0]),
        "w_bh": f(inp["w_branch_hy"][0]), "w_bm": f(inp["w_branch_ml"][0]), "w_out": f(inp["w_out"][0]),
        "w_rt": f(inp["w_router"][0]),
        "w_e1": f(inp["w_exp1"][0]), "w_e3": f(inp["w_exp3"][0]), "w_e2": f(inp["w_exp2"][0]),
    }
    for name, arr in hc.items():
        shared["k_" + name] = arr
    maps = []
    for b in range(ncores):
        m = dict(shared)
        m["x"] = f(inp["x"][b])
        m["c"] = f(inp["c"][b]).reshape(128, 8)
        m["ctx"] = f(inp["ctx"][b])
        maps.append(m)
    return maps


def kernel(**inputs):
    nc = build()
    maps = _in_maps(inputs)
    res = run_bass_kernel_spmd(nc, maps, core_ids=list(range(8)))
    return np.stack([np.asarray(r["out"], np.float32) for r in res.results], axis=0)
```
